# Optimizing a Trainium2 kernel written in Bass

```python
import jax, jax.numpy as jnp
from jax import lax
import numpy as np

D_MODEL = 1024
BATCH = 1
SEQ = 16384
DEPTH = 1
DEC_BATCH = 128
DEC_SEQ = 1
PAST_LEN = 8192
PAGE_SIZE = 128

HEAD_DIM = 64
A_HEADS = 8
A_WIDTH = A_HEADS * HEAD_DIM
B_GROUPS = 8
B_GROUP_DIM = 64
B_WIDTH = B_GROUPS * B_GROUP_DIM
MIX_WIDTH = A_WIDTH + B_WIDTH
IN_COLS = 3 * A_WIDTH + 2 * B_WIDTH
PATTERNS = ((128, 1), (512, 4), (2048, 16))
MAX_WINDOW = 2048
STRIDED_BLOCK = 128
ROT_DIM = HEAD_DIM // 4
ROPE_THETA = 500000.0
CHUNK = 128
PEER_HEADS = 8
PEER_NKEYS = 128
PEER_EXPERTS = PEER_NKEYS * PEER_NKEYS
PEER_QDIM = 256
PEER_HALF = PEER_QDIM // 2
PEER_TOPK = 16
PEER_BLOCK = 128
PLE_DIM = 256
EPS = 1e-6

kernel_name = "hymba_dilated_gmlp_peer_step"


def rmsnorm(x, g):
    xf = x.astype(jnp.float32)
    y = xf * lax.rsqrt(jnp.mean(xf * xf, axis=-1, keepdims=True) + EPS)
    return (y * g.astype(jnp.float32)).astype(x.dtype)


def rope(x, pos):
    half = ROT_DIM // 2
    inv = ROPE_THETA ** (-jnp.arange(0, ROT_DIM, 2, dtype=jnp.float32) / ROT_DIM)
    ang = pos.astype(jnp.float32)[:, None] * inv[None, :]
    cos = jnp.cos(ang)[:, None, :]
    sin = jnp.sin(ang)[:, None, :]
    xr = x[..., :ROT_DIM].astype(jnp.float32)
    x1, x2 = xr[..., :half], xr[..., half:]
    rot = jnp.concatenate([x1 * cos - x2 * sin, x2 * cos + x1 * sin], axis=-1)
    return jnp.concatenate([rot.astype(x.dtype), x[..., ROT_DIM:]], axis=-1)


def project(n, w_in, q_norm_g, k_norm_g, pos):
    bsz, t, _ = n.shape
    z = n @ w_in
    q, k, v, u_b, v_b = jnp.split(z, [A_WIDTH, 2 * A_WIDTH, 3 * A_WIDTH, 3 * A_WIDTH + B_WIDTH], axis=-1)
    heads = lambda a: a.reshape(bsz, t, A_HEADS, HEAD_DIM)
    q = rope(rmsnorm(heads(q), q_norm_g), pos)
    k = rope(rmsnorm(heads(k), k_norm_g), pos)
    return q, k, heads(v), jax.nn.gelu(u_b), jax.nn.gelu(v_b)


def combine_patterns(outs, lses):
    alpha = jax.nn.softmax(jnp.stack(lses, axis=0), axis=0)
    return jnp.sum(alpha[..., None] * jnp.stack(outs, axis=0), axis=0)


def _to_strided(x, dil, s_pad):
    b, s, h, c = x.shape
    x = jnp.pad(x, ((0, 0), (0, s_pad - s), (0, 0), (0, 0)))
    length = s_pad // dil
    x = x.reshape(b, length, dil, h, c).transpose(0, 2, 3, 1, 4)
    return x.reshape(b, dil, h, length // STRIDED_BLOCK, STRIDED_BLOCK, c)


def _with_prev(t):
    prev = jnp.pad(t[:, :, :, :-1], ((0, 0), (0, 0), (0, 0), (1, 0), (0, 0), (0, 0)))
    return jnp.concatenate([prev, t], axis=4)


def dilated_attention_prompt(q, k, v):
    b, s, h, c = q.shape
    scale = c ** -0.5
    qi = jnp.arange(STRIDED_BLOCK)[:, None]
    kj = jnp.arange(2 * STRIDED_BLOCK)[None, :]
    dist = STRIDED_BLOCK + qi - kj
    outs, lses = [], []
    for window, dil in PATTERNS:
        n_back = window // dil
        unit = dil * STRIDED_BLOCK
        s_pad = -(-s // unit) * unit
        nb = s_pad // unit
        qs = _to_strided(q, dil, s_pad).astype(jnp.float32)
        kc = _with_prev(_to_strided(k, dil, s_pad)).astype(jnp.float32)
        vc = _with_prev(_to_strided(v, dil, s_pad)).astype(jnp.float32)
        scores = jnp.einsum('bdhnqc,bdhnkc->bdhnqk', qs, kc) * scale
        blk = jnp.arange(nb)[:, None, None]
        valid = (dist >= 0) & (dist <= n_back) & ((blk - 1) * STRIDED_BLOCK + kj >= 0)
        scores = jnp.where(valid, scores, -jnp.inf)
        m = jnp.max(scores, axis=-1, keepdims=True)
        e = jnp.exp(scores - m)
        den = jnp.sum(e, axis=-1)
        o = jnp.einsum('bdhnqk,bdhnkc->bdhnqc', e, vc) / den[..., None]
        lse = m[..., 0] + jnp.log(den)
        length = s_pad // dil
        o = o.reshape(b, dil, h, length, c).transpose(0, 3, 1, 2, 4).reshape(b, s_pad, h, c)[:, :s]
        lse = lse.reshape(b, dil, h, length).transpose(0, 3, 1, 2).reshape(b, s_pad, h)[:, :s]
        outs.append(o)
        lses.append(lse)
    return combine_patterns(outs, lses).astype(q.dtype)


def dilated_attention_sample(q, k_all, v_all, buf_start):
    t = q.shape[1]
    scale = q.shape[-1] ** -0.5
    t_idx = jnp.arange(t)[:, None]
    qf = q.astype(jnp.float32)
    outs, lses = [], []
    for window, dil in PATTERNS:
        n_back = window // dil
        kk = jnp.arange(n_back + 1)[None, :]
        pos = PAST_LEN + t_idx - kk * dil
        idx = pos - buf_start
        valid = (pos >= 0) & (idx >= 0)
        idx_c = jnp.clip(idx, 0, k_all.shape[1] - 1)
        kg = jnp.take(k_all, idx_c, axis=1).astype(jnp.float32)
        vg = jnp.take(v_all, idx_c, axis=1).astype(jnp.float32)
        scores = jnp.einsum('bthc,btkhc->bthk', qf, kg) * scale
        scores = jnp.where(valid[None, :, None, :], scores, -jnp.inf)
        m = jnp.max(scores, axis=-1, keepdims=True)
        e = jnp.exp(scores - m)
        den = jnp.sum(e, axis=-1)
        o = jnp.einsum('bthk,btkhc->bthc', e, vg) / den[..., None]
        outs.append(o)
        lses.append(m[..., 0] + jnp.log(den))
    return combine_patterns(outs, lses).astype(q.dtype)


def spatial_gate(u, v, w_s, b_s, v_norm_g):
    bsz, t, _ = v.shape
    vn = rmsnorm(v.reshape(bsz, t, B_GROUPS, B_GROUP_DIM), v_norm_g)
    t_pad = -(-t // CHUNK) * CHUNK
    vc = jnp.pad(vn, ((0, 0), (0, t_pad - t), (0, 0), (0, 0))).reshape(bsz, t_pad // CHUNK, CHUNK, B_GROUPS, B_GROUP_DIM)
    wm = w_s * jnp.tril(jnp.ones((CHUNK, CHUNK), dtype=w_s.dtype))
    mixed = jnp.einsum('gij,bnjgc->bnigc', wm, vc) + b_s.T[:, :, None]
    mixed = mixed.reshape(bsz, t_pad, B_GROUPS, B_GROUP_DIM)[:, :t]
    out = u.reshape(bsz, t, B_GROUPS, B_GROUP_DIM) * mixed
    return out.reshape(bsz, t, B_WIDTH), vn


def merge_groups(att, gm, out_norm_a_g, out_norm_b_g, w_out):
    bsz, t = att.shape[:2]
    a = rmsnorm(att.reshape(bsz, t, A_WIDTH), out_norm_a_g)
    b = rmsnorm(gm, out_norm_b_g)
    return jnp.concatenate([a, b], axis=-1) @ w_out


def peer(x, w_query, sub_keys, expert_u, expert_v):
    n_tok = x.shape[0]
    n_pad = -(-n_tok // PEER_BLOCK) * PEER_BLOCK
    xb = jnp.pad(x, ((0, n_pad - n_tok), (0, 0))).reshape(n_pad // PEER_BLOCK, PEER_BLOCK, D_MODEL)

    def block(xt):
        q = (xt @ w_query).reshape(PEER_BLOCK, PEER_HEADS, 2, PEER_HALF).astype(jnp.float32)
        s = jnp.einsum('thpc,hpkc->thpk', q, sub_keys.astype(jnp.float32))
        top_s, top_i = lax.top_k(s, PEER_TOPK)
        cand_s = top_s[:, :, 0, :, None] + top_s[:, :, 1, None, :]
        cand_i = top_i[:, :, 0, :, None] * PEER_NKEYS + top_i[:, :, 1, None, :]
        cand_s = cand_s.reshape(PEER_BLOCK, PEER_HEADS, PEER_TOPK * PEER_TOPK)
        cand_i = cand_i.reshape(PEER_BLOCK, PEER_HEADS, PEER_TOPK * PEER_TOPK)
        best_s, best_pos = lax.top_k(cand_s, PEER_TOPK)
        eidx = jnp.take_along_axis(cand_i, best_pos, axis=-1)
        g = jax.nn.softmax(best_s, axis=-1)
        act = jax.nn.gelu(jnp.einsum('thkd,td->thk', expert_u[eidx], xt).astype(jnp.float32))
        return jnp.einsum('thk,thkd->td', (g * act).astype(x.dtype), expert_v[eidx])

    out = lax.map(block, xb)
    return out.reshape(n_pad, D_MODEL)[:n_tok]


def channel_and_ple(h, p, norm2_g, w_query, sub_keys, expert_u, expert_v, norm3_g, w_gate, w_proj):
    bsz, t, _ = h.shape
    n2 = rmsnorm(h, norm2_g)
    h = h + peer(n2.reshape(bsz * t, D_MODEL), w_query, sub_keys, expert_u, expert_v).reshape(bsz, t, D_MODEL)
    n3 = rmsnorm(h, norm3_g)
    return h + jax.nn.sigmoid(n3 @ w_gate) * (p.astype(h.dtype) @ w_proj)


def setup_inputs(seed: int = 0) -> dict:
    key = jax.random.key(seed)
    ks = jax.random.split(key, 24)
    nrm = lambda k, shape, sc: jax.random.normal(k, shape, jnp.float32) * sc
    gain = lambda k, shape: 1.0 + 0.02 * jax.random.normal(k, shape, jnp.float32)
    buf = min(MAX_WINDOW, PAST_LEN)
    return {
        "x_prompt": nrm(ks[0], (BATCH, SEQ, D_MODEL), 1.0),
        "x_sample": nrm(ks[1], (DEC_BATCH, DEC_SEQ, D_MODEL), 1.0),
        "cache_k": nrm(ks[2], (DEPTH, DEC_BATCH, buf, A_HEADS, HEAD_DIM), 1.0),
        "cache_v": nrm(ks[3], (DEPTH, DEC_BATCH, buf, A_HEADS, HEAD_DIM), 1.0),
        "p_prompt": nrm(ks[4], (DEPTH, BATCH, SEQ, PLE_DIM), 1.0),
        "p_sample": nrm(ks[5], (DEPTH, DEC_BATCH, DEC_SEQ, PLE_DIM), 1.0),
        "norm1_g": gain(ks[6], (DEPTH, D_MODEL)),
        "w_in": nrm(ks[7], (DEPTH, D_MODEL, IN_COLS), D_MODEL ** -0.5),
        "q_norm_g": gain(ks[8], (DEPTH, HEAD_DIM)),
        "k_norm_g": gain(ks[9], (DEPTH, HEAD_DIM)),
        "v_norm_g": gain(ks[10], (DEPTH, B_GROUPS, B_GROUP_DIM)),
        "spatial_w": nrm(ks[11], (DEPTH, B_GROUPS, CHUNK, CHUNK), CHUNK ** -0.5),
        "spatial_b": gain(ks[12], (DEPTH, B_GROUPS, CHUNK)),
        "out_norm_a_g": gain(ks[13], (DEPTH, A_WIDTH)),
        "out_norm_b_g": gain(ks[14], (DEPTH, B_WIDTH)),
        "w_out": nrm(ks[15], (DEPTH, MIX_WIDTH, D_MODEL), MIX_WIDTH ** -0.5),
        "norm2_g": gain(ks[16], (DEPTH, D_MODEL)),
        "peer_w_query": nrm(ks[17], (DEPTH, D_MODEL, PEER_HEADS * PEER_QDIM), D_MODEL ** -0.5),
        "peer_sub_keys": nrm(ks[18], (DEPTH, PEER_HEADS, 2, PEER_NKEYS, PEER_HALF), PEER_HALF ** -0.5),
        "peer_u": nrm(ks[19], (DEPTH, PEER_EXPERTS, D_MODEL), D_MODEL ** -0.5),
        "peer_v": nrm(ks[20], (DEPTH, PEER_EXPERTS, D_MODEL), PEER_HEADS ** -0.5),
        "norm3_g": gain(ks[21], (DEPTH, D_MODEL)),
        "ple_w_gate": nrm(ks[22], (DEPTH, D_MODEL, D_MODEL), D_MODEL ** -0.5),
        "ple_w_proj": nrm(ks[23], (DEPTH, PLE_DIM, D_MODEL), PLE_DIM ** -0.5),
    }


def reference(x_prompt, x_sample, cache_k, cache_v, p_prompt, p_sample, norm1_g, w_in, q_norm_g, k_norm_g, v_norm_g, spatial_w, spatial_b, out_norm_a_g, out_norm_b_g, w_out, norm2_g, peer_w_query, peer_sub_keys, peer_u, peer_v, norm3_g, ple_w_gate, ple_w_proj):
    s = x_prompt.shape[1]
    t = x_sample.shape[1]
    pos_p = jnp.arange(s, dtype=jnp.int32)
    pos_s = PAST_LEN + jnp.arange(t, dtype=jnp.int32)
    keep = min(MAX_WINDOW, s)
    buf_start = PAST_LEN - cache_k.shape[2]
    hp, hs = x_prompt, x_sample
    nkp, nvp, nks, nvs, ncv = [], [], [], [], []
    for l in range(DEPTH):
        q, k, v, ub, vb = project(rmsnorm(hp, norm1_g[l]), w_in[l], q_norm_g[l], k_norm_g[l], pos_p)
        att = dilated_attention_prompt(q, k, v)
        gm, _ = spatial_gate(ub, vb, spatial_w[l], spatial_b[l], v_norm_g[l])
        hp = hp + merge_groups(att, gm, out_norm_a_g[l], out_norm_b_g[l], w_out[l])
        hp = channel_and_ple(hp, p_prompt[l], norm2_g[l], peer_w_query[l], peer_sub_keys[l], peer_u[l], peer_v[l], norm3_g[l], ple_w_gate[l], ple_w_proj[l])
        nkp.append(k[:, s - keep:])
        nvp.append(v[:, s - keep:])
        q, k, v, ub, vb = project(rmsnorm(hs, norm1_g[l]), w_in[l], q_norm_g[l], k_norm_g[l], pos_s)
        k_all = jnp.concatenate([cache_k[l].astype(k.dtype), k], axis=1)
        v_all = jnp.concatenate([cache_v[l].astype(v.dtype), v], axis=1)
        att = dilated_attention_sample(q, k_all, v_all, buf_start)
        gm, vn = spatial_gate(ub, vb, spatial_w[l], spatial_b[l], v_norm_g[l])
        hs = hs + merge_groups(att, gm, out_norm_a_g[l], out_norm_b_g[l], w_out[l])
        hs = channel_and_ple(hs, p_sample[l], norm2_g[l], peer_w_query[l], peer_sub_keys[l], peer_u[l], peer_v[l], norm3_g[l], ple_w_gate[l], ple_w_proj[l])
        nks.append(k)
        nvs.append(v)
        ncv.append(vn)
    return (hp, hs, jnp.stack(nkp), jnp.stack(nvp), jnp.stack(nks), jnp.stack(nvs), jnp.stack(ncv))
```

```python
import numpy as np
from contextlib import ExitStack
import concourse.bass as bass
import concourse.mybir as mybir
from concourse.bass_utils import run_bass_kernel_spmd

F32 = mybir.dt.float32
BF16 = mybir.dt.bfloat16
I32 = mybir.dt.int32
U32 = mybir.dt.uint32
AF = mybir.ActivationFunctionType
ALU = mybir.AluOpType
AX = mybir.AxisListType

ENGS = ("pe", "act", "dve", "pool", "sp")
EPS = 1e-6
NSLOT = 128
CUT = 99


class Trk:
    __slots__ = ("w", "r", "dsem", "excl")

    def __init__(self):
        self.w = None
        self.r = []
        self.dsem = None
        self.excl = False


class DSem:
    __slots__ = ("sem", "n", "bg")

    def __init__(self, sem):
        self.sem = sem
        self.n = 0
        self.bg = False


class Sched:
    def __init__(self, nc, stack):
        self.nc = nc
        self.stack = stack
        self.ops = {e: [] for e in ENGS}
        self.cnt = {e: 0 for e in ENGS}
        self.sem = {e: stack.enter_context(nc.semaphore("es_" + e)) for e in ENGS}
        self.seen = {e: {} for e in ENGS}
        self.nds = 0
        self.final = []
        self.dsems = []
        self.pending = {e: [] for e in ENGS}

    def new_dsem(self):
        self.nds += 1
        d = DSem(self.stack.enter_context(self.nc.semaphore("ds%d" % self.nds)))
        self.dsems.append(d)
        return d

    def barrier(self, include_bg=False):
        evs = [(self.sem[e], self.cnt[e]) for e in ENGS if self.cnt[e] > 0]
        evs += [(d.sem, 16 * d.n) for d in self.dsems if d.n > 0 and (include_bg or not d.bg)]
        for e in ENGS:
            own = id(self.sem[e])
            for s_, v in evs:
                k = id(s_)
                if e == "pe" and k == own:
                    continue
                if self.seen[e].get(k, 0) >= v:
                    continue
                self.seen[e][k] = v
                self.pending[e].append((s_, v))

    def _deps(self, eng, reads, writes):
        need = {}

        def add(ev):
            if ev is None:
                return
            s, v = ev
            k = id(s)
            if k not in need or need[k][1] < v:
                need[k] = (s, v)
        for t in reads:
            add(t.w)
        for t in writes:
            add(t.w)
            for ev in t.r:
                add(ev)
        waits = []
        seen = self.seen[eng]
        own = id(self.sem[eng])
        for k, (s, v) in need.items():
            if eng == "pe" and k == own:
                continue
            if seen.get(k, 0) >= v:
                continue
            seen[k] = v
            waits.append((s, v))
        if self.pending[eng]:
            waits = self.pending[eng] + waits
            self.pending[eng] = []
        return waits

    def _mark(self, ev, reads, writes):
        for t in writes:
            t.w = ev
            t.r = []
        for t in reads:
            if t not in writes:
                t.r.append(ev)
                if len(t.r) > 64:
                    t.r = t.r[-48:]

    def op(self, eng, fn, reads=(), writes=()):
        ex = [t for t in reads if t.excl and t not in writes]
        if ex:
            writes = list(writes) + ex
        waits = self._deps(eng, reads, writes)
        self.cnt[eng] += 1
        ev = (self.sem[eng], self.cnt[eng])
        self.ops[eng].append((waits, fn, (self.sem[eng], 1)))
        self._mark(ev, reads, writes)
        return ev

    def dma(self, q, fn, reads=(), writes=(), dsem=None, final=False):
        if dsem is None:
            host = writes[0] if writes else reads[0]
            if host.dsem is None:
                host.dsem = self.new_dsem()
            dsem = host.dsem
        waits = self._deps(q, reads, writes)
        if dsem.n > 0:
            k = id(dsem.sem)
            v = 16 * dsem.n
            if self.seen[q].get(k, 0) < v:
                self.seen[q][k] = v
                waits.append((dsem.sem, v))
        dsem.n += 1
        ev = (dsem.sem, 16 * dsem.n)
        self.ops[q].append((waits, fn, (dsem.sem, 16)))
        self._mark(ev, reads, writes)
        if final:
            self.final.append(ev)
        return ev

    def emit(self):
        nc = self.nc
        fin = {}
        for s, v in self.final:
            k = id(s)
            if k not in fin or fin[k][1] < v:
                fin[k] = (s, v)
        finals = list(fin.values())
        with nc.Block() as block:
            def run(engname):
                def body(e):
                    for waits, fn, (s, amt) in self.ops[engname]:
                        for ws, wv in waits:
                            e.wait_ge(ws, wv)
                        fn(e).then_inc(s, amt)
                    if engname == "sp":
                        for ws, wv in finals:
                            e.wait_ge(ws, wv)
                return body
            block.tensor(run("pe"))
            block.scalar(run("act"))
            block.vector(run("dve"))
            block.gpsimd(run("pool"))
            block.sync(run("sp"))


def fv(ap, dims):
    base = ap.ap
    return bass.AP(ap.tensor, ap.offset, [list(base[0])] + [list(d) for d in dims])


class Buf:
    def __init__(self, t):
        self.t = t
        self.k = Trk()


def build(dbg=None, slots1=None, p2=None, p3=None):
    dbg = dbg or {}
    slots1 = list(range(33)) if slots1 is None else slots1
    p2 = list(range(17)) if p2 is None else p2
    p3 = list(range(17)) if p3 is None else p3
    nc = bass.Bass("TRN2", target_bir_lowering=False)

    def din(name, shape, dt=F32):
        return nc.dram_tensor(name, list(shape), dt, kind="ExternalInput").ap()

    def dout(name, shape, dt=F32):
        return nc.dram_tensor(name, list(shape), dt, kind="ExternalOutput").ap()

    xall = din("xall", [33 * 128, 1024]); pall = din("pall", [17 * 128, 256]); cs_d = din("cs", [33 * 128, 16])
    flag_d = din("flag", [128, 1]); ck = din("ck", [16, 2048, 512]); cv = din("cv", [16, 2048, 512])
    ident_d = din("ident", [128, 128]); mask_d = din("maskT", [128, 17 * 128]); sel_d = din("sel", [128, 256])
    iota_d = din("iota16", [128, 16]); tri_d = din("triT", [128, 128])
    g1T_d = din("g1T", [128, 8]); gabT_d = din("gabT", [128, 8]); g2T_d = din("g2T", [128, 8]); g3T_d = din("g3T", [128, 8])
    g2_d = din("g2row", [1, 1024]); gqk_d = din("gqk", [1, 1024]); gv_d = din("gv", [1, 512])
    bsT_d = din("bsT", [128, 8]); w00_d = din("w00", [1, 8]); b0_d = din("b0", [1, 8])
    w_in = din("w_in", [1024, 2560]); wsT_d = din("wsT", [128, 8 * 128]); w_out = din("w_out", [1024, 1024])
    wqT_d = din("wqT", [2048, 1024]); skT_d = din("skT", [128, 16 * 128])
    peer_u = din("peer_u", [16384, 1024]); peer_v = din("peer_v", [16384, 1024])
    w_gate = din("w_gate", [1024, 1024]); w_proj = din("w_proj", [256, 1024])

    y_own = dout("y_own", [2048, 1024]); y_smp = dout("y_smp", [16, 1024])
    k_own = dout("k_own", [2048, 512]); v_own = dout("v_own", [2048, 512])
    ks_o = dout("ks", [16, 512]); vs_o = dout("vs", [16, 512]); cvs_o = dout("cvs", [16, 512])
    dbg_o = {k: dout("dbg_" + k, shp) for k, shp in dbg.items()}

    h1_d = nc.dram_tensor("h1s", [17 * 128, 1024], F32, kind="Internal").ap()
    qs_d = nc.dram_tensor("qsc", [1, 16 * 512], F32, kind="Internal").ap()
    ptab = nc.dram_tensor("ptab", [16384, 2048], BF16, kind="Internal").ap()

    with ExitStack() as st0:
        S = Sched(nc, st0)

        def sbuf(st, name, shape, dt=F32):
            return Buf(st.enter_context(nc.sbuf_tensor("sb_" + name, list(shape), dt)))

        def K(bufs):
            return [b.k for b in bufs]

        def act(out, in_, func, r, w, **kw):
            S.op("act", lambda e: e.activation(out=out, in_=in_, func=func, **kw), K(r), K(w))

        def tt(eng, out, in0, in1, op, r, w):
            S.op(eng, lambda e: e.tensor_tensor(out=out, in0=in0, in1=in1, op=op), K(r), K(w))

        def ts(eng, out, in0, s1, s2, op0, op1, r, w):
            S.op(eng, lambda e: e.tensor_scalar(out=out, in0=in0, scalar1=s1, scalar2=s2, op0=op0, op1=op1), K(r), K(w))

        def tss(eng, out, in_, scalar, op, r, w):
            S.op(eng, lambda e: e.tensor_single_scalar(out=out, in_=in_, scalar=scalar, op=op), K(r), K(w))

        def stt(eng, out, in0, scalar, in1, op0, op1, r, w, accum_out=None):
            if accum_out is None:
                S.op(eng, lambda e: e.scalar_tensor_tensor(out=out, in0=in0, scalar=scalar, in1=in1, op0=op0, op1=op1), K(r), K(w))
            else:
                S.op(eng, lambda e: e.scalar_tensor_tensor(out=out, in0=in0, scalar=scalar, in1=in1, op0=op0, op1=op1, accum_out=accum_out), K(r), K(w))

        def cp(eng, out, in_, r, w):
            S.op(eng, lambda e: e.tensor_copy(out=out, in_=in_), K(r), K(w))

        def mset(eng, out, val, w):
            S.op(eng, lambda e: e.memset(out, val), [], K(w))

        def red(eng, out, in_, op, r, w):
            S.op(eng, lambda e: e.tensor_reduce(out=out, in_=in_, axis=AX.X, op=op), K(r), K(w))

        def recip(out, in_, r, w):
            S.op("dve", lambda e: e.reciprocal(out=out, in_=in_), K(r), K(w))

        def mm(out, lhsT, rhs, start, stop, r, w):
            S.op("pe", lambda e: e.matmul(out, lhsT=lhsT, rhs=rhs, start=start, stop=stop), K(r), K(w))

        def tr(out, in_, ident, r, w):
            S.op("pe", lambda e: e.transpose(out=out, in_=in_, identity=ident), K(r), K(w))

        def dma(q, out, in_, r, w, final=False, dsem=None):
            S.dma(q, lambda e: e.dma_start(out=out, in_=in_), K(r), K(w), final=final, dsem=dsem)

        def dbgout(name, ap, b):
            if name in dbg_o:
                dma("sp", dbg_o[name], ap, [b], [], final=True)

        ident = sbuf(st0, "ident", [128, 128]); identb = sbuf(st0, "identb", [128, 128], BF16)
        iota16 = sbuf(st0, "iota16", [128, 16])
        rstd2_all = sbuf(st0, "rstd2_all", [128, 17])
        dma("sp", ident.t[:], ident_d, [], [ident])
        dma("sp", iota16.t[:], iota_d, [], [iota16])
        act(identb.t[:], ident.t[:], AF.Copy, [ident], [identb])

        PS = [Buf(st0.enter_context(nc.psum_tensor("ps%d" % i, [128, 1024], F32))) for i in range(4)]
        bank = [Buf(None) for _ in range(8)]
        for b_ in bank:
            b_.k.excl = True

        def pbank(i, P=128, lo=0, hi=512):
            return PS[i // 2].t[0:P, (i % 2) * 512 + lo:(i % 2) * 512 + hi]

        def pbank_b(i):
            return PS[i // 2].t[:].bitcast(BF16)[:, (i % 2) * 1024:(i % 2) * 1024 + 1024]

        def rms_scale(st_small, src_ap, P, n, r, name):
            ssq, rs, junk = st_small
            mset("dve", ssq.t[0:P, :], 0.0, [ssq])
            act(junk.t[0:P, 0:n], src_ap, AF.Square, r + [ssq], [junk, ssq], accum_out=ssq.t[0:P, :])
            act(rs.t[0:P, :], ssq.t[0:P, :], AF.Sqrt, [ssq], [rs], bias=EPS, scale=1.0 / n)
            recip(rs.t[0:P, :], rs.t[0:P, :], [rs], [rs])
            return rs

        def load_weight(st_tmp, stage, w_dram, n_chunks, ncols, dst, gcol):
            for c in range(n_chunks):
                sb_ = stage[c % 2]
                dma("sp", sb_.t[:, 0:ncols], w_dram[c * 128:(c + 1) * 128, :], [], [sb_])
                if gcol is None:
                    act(dst.t[:, c, 0:ncols], sb_.t[:, 0:ncols], AF.Copy, [sb_], [dst])
                else:
                    act(dst.t[:, c, 0:ncols], sb_.t[:, 0:ncols], AF.Copy, [sb_, gcol], [dst], scale=gcol.t[:, c:c + 1])

        with ExitStack() as st1:
            S.barrier()
            w_in_b = sbuf(st1, "w_in_b", [128, 8, 2560], BF16)
            w_out_b = sbuf(st1, "w_out_b", [128, 8, 1024], BF16)
            wmT = sbuf(st1, "wmT", [128, 8, 128], BF16)
            KT = sbuf(st1, "KT", [128, 4, 32 * 128], BF16)
            VA = sbuf(st1, "VA", [128, 32, 8, 65], BF16)
            maskb = sbuf(st1, "maskb", [128, 17 * 128], BF16)
            selb = sbuf(st1, "selb", [128, 256], BF16)
            gqk = sbuf(st1, "gqk", [128, 1024]); gv = sbuf(st1, "gv", [128, 512])
            bsT = sbuf(st1, "bsT", [128, 8]); w00 = sbuf(st1, "w00", [128, 8]); b0 = sbuf(st1, "b0", [128, 8])
            g1T = sbuf(st1, "g1T", [128, 8]); gabT = sbuf(st1, "gabT", [128, 8]); flag = sbuf(st1, "flag", [128, 1])
            KTs = [Buf(None) for _ in range(32)]
            VAs = [Buf(None) for _ in range(32)]

            with ExitStack() as sts:
                S.barrier()
                stage = [sbuf(sts, "stage%d" % i, [128, 2560]) for i in range(2)]
                tri = sbuf(sts, "tri", [128, 128])
                for (dst, src) in ((g1T, g1T_d), (gabT, gabT_d), (flag, flag_d), (bsT, bsT_d), (tri, tri_d)):
                    dma("sp", dst.t[:], src, [], [dst])
                dma("sp", gqk.t[:], gqk_d.partition_broadcast(128), [], [gqk])
                dma("sp", gv.t[:], gv_d.partition_broadcast(128), [], [gv])
                dma("sp", w00.t[:], w00_d.partition_broadcast(128), [], [w00])
                dma("sp", b0.t[:], b0_d.partition_broadcast(128), [], [b0])
                load_weight(sts, stage, w_in, 8, 2560, w_in_b, g1T)
                load_weight(sts, stage, w_out, 8, 1024, w_out_b, gabT)
                dma("sp", stage[0].t[:, 0:17 * 128], mask_d, [], [stage[0]])
                act(maskb.t[:], stage[0].t[:, 0:17 * 128], AF.Copy, [stage[0]], [maskb])
                dma("sp", stage[1].t[:, 0:256], sel_d, [], [stage[1]])
                act(selb.t[:], stage[1].t[:, 0:256], AF.Copy, [stage[1]], [selb])
                dma("sp", stage[0].t[:, 0:1024], wsT_d, [], [stage[0]])
                tt("dve", wmT.t[:], fv(stage[0].t[:, 0:1], [(128, 8), (1, 128)]), fv(tri.t[:, 0:1], [(0, 8), (1, 128)]),
                   ALU.mult, [stage[0], tri], [wmT])
                if "wmT" in dbg_o:
                    act(stage[1].t[:, 0:1024], wmT.t[:].rearrange("p g i -> p (g i)"), AF.Copy, [wmT], [stage[1]])
                    dbgout("wmT", stage[1].t[:, 0:1024], stage[1])
                    dbgout("tri", tri.t[:, :], tri)
                mset("dve", VA.t[:, 16:32, :, 64:65], 1.0, [VA])
                cp("dve", VA.t[:, 0:16, :, 64], fv(flag.t[:, 0:1], [(0, 16), (0, 8)]), [flag], [VA])

            with ExitStack() as stt_:
                S.barrier()
                tabk = Buf(None)
                tsems = [S.new_dsem() for _ in range(16)]
                for d_ in tsems:
                    d_.bg = True
                kk_ = 0
                for r0 in range(0, 16384, 1024):
                    for (src_, c0_) in ((peer_u, 0), (peer_v, 1024)):
                        def f_(e, src_=src_, c0_=c0_, r0=r0):
                            return e.dma_start(out=ptab[r0:r0 + 1024, c0_:c0_ + 1024], in_=src_[r0:r0 + 1024, :])
                        S.dma("pool", f_, [], K([tabk]), dsem=tsems[kk_ % 16])
                        kk_ += 1
                xb = [sbuf(stt_, "xb%d" % i, [128, 1024]) for i in range(2)]
                csb = [sbuf(stt_, "csb%d" % i, [128, 16]) for i in range(2)]
                ssq = sbuf(stt_, "ssq", [128, 1]); rs = sbuf(stt_, "rs", [128, 1]); junk = sbuf(stt_, "junk", [128, 1024], BF16)
                small = (ssq, rs, junk)
                n1 = sbuf(stt_, "n1", [128, 1024], BF16); n1T = sbuf(stt_, "n1T", [128, 8, 128], BF16)
                sq = sbuf(stt_, "sq", [128, 1024]); qkn = sbuf(stt_, "qkn", [128, 1024]); qkb = sbuf(stt_, "qkb", [128, 1024], BF16)
                ssh = sbuf(stt_, "ssh", [128, 16]); rh = sbuf(stt_, "rh", [128, 16]); rt = sbuf(stt_, "rt", [128, 4, 16, 8])
                QT = sbuf(stt_, "QT", [128, 4, 256], BF16); vt = sbuf(stt_, "vt", [128, 512])
                mset("dve", QT.t[:, :, :], 0.0, [QT])
                ug = sbuf(stt_, "ug", [128, 1024]); vn = sbuf(stt_, "vn", [128, 512]); vnb = sbuf(stt_, "vnb", [128, 512], BF16)
                gm = sbuf(stt_, "gm", [128, 512]); ab = sbuf(stt_, "ab", [128, 1024], BF16); abT = sbuf(stt_, "abT", [128, 8, 128], BF16)
                eT = [sbuf(stt_, "eT%d" % i, [128, 512], BF16) for i in range(2)]
                rden = sbuf(stt_, "rden", [128, 8]); att = sbuf(stt_, "att", [128, 512]); h1t = sbuf(stt_, "h1t", [128, 1024])
                NBG = 2
                qbc = sbuf(stt_, "qbc", [128, NBG * 512])
                kcs = [sbuf(stt_, "kc%d" % i, [128, NBG * 512]) for i in range(2)]
                vcs = [sbuf(stt_, "vc%d" % i, [128, NBG * 512]) for i in range(2)]
                prod = Buf(None); prod.t = sq.t; prod.k = sq.k
                sc = sbuf(stt_, "sc", [128, NBG * 8]); evt = sbuf(stt_, "evt", [128, NBG, 8, 65], BF16)
                osb = Buf(None); osb.t = ug.t[:, 0:520].rearrange("p (a b c) -> p a b c", a=2, b=4); osb.k = ug.k
                sself = sbuf(stt_, "sself", [128, 8]); qsrc = Buf(None)

                def load_x(slot):
                    b = slot % 2
                    P = 16 if slot == 32 else 128
                    dma("sp", xb[b].t[0:P, :], xall[slot * 128:slot * 128 + P, :], [], [xb[b]])
                    dma("sp", csb[b].t[0:P, :], cs_d[slot * 128:slot * 128 + P, :], [], [csb[b]])

                def tile1(slot, nxt):
                    mode = "halo" if slot < 16 else ("own" if slot < 32 else "sample")
                    P = 16 if mode == "sample" else 128
                    x = xb[slot % 2]; c_s = csb[slot % 2]
                    if nxt is not None:
                        load_x(nxt)
                    r1 = rms_scale(small, x.t[0:P, :], P, 1024, [x], "r1")
                    act(n1.t[0:P, :], x.t[0:P, :], AF.Copy, [x, r1], [n1], scale=r1.t[0:P, 0:1])
                    b3 = pbank_b(3)
                    for c in range(8):
                        tr(b3[:, c * 128:c * 128 + P], n1.t[0:P, c * 128:(c + 1) * 128], identb.t[0:P, 0:P], [n1, identb], [bank[3]])
                    act(n1T.t[:, :, 0:P], fv(b3[:, 0:1], [(128, 8), (1, P)]), AF.Copy, [bank[3]], [n1T])
                    colblocks = [(1, 512), (2, 1024)] if mode == "halo" else [(0, 0), (1, 512), (2, 1024), (4, 1536), (5, 2048)]
                    for bk, col in colblocks:
                        for c in range(8):
                            mm(pbank(bk, P), n1T.t[:, c, 0:P], w_in_b.t[:, c, col:col + 512], c == 0, c == 7, [n1T, w_in_b], [bank[bk]])
                    zqk = PS[0].t[0:P, :]
                    act(sq.t[0:P, :], zqk, AF.Square, [bank[0], bank[1]], [sq])
                    red("dve", ssh.t[0:P, :], sq.t[0:P, :].rearrange("p (a b) -> p a b", a=16), ALU.add, [sq], [ssh])
                    act(rh.t[0:P, :], ssh.t[0:P, :], AF.Sqrt, [ssh], [rh], bias=EPS, scale=1.0 / 64)
                    recip(rh.t[0:P, :], rh.t[0:P, :], [rh], [rh])
                    q3 = qkn.t[0:P, :].rearrange("p (a b) -> p a b", a=16)
                    tt("dve", q3, zqk.rearrange("p (a b) -> p a b", a=16), fv(rh.t[0:P, 0:1], [(1, 16), (0, 64)]), ALU.mult,
                       [bank[0], bank[1], rh], [qkn])
                    tt("dve", qkn.t[0:P, :], qkn.t[0:P, :], gqk.t[0:P, :], ALU.mult, [qkn, gqk], [qkn])
                    x1 = q3[:, :, 0:8]; x2 = q3[:, :, 8:16]
                    cosb = fv(c_s.t[0:P, 0:1], [(0, 16), (1, 8)]); sinb = fv(c_s.t[0:P, 8:9], [(0, 16), (1, 8)])
                    tt("dve", rt.t[0:P, 0], x1, cosb, ALU.mult, [qkn, c_s], [rt])
                    tt("dve", rt.t[0:P, 1], x2, sinb, ALU.mult, [qkn, c_s], [rt])
                    tt("dve", rt.t[0:P, 2], x2, cosb, ALU.mult, [qkn, c_s], [rt])
                    tt("dve", rt.t[0:P, 3], x1, sinb, ALU.mult, [qkn, c_s], [rt])
                    tt("dve", x1, rt.t[0:P, 0], rt.t[0:P, 1], ALU.subtract, [rt], [qkn])
                    tt("dve", x2, rt.t[0:P, 2], rt.t[0:P, 3], ALU.add, [rt], [qkn])
                    act(qkb.t[0:P, :], qkn.t[0:P, :], AF.Copy, [qkn], [qkb])
                    if CUT <= -2:
                        return
                    if mode == "own":
                        o0 = (slot - 16) * 128
                        dma("sp", k_own[o0:o0 + 128, :], qkn.t[:, 512:1024], [qkn], [], final=True)
                        if CUT <= -1:
                            return
                    elif mode == "sample":
                        dma("sp", ks_o, qkn.t[0:16, 512:1024], [qkn], [], final=True)
                    crange = range(4, 8) if mode == "halo" else range(8)
                    for c in crange:
                        tr(b3[:, c * 128:c * 128 + P], qkb.t[0:P, c * 128:(c + 1) * 128], identb.t[0:P, 0:P], [qkb, identb], [bank[3]])
                    if mode == "own":
                        act(QT.t[0:64, :, 0:128], fv(b3[0:64, 0:1], [(128, 4), (1, 128)]), AF.Copy, [bank[3]], [QT])
                        act(QT.t[64:128, :, 128:256], fv(b3[64:128, 0:1], [(128, 4), (1, 128)]), AF.Copy, [bank[3]], [QT])
                    if mode != "sample":
                        cp("dve", KT.t[:, :, slot * 128:(slot + 1) * 128], fv(b3[:, 512:513], [(128, 4), (1, 128)]), [bank[3]], [KTs[slot]])
                    if CUT <= 0 and mode == "own":
                        return
                    if mode != "halo":
                        act(vt.t[0:P, :], pbank(2, P), AF.Copy, [bank[2]], [vt])
                        if mode == "own":
                            dma("sp", v_own[o0:o0 + 128, :], vt.t[:, :], [vt], [], final=True)
                        else:
                            dma("sp", vs_o, vt.t[0:16, :], [vt], [], final=True)
                    if mode != "sample":
                        cp("dve", VA.t[:, slot, :, 0:64], pbank(2, P).rearrange("p (h c) -> p h c", h=8), [bank[2]], [VAs[slot]])
                    if mode == "halo" or CUT <= 1:
                        return
                    vn3 = vn.t[0:P, :].rearrange("p (a b) -> p a b", a=8)
                    gm3 = gm.t[0:P, :].rearrange("p (a b) -> p a b", a=8)

                    def gmlp_front():
                        act(ug.t[0:P, :], PS[2].t[0:P, :], AF.Gelu_apprx_tanh, [bank[4], bank[5]], [ug])
                        act(sq.t[0:P, 0:512], ug.t[0:P, 512:1024], AF.Square, [ug], [sq])
                        red("dve", ssh.t[0:P, 0:8], sq.t[0:P, 0:512].rearrange("p (a b) -> p a b", a=8), ALU.add, [sq], [ssh])
                        act(rh.t[0:P, 0:8], ssh.t[0:P, 0:8], AF.Sqrt, [ssh], [rh], bias=EPS, scale=1.0 / 64)
                        recip(rh.t[0:P, 0:8], rh.t[0:P, 0:8], [rh], [rh])
                        tt("dve", vn3, ug.t[0:P, 512:1024].rearrange("p (a b) -> p a b", a=8), fv(rh.t[0:P, 0:1], [(1, 8), (0, 64)]),
                           ALU.mult, [ug, rh], [vn])
                        tt("dve", vn.t[0:P, :], vn.t[0:P, :], gv.t[0:P, :], ALU.mult, [vn, gv], [vn])
                        if mode == "own":
                            act(vnb.t[0:P, :], vn.t[0:P, :], AF.Copy, [vn], [vnb])

                    def gmlp_back():
                        if mode == "own":
                            for g in range(8):
                                mm(pbank(2, P, g * 64, (g + 1) * 64), wmT.t[:, g, 0:P], vnb.t[:, g * 64:(g + 1) * 64], True, True, [wmT, vnb], [bank[2]])
                            tt("dve", gm3, pbank(2, P).rearrange("p (a b) -> p a b", a=8), fv(bsT.t[0:P, 0:1], [(1, 8), (0, 64)]), ALU.add,
                               [bank[2], bsT], [gm])
                        else:
                            dma("sp", cvs_o, vn.t[0:16, :], [vn], [], final=True)
                            tt("dve", gm3, vn3, fv(w00.t[0:P, 0:1], [(1, 8), (0, 64)]), ALU.mult, [vn, w00], [gm])
                            tt("dve", gm3, gm3, fv(b0.t[0:P, 0:1], [(1, 8), (0, 64)]), ALU.add, [gm, b0], [gm])
                        if slot == 16:
                            dbgout("ug0", ug.t[:, :], ug)
                            dbgout("vn0", vn.t[:, :], vn)
                            dbgout("mix0", gm.t[:, :], gm)
                        tt("dve", gm.t[0:P, :], gm.t[0:P, :], ug.t[0:P, 0:512], ALU.mult, [gm, ug], [gm])

                    def gmlp_back2():
                        rb = rms_scale(small, gm.t[0:P, :], P, 512, [gm], "rb")
                        act(ab.t[0:P, 512:1024], gm.t[0:P, :], AF.Copy, [gm, rb], [ab], scale=rb.t[0:P, 0:1])

                    gmlp_front()
                    if mode != "own":
                        gmlp_back()
                        gmlp_back2()
                    if CUT <= 2:
                        return
                    if mode == "own":
                        i0 = slot - 16
                        groups = [(hp, grp) for hp in range(4) for grp in range(9)]

                        def a_scores(gi):
                            hp, grp = groups[gi]
                            j0 = grp * 2
                            nj = min(2, 17 - j0)
                            sb_i = gi % 2
                            for jj in range(nj):
                                s_ = i0 + j0 + jj
                                mm(pbank(sb_i, 128, jj * 256, (jj + 1) * 256), KT.t[:, hp, s_ * 128:(s_ + 1) * 128],
                                   QT.t[:, hp, :], True, True, [KTs[s_], QT], [bank[sb_i]])

                        def a_soft(gi):
                            hp, grp = groups[gi]
                            j0 = grp * 2
                            nj = min(2, 17 - j0)
                            sb_i = gi % 2
                            et = eT[sb_i]
                            act(et.t[:, 0:nj * 256], pbank(sb_i, 128, 0, nj * 256), AF.Exp, [bank[sb_i]], [et], scale=0.125)
                            tt("pool", et.t[:, 0:nj * 256].rearrange("p (j e q) -> p j e q", j=nj, e=2),
                               et.t[:, 0:nj * 256].rearrange("p (j e q) -> p j e q", j=nj, e=2),
                               fv(maskb.t[:, j0 * 128:j0 * 128 + 1], [(128, nj), (0, 2), (1, 128)]), ALU.mult, [et, maskb], [et])

                        def a_pv(gi):
                            hp, grp = groups[gi]
                            j0 = grp * 2
                            nj = min(2, 17 - j0)
                            et = eT[gi % 2]
                            for jj in range(nj):
                                s_ = i0 + j0 + jj
                                j = j0 + jj
                                for e_ in range(2):
                                    h = 2 * hp + e_
                                    ob = 6 + e_
                                    o_ap = pbank(ob, 128, hp * 65, hp * 65 + 65)
                                    c0 = jj * 256 + e_ * 128
                                    mm(o_ap, et.t[:, c0:c0 + 128], VA.t[:, s_, h, :], j == 0, j == 16, [et, VAs[s_], VA], [bank[ob]])

                        a_scores(0)
                        for gi in range(len(groups)):
                            if gi + 1 < len(groups):
                                a_scores(gi + 1)
                            a_soft(gi)
                            a_pv(gi)
                            if gi == 2:
                                gmlp_back()
                            if gi == 8:
                                gmlp_back2()
                        o_src = fv(PS[3].t[0:128, 0:1], [(512, 2), (65, 4), (1, 65)])
                        o_r = [bank[6], bank[7]]
                        att_v = fv(att.t[0:P, 0:1], [(64, 2), (128, 4), (1, 64)])
                        rden_v = fv(rden.t[0:P, 0:1], [(1, 2), (2, 4)])
                        rden_b = fv(rden.t[0:P, 0:1], [(1, 2), (2, 4), (0, 64)])
                    else:
                        dma("sp", qs_d.rearrange("o (b f) -> (o b) f", b=16), qkn.t[0:16, 0:512], [qkn], [qsrc])
                        first = True
                        for bg in range(16 // NBG):
                            dma("sp", qbc.t[:, :], qs_d[0:1, bg * NBG * 512:(bg + 1) * NBG * 512].partition_broadcast(128), [qsrc], [qbc])
                            for pi, dil in enumerate((1, 4, 16)):
                                st_ = 2048 - 128 * dil
                                kc = kcs[(bg * 3 + pi) % 2]; vc = vcs[(bg * 3 + pi) % 2]
                                dma("sp", kc.t[:, :].rearrange("p (b f) -> p b f", b=NBG),
                                    ck[bg * NBG:(bg + 1) * NBG, st_:2048:dil, :].rearrange("b k f -> k b f"), [], [kc])
                                dma("sp", vc.t[:, :].rearrange("p (b f) -> p b f", b=NBG),
                                    cv[bg * NBG:(bg + 1) * NBG, st_:2048:dil, :].rearrange("b k f -> k b f"), [], [vc])
                                tt("pool", prod.t[:, :], kc.t[:, :], qbc.t[:, :], ALU.mult, [kc, qbc], [prod])
                                red("dve", sc.t[:, :], prod.t[:, :].rearrange("p (a b) -> p a b", a=NBG * 8), ALU.add, [prod], [sc])
                                act(sc.t[:, :], sc.t[:, :], AF.Exp, [sc], [sc], scale=0.125)
                                tt("dve", evt.t[:, :, :, 0:64], vc.t[:, :].rearrange("p (b h c) -> p b h c", b=NBG, h=8),
                                   fv(sc.t[:, 0:1], [(8, NBG), (1, 8), (0, 64)]), ALU.mult, [vc, sc], [evt])
                                cp("dve", evt.t[:, :, :, 64], sc.t[:, :].rearrange("p (b h) -> p b h", b=NBG), [sc], [evt])
                                for bb in range(NBG):
                                    bglob = bg * NBG + bb
                                    last = (bg == 16 // NBG - 1 and pi == 2 and bb == NBG - 1)
                                    for half in range(2):
                                        mm(pbank(6 + half, 16, 0, 260), selb.t[:, bglob * 16:(bglob + 1) * 16],
                                           evt.t[:, bb, half * 4:(half + 1) * 4, :].rearrange("p h c -> p (h c)"), first, last, [selb, evt], [bank[6 + half]])
                                    first = False
                        cp("dve", osb.t[0:16], fv(PS[3].t[0:16, 0:1], [(512, 2), (65, 4), (1, 65)]), [bank[6], bank[7]], [osb])
                        tt("dve", prod.t[0:16, 0:512], qkn.t[0:16, 0:512], qkn.t[0:16, 512:1024], ALU.mult, [qkn], [prod])
                        red("dve", sself.t[0:16, :], prod.t[0:16, 0:512].rearrange("p (a b) -> p a b", a=8), ALU.add, [prod], [sself])
                        act(sself.t[0:16, :], sself.t[0:16, :], AF.Exp, [sself], [sself], scale=0.125)
                        tss("dve", sself.t[0:16, :], sself.t[0:16, :], 3.0, ALU.mult, [sself], [sself])
                        o4 = osb.t[0:16].rearrange("p a b c -> p (a b) c")
                        tt("dve", prod.t[0:16, 0:512].rearrange("p (h c) -> p h c", h=8), vt.t[0:16, :].rearrange("p (h c) -> p h c", h=8),
                           fv(sself.t[0:16, 0:1], [(1, 8), (0, 64)]), ALU.mult, [vt, sself], [prod])
                        tt("dve", o4[:, :, 0:64], o4[:, :, 0:64], prod.t[0:16, 0:512].rearrange("p (h c) -> p h c", h=8), ALU.add, [osb, prod], [osb])
                        tt("dve", o4[:, :, 64], o4[:, :, 64], sself.t[0:16, :], ALU.add, [osb, sself], [osb])
                        o_src = osb.t[0:16]
                        o_r = [osb]
                        att_v = att.t[0:P, :].rearrange("p (a b c) -> p a b c", a=2, b=4)
                        rden_v = rden.t[0:P, :].rearrange("p (a b) -> p a b", a=2)
                        rden_b = fv(rden.t[0:P, 0:1], [(4, 2), (1, 4), (0, 64)])
                    if CUT <= 3:
                        return
                    recip(rden_v, o_src[:, :, :, 64], o_r, [rden])
                    tt("dve", att_v, o_src[:, :, :, 0:64], rden_b, ALU.mult, o_r + [rden], [att])
                    ra = rms_scale(small, att.t[0:P, :], P, 512, [att], "ra")
                    act(ab.t[0:P, 0:512], att.t[0:P, :], AF.Copy, [att, ra], [ab], scale=ra.t[0:P, 0:1])
                    for c in range(8):
                        tr(b3[:, c * 128:c * 128 + P], ab.t[0:P, c * 128:(c + 1) * 128], identb.t[0:P, 0:P], [ab, identb], [bank[3]])
                    act(abT.t[:, :, 0:P], fv(b3[:, 0:1], [(128, 8), (1, P)]), AF.Copy, [bank[3]], [abT])
                    for nb in range(2):
                        for c in range(8):
                            mm(pbank(4 + nb, P), abT.t[:, c, 0:P], w_out_b.t[:, c, nb * 512:(nb + 1) * 512], c == 0, c == 7, [abT, w_out_b], [bank[4 + nb]])
                    tt("dve", h1t.t[0:P, :], PS[2].t[0:P, :], x.t[0:P, :], ALU.add, [bank[4], bank[5], x], [h1t])
                    ti = slot - 16
                    dma("sp", h1_d[ti * 128:ti * 128 + P, :], h1t.t[0:P, :], [h1t], [h1s[ti]], dsem=h1sems[ti % 2])
                    if ti == 0:
                        dbgout("att0", att.t[:, :], att)
                        dbgout("gm0", gm.t[:, :], gm)
                        dbgout("h10", h1t.t[:, :], h1t)
                    if mode == "sample":
                        dbgout("atts", att.t[:, :], att)
                        dbgout("h1s_", h1t.t[:, :], h1t)

                h1s = [Buf(None) for _ in range(17)]
                h1sems = [S.new_dsem() for _ in range(2)]
                if slots1:
                    load_x(slots1[0])
                for si, slot in enumerate(slots1):
                    tile1(slot, slots1[si + 1] if si + 1 < len(slots1) else None)

        eidx_all = sbuf(st0, "eidx_all", [128, 17, NSLOT], I32)
        g_all = sbuf(st0, "g_all", [128, 17, NSLOT])
        w_gate_b = sbuf(st0, "w_gate_b", [128, 8, 1024], BF16)
        w_proj_b = sbuf(st0, "w_proj_b", [128, 2, 1024], BF16)
        g2bc = sbuf(st0, "g2bc", [128, 1024]); g3T = sbuf(st0, "g3T", [128, 8])
        with ExitStack() as st2:
            S.barrier()
            Wqk = sbuf(st2, "Wqk", [128, 8, 2048], BF16)
            Wqk_e = Buf(None); Wqk_e.t = Wqk.t
            Wqk_o = Buf(None); Wqk_o.t = Wqk.t
            g2T = sbuf(st2, "g2T", [128, 8])
            dma("sp", g2T.t[:], g2T_d, [], [g2T])
            with ExitStack() as sts:
                S.barrier()
                skT = sbuf(sts, "skT", [128, 2048])
                wq = [sbuf(sts, "wq%d" % i, [128, 1024]) for i in range(2)]
                dma("sp", skT.t[:], skT_d, [], [skT])
                for b in range(16):
                    w_ = wq[b % 2]
                    dma("sp", w_.t[:], wqT_d[b * 128:(b + 1) * 128, :], [], [w_])
                    for dc in range(8):
                        pb_i = 2 * (b % 2) + dc % 2
                        mm(pbank(pb_i, 128, (dc // 2) * 128, (dc // 2) * 128 + 128), w_.t[:, dc * 128:(dc + 1) * 128], skT.t[:, b * 128:(b + 1) * 128],
                           True, True, [w_, skT], [bank[pb_i]])
                    for dc in range(8):
                        pb_i = 2 * (b % 2) + dc % 2
                        if dc % 2 == 0:
                            act(Wqk.t[:, dc, b * 128:(b + 1) * 128], pbank(pb_i, 128, (dc // 2) * 128, (dc // 2) * 128 + 128), AF.Copy,
                                [bank[pb_i], g2T], [Wqk_e], scale=g2T.t[:, dc:dc + 1])
                        else:
                            tss("dve", Wqk.t[:, dc, b * 128:(b + 1) * 128], pbank(pb_i, 128, (dc // 2) * 128, (dc // 2) * 128 + 128),
                                g2T.t[:, dc:dc + 1], ALU.mult, [bank[pb_i], g2T], [Wqk_o])
            with ExitStack() as stt_:
                S.barrier()
                stg3 = [sbuf(stt_, "stg3_%d" % i, [128, 1024]) for i in range(2)]
                hb_ = [sbuf(stt_, "h1b%d" % i, [128, 1024]) for i in range(2)]
                ssq = sbuf(stt_, "ssq2", [128, 1]); rs = sbuf(stt_, "rs2", [128, 1]); junk = sbuf(stt_, "junk2", [128, 1024], BF16)
                hbf = sbuf(stt_, "hbf", [128, 1024], BF16); hT = sbuf(stt_, "hT", [128, 8, 128], BF16)
                ssbs = [sbuf(stt_, "ssb%d" % i, [128, 2048]) for i in range(2)]; s2 = sbuf(stt_, "s2b", [128, 2048])
                tops = sbuf(stt_, "tops", [128, 16, 16]); topi = sbuf(stt_, "topi", [128, 16, 16], U32); topf = sbuf(stt_, "topf", [128, 16, 16])
                cand = sbuf(stt_, "cand", [128, 8, 256]); cand2 = sbuf(stt_, "cand2", [128, 8, 256])
                best = sbuf(stt_, "best", [128, 8, 16]); pos = sbuf(stt_, "pos", [128, 8, 16], U32)
                pa = sbuf(stt_, "pa", [128, 8, 16], I32); pb_ = sbuf(stt_, "pb", [128, 8, 16], I32)
                paf = sbuf(stt_, "paf", [128, 8, 16]); pbf = sbuf(stt_, "pbf", [128, 8, 16])
                oh = sbuf(stt_, "oh", [128, 8, 16, 16]); e0 = sbuf(stt_, "e0", [128, 8, 16]); e1 = sbuf(stt_, "e1", [128, 8, 16])
                mx = sbuf(stt_, "mx", [128, 8]); sm = sbuf(stt_, "sm", [128, 8]); gex = sbuf(stt_, "gex", [128, 8, 16]); gsum = sbuf(stt_, "gsum", [128, 8, 16])

                def load_h(ti, bufs):
                    P = 16 if ti == 16 else 128
                    dma("sp", bufs[ti % 2].t[0:P, :], h1_d[ti * 128:ti * 128 + P, :], [h1s[ti]], [bufs[ti % 2]])

                def front2(ti, nxt):
                    P = 16 if ti == 16 else 128
                    h = hb_[ti % 2]
                    ssb = ssbs[ti % 2]
                    if nxt is not None:
                        load_h(nxt, hb_)
                    r2 = rms_scale((ssq, rs, junk), h.t[0:P, :], P, 1024, [h], "r2")
                    cp("dve", rstd2_all.t[0:P, ti:ti + 1], r2.t[0:P, :], [r2], [rstd2_all])
                    act(hbf.t[0:P, :], h.t[0:P, :], AF.Copy, [h], [hbf])
                    b3 = pbank_b(7)
                    for c in range(8):
                        tr(b3[:, c * 128:c * 128 + P], hbf.t[0:P, c * 128:(c + 1) * 128], identb.t[0:P, 0:P], [hbf, identb], [bank[7]])
                    act(hT.t[:, :, 0:P], fv(b3[:, 0:1], [(128, 8), (1, P)]), AF.Copy, [bank[7]], [hT])
                    for nb in range(4):
                        for c in range(8):
                            mm(pbank(nb, P), hT.t[:, c, 0:P], Wqk.t[:, c, nb * 512:(nb + 1) * 512], c == 0, c == 7, [hT, Wqk_e, Wqk_o], [bank[nb]])
                    for half in range(2):
                        act(ssb.t[0:P, half * 1024:(half + 1) * 1024], PS[half].t[0:P, :], AF.Copy, [bank[2 * half], bank[2 * half + 1], r2], [ssb],
                            scale=r2.t[0:P, 0:1])

                def back2(ti):
                    P = 16 if ti == 16 else 128
                    ssb = ssbs[ti % 2]

                    def top16(src, scr, g, n, vals, idxs):
                        sv = src.t[0:P, g * n:(g + 1) * n] if len(src.t.shape) == 2 else src.t[0:P, g, :]
                        s2v = scr.t[0:P, g * n:(g + 1) * n] if len(scr.t.shape) == 2 else scr.t[0:P, g, :]
                        v0 = vals.t[0:P, g, 0:8]; v1 = vals.t[0:P, g, 8:16]
                        S.op("dve", lambda e: e.max(out=v0, in_=sv), K([src]), K([vals]))
                        S.op("dve", lambda e: e.max_index(out=idxs.t[0:P, g, 0:8], in_max=v0, in_values=sv), K([src, vals]), K([idxs]))
                        S.op("dve", lambda e: e.match_replace(out=s2v, in_to_replace=v0, in_values=sv, imm_value=-1e30), K([src, vals]), K([scr]))
                        S.op("dve", lambda e: e.max(out=v1, in_=s2v), K([scr]), K([vals]))
                        S.op("dve", lambda e: e.max_index(out=idxs.t[0:P, g, 8:16], in_max=v1, in_values=s2v), K([scr, vals]), K([idxs]))

                    for g in range(16):
                        top16(ssb, s2, g, 128, tops, topi)
                    cp("dve", topf.t[0:P], topi.t[0:P], [topi], [topf])
                    c4 = cand.t[0:P].rearrange("p h (a b) -> p h a b", a=16)
                    tt("dve", c4, fv(tops.t[0:P, 0, 0:1], [(32, 8), (1, 16), (0, 16)]), fv(tops.t[0:P, 1, 0:1], [(32, 8), (0, 16), (1, 16)]),
                       ALU.add, [tops], [cand])
                    for hh in range(8):
                        top16(cand, cand2, hh, 256, best, pos)
                    tss("dve", pa.t[0:P], pos.t[0:P].bitcast(I32), 4, ALU.arith_shift_right, [pos], [pa])
                    tss("dve", pb_.t[0:P], pos.t[0:P].bitcast(I32), 15, ALU.bitwise_and, [pos], [pb_])
                    cp("dve", paf.t[0:P], pa.t[0:P], [pa], [paf])
                    cp("dve", pbf.t[0:P], pb_.t[0:P], [pb_], [pbf])
                    iob = fv(iota16.t[0:P, 0:1], [(0, 8), (0, 16), (1, 16)])
                    for (pf, pidx, eo) in ((paf, 0, e0), (pbf, 1, e1)):
                        tt("dve", oh.t[0:P], iob, fv(pf.t[0:P, 0, 0:1], [(16, 8), (1, 16), (0, 16)]), ALU.is_equal, [iota16, pf], [oh])
                        tt("dve", oh.t[0:P], oh.t[0:P], fv(topf.t[0:P, pidx, 0:1], [(32, 8), (0, 16), (1, 16)]), ALU.mult, [oh, topf], [oh])
                        red("dve", eo.t[0:P], oh.t[0:P], ALU.add, [oh], [eo])
                    stt("dve", e0.t[0:P], e0.t[0:P], 128.0, e1.t[0:P], ALU.mult, ALU.add, [e0, e1], [e0])
                    cp("dve", eidx_all.t[0:P, ti, :], e0.t[0:P].rearrange("p h k -> p (h k)"), [e0], [eidx_all])
                    red("dve", mx.t[0:P], best.t[0:P], ALU.max, [best], [mx])
                    tt("dve", best.t[0:P], best.t[0:P], fv(mx.t[0:P, 0:1], [(1, 8), (0, 16)]), ALU.subtract, [best, mx], [best])
                    act(best.t[0:P], best.t[0:P], AF.Exp, [best], [best])
                    red("dve", sm.t[0:P], best.t[0:P], ALU.add, [best], [sm])
                    recip(sm.t[0:P], sm.t[0:P], [sm], [sm])
                    tt("dve", g_all.t[0:P, ti, :].rearrange("p (h k) -> p h k", h=8), best.t[0:P], fv(sm.t[0:P, 0:1], [(1, 8), (0, 16)]),
                       ALU.mult, [best, sm], [g_all])
                    if ti == 0:
                        dbgout("s0", ssb.t[:, :], ssb)
                        dbgout("eidx0", e0.t[:].rearrange("p h k -> p (h k)"), e0)
                        dbgout("g0", g_all.t[:, 0, :], g_all)

                if p2:
                    load_h(p2[0], hb_)
                    front2(p2[0], p2[1] if len(p2) > 1 else None)
                dma("sp", g3T.t[:], g3T_d, [], [g3T])
                dma("sp", g2bc.t[:], g2_d.partition_broadcast(128), [], [g2bc])
                load_weight(stt_, stg3, w_gate, 8, 1024, w_gate_b, g3T)
                load_weight(stt_, stg3, w_proj, 2, 1024, w_proj_b, None)
                for i_, ti in enumerate(p2):
                    if i_ + 1 < len(p2):
                        front2(p2[i_ + 1], p2[i_ + 2] if i_ + 2 < len(p2) else None)
                    back2(ti)

        with ExitStack() as st3:
            S.barrier(include_bg=True)
            with ExitStack() as stt_:
                S.barrier()
                NB = 24
                hb_ = [sbuf(stt_, "h3b%d" % i, [128, 1024]) for i in range(2)]
                pbuf = [sbuf(stt_, "pbuf%d" % i, [128, 256]) for i in range(2)]
                gb = [sbuf(stt_, "gb%d" % i, [128, 2048], BF16) for i in range(NB)]
                dgs = [sbuf(stt_, "dg%d" % i, [128, 128], BF16) for i in range(4)]
                n2b = sbuf(stt_, "n2b", [128, 1024], BF16); junkb = sbuf(stt_, "junkb", [128, 1024], BF16)
                accs = [sbuf(stt_, "acc%d" % i, [128, 1024]) for i in range(2)]
                actv = sbuf(stt_, "actv", [128, NSLOT]); coef = sbuf(stt_, "coef", [128, NSLOT]); coefg = sbuf(stt_, "coefg", [128, NSLOT])
                ssq = sbuf(stt_, "ssq3", [128, 1]); rs = sbuf(stt_, "rs3", [128, 1]); junk = sbuf(stt_, "junk3", [128, 1024], BF16)
                hbf = sbuf(stt_, "hbf3", [128, 1024], BF16); hT = sbuf(stt_, "hT3", [128, 8, 128], BF16)
                pbf16 = sbuf(stt_, "pbf16", [128, 256], BF16); pT = sbuf(stt_, "pT", [128, 2, 128], BF16)
                sg = sbuf(stt_, "sg", [128, 1024]); yt = sbuf(stt_, "yt", [128, 1024])

                def load3(ti):
                    P = 16 if ti == 16 else 128
                    dma("sp", hb_[ti % 2].t[0:P, :], h1_d[ti * 128:ti * 128 + P, :], [h1s[ti]], [hb_[ti % 2]])
                    dma("sp", pbuf[ti % 2].t[0:P, :], pall[ti * 128:ti * 128 + P, :], [], [pbuf[ti % 2]])

                def gather(dstb, ti, slot, P):
                    off = eidx_all.t[0:P, ti, slot:slot + 1]
                    S.dma("pool", lambda e: e.indirect_dma_start(out=dstb.t[0:P, :], out_offset=None, in_=ptab,
                                                                in_offset=bass.IndirectOffsetOnAxis(ap=off, axis=0)),
                          K([eidx_all]), K([dstb]))

                BS = 8
                NBAT = NSLOT // BS

                def colbufs(buf):
                    out = []
                    for _ in range(NBAT):
                        b_ = Buf(None); b_.t = buf.t
                        out.append(b_)
                    return out
                actv_b = colbufs(actv); coef_b = colbufs(coef); coefg_b = colbufs(coefg)

                gcnt = [0]

                def acc_add(ti):
                    P = 16 if ti == 16 else 128
                    h = hb_[ti % 2]; acc = accs[ti % 2]
                    tt("dve", acc.t[0:P, :], PS[0].t[0:P, :], h.t[0:P, :], ALU.add, [bank[0], bank[1], h], [acc])
                    if ti == 0:
                        dbgout("h20", acc.t[:, :], acc)

                def ple_s0(ti):
                    P = 16 if ti == 16 else 128
                    pp = pbuf[ti % 2]; acc = accs[ti % 2]
                    mset("dve", ssq.t[0:P, :], 0.0, [ssq])
                    act(junk.t[0:P, :], acc.t[0:P, :], AF.Square, [acc, ssq], [junk, ssq], accum_out=ssq.t[0:P, :])
                    act(rs.t[0:P, :], ssq.t[0:P, :], AF.Sqrt, [ssq], [rs], bias=EPS, scale=1.0 / 1024)
                    act(hbf.t[0:P, :], acc.t[0:P, :], AF.Copy, [acc], [hbf])
                    act(pbf16.t[0:P, :], pp.t[0:P, :], AF.Copy, [pp], [pbf16])
                    b3 = pbank_b(7)
                    for c in range(8):
                        tr(b3[:, c * 128:c * 128 + P], hbf.t[0:P, c * 128:(c + 1) * 128], identb.t[0:P, 0:P], [hbf, identb], [bank[7]])
                    act(hT.t[:, :, 0:P], fv(b3[:, 0:1], [(128, 8), (1, P)]), AF.Copy, [bank[7]], [hT])
                    for c in range(2):
                        tr(b3[:, c * 128:c * 128 + P], pbf16.t[0:P, c * 128:(c + 1) * 128], identb.t[0:P, 0:P], [pbf16, identb], [bank[7]])
                    act(pT.t[:, :, 0:P], fv(b3[:, 0:1], [(128, 2), (1, P)]), AF.Copy, [bank[7]], [pT])
                    for nb in range(2):
                        for c in range(8):
                            mm(pbank(2 + nb, P), hT.t[:, c, 0:P], w_gate_b.t[:, c, nb * 512:(nb + 1) * 512], c == 0, c == 7, [hT, w_gate_b], [bank[2 + nb]])
                    for nb in range(2):
                        for c in range(2):
                            mm(pbank(4 + nb, P), pT.t[:, c, 0:P], w_proj_b.t[:, c, nb * 512:(nb + 1) * 512], c == 0, c == 1, [pT, w_proj_b], [bank[4 + nb]])

                def ple_s1(ti):
                    P = 16 if ti == 16 else 128
                    recip(rs.t[0:P, :], rs.t[0:P, :], [rs], [rs])
                    act(sg.t[0:P, :], PS[1].t[0:P, :], AF.Sigmoid, [bank[2], bank[3], rs], [sg], scale=rs.t[0:P, 0:1])

                def ple_s2(ti):
                    P = 16 if ti == 16 else 128
                    acc = accs[ti % 2]
                    tt("dve", sg.t[0:P, :], sg.t[0:P, :], PS[2].t[0:P, :], ALU.mult, [sg, bank[4], bank[5]], [sg])
                    tt("dve", yt.t[0:P, :], sg.t[0:P, :], acc.t[0:P, :], ALU.add, [sg, acc], [yt])
                    if ti < 16:
                        dma("sp", y_own[ti * 128:(ti + 1) * 128, :], yt.t[:, :], [yt], [], final=True)
                    else:
                        dma("sp", y_smp, yt.t[0:16, :], [yt], [], final=True)

                def peer(ti, prev, nxt):
                    P = 16 if ti == 16 else 128
                    h = hb_[ti % 2]
                    base_ = gcnt[0]
                    gcnt[0] += NSLOT
                    stt("dve", n2b.t[0:P, :], h.t[0:P, :], rstd2_all.t[0:P, ti:ti + 1], g2bc.t[0:P, :], ALU.mult, ALU.mult, [h, rstd2_all, g2bc], [n2b])
                    mset("dve", actv.t[0:P, :], 0.0, actv_b)

                    def vphase(bq):
                        slq = slice(bq * BS, (bq + 1) * BS)
                        tt("dve", coefg.t[0:P, slq], coef.t[0:P, slq], g_all.t[0:P, ti, slq], ALU.mult, [coef_b[bq], g_all], [coefg_b[bq]])
                        for slot in range(bq * BS, (bq + 1) * BS):
                            g_ = gb[(base_ + slot) % NB]
                            d_ = dgs[slot % 4]
                            act(d_.t[0:P, 0:P], identb.t[0:P, 0:P], AF.Copy, [identb, coefg_b[bq]], [d_], scale=coefg.t[0:P, slot:slot + 1])
                            for nb in range(2):
                                mm(pbank(nb, P), d_.t[0:P, 0:P], g_.t[0:P, 1024 + nb * 512:1024 + (nb + 1) * 512], slot == 0, slot == NSLOT - 1,
                                   [d_, g_], [bank[nb]])

                    for b8 in range(NBAT):
                        sl = slice(b8 * BS, (b8 + 1) * BS)
                        for slot in range(b8 * BS, (b8 + 1) * BS):
                            g_ = gb[(base_ + slot) % NB]
                            gather(g_, ti, slot, P)
                            stt("dve", junkb.t[0:P, :], g_.t[0:P, 0:1024], 1.0, n2b.t[0:P, :], ALU.mult, ALU.mult, [g_, n2b], [junkb, actv_b[b8]],
                                accum_out=actv.t[0:P, slot:slot + 1])
                        act(coef.t[0:P, sl], actv.t[0:P, sl], AF.Gelu_apprx_tanh, [actv_b[b8]], [coef_b[b8]])
                        if b8 == 1 and prev is not None:
                            acc_add(prev)
                        if b8 >= 1:
                            vphase(b8 - 1)
                        if b8 == 1:
                            if prev is not None:
                                ple_s0(prev)
                            if nxt is not None:
                                load3(nxt)
                        if b8 == 3 and prev is not None:
                            ple_s1(prev)
                        if b8 == 5 and prev is not None:
                            ple_s2(prev)
                    vphase(NBAT - 1)

                if p3:
                    load3(p3[0])
                prev_ = None
                for i_, ti in enumerate(p3):
                    nxt_ = p3[i_ + 1] if i_ + 1 < len(p3) else None
                    peer(ti, prev_, nxt_)
                    prev_ = ti
                if p3:
                    acc_add(prev_)
                    ple_s0(prev_); ple_s1(prev_); ple_s2(prev_)

        S.emit()
    return nc


def _consts():
    k = np.arange(128)[:, None, None]
    j = np.arange(17)[None, :, None]
    q = np.arange(128)[None, None, :]
    d = (16 - j) * 128 + q - k
    m = ((d >= 0) & (d <= 128)).astype(np.float32)
    m += ((d >= 0) & (d <= 512) & (d % 4 == 0)).astype(np.float32)
    m += ((d >= 0) & (d <= 2048) & (d % 16 == 0)).astype(np.float32)
    maskT = np.ascontiguousarray(m.reshape(128, 17 * 128))
    sel = np.zeros((128, 16, 16), np.float32)
    for b in range(16):
        sel[:, b, b] = 1.0
    iota16 = np.broadcast_to(np.arange(16, dtype=np.float32), (128, 16)).copy()
    jj = np.arange(128)[:, None]
    ii = np.arange(128)[None, :]
    triT = (jj <= ii).astype(np.float32)
    return maskT, sel.reshape(128, 256), iota16, triT


def _rope_tab(pos):
    inv = 500000.0 ** (-np.arange(0, 16, 2, dtype=np.float64) / 16)
    ang = pos.astype(np.float64)[:, None] * inv[None, :]
    return np.concatenate([np.cos(ang), np.sin(ang)], axis=1).astype(np.float32)


_NC_CACHE = {}


def make_in_maps(inp):
    f = lambda a: np.ascontiguousarray(np.asarray(a, dtype=np.float32))
    xp = f(inp["x_prompt"])[0]
    xs = f(inp["x_sample"])[:, 0]
    pp = f(inp["p_prompt"])[0, 0]
    psm = f(inp["p_sample"])[0, :, 0]
    ck = f(inp["cache_k"])[0].reshape(128, 2048, 512)
    cv = f(inp["cache_v"])[0].reshape(128, 2048, 512)
    maskT, sel, iota16, triT = _consts()
    colT = lambda g: np.ascontiguousarray(f(g).reshape(-1, 128).T)
    shared = {
        "ident": np.eye(128, dtype=np.float32), "maskT": maskT, "sel": sel, "iota16": iota16, "triT": triT,
        "g1T": colT(inp["norm1_g"][0]),
        "gabT": colT(np.concatenate([f(inp["out_norm_a_g"])[0], f(inp["out_norm_b_g"])[0]])),
        "g2T": colT(inp["norm2_g"][0]), "g3T": colT(inp["norm3_g"][0]),
        "g2row": f(inp["norm2_g"]).reshape(1, 1024),
        "gqk": np.concatenate([np.tile(f(inp["q_norm_g"])[0], 8), np.tile(f(inp["k_norm_g"])[0], 8)]).reshape(1, 1024),
        "gv": f(inp["v_norm_g"]).reshape(1, 512),
        "bsT": np.ascontiguousarray(f(inp["spatial_b"])[0].T),
        "w00": np.ascontiguousarray(f(inp["spatial_w"])[0, :, 0, 0]).reshape(1, 8),
        "b0": np.ascontiguousarray(f(inp["spatial_b"])[0, :, 0]).reshape(1, 8),
        "w_in": f(inp["w_in"])[0],
        "wsT": np.ascontiguousarray(f(inp["spatial_w"])[0].transpose(2, 0, 1)).reshape(128, 1024),
        "w_out": f(inp["w_out"])[0],
        "wqT": np.ascontiguousarray(f(inp["peer_w_query"])[0].T),
        "skT": np.ascontiguousarray(f(inp["peer_sub_keys"])[0].reshape(16, 128, 128).transpose(2, 0, 1)).reshape(128, 2048),
        "peer_u": f(inp["peer_u"])[0], "peer_v": f(inp["peer_v"])[0],
        "w_gate": f(inp["ple_w_gate"])[0], "w_proj": f(inp["ple_w_proj"])[0],
    }
    in_maps = []
    for c in range(8):
        xall = np.zeros((33 * 128, 1024), np.float32)
        if c > 0:
            xall[0:2048] = xp[2048 * (c - 1):2048 * c]
        xall[2048:4096] = xp[2048 * c:2048 * (c + 1)]
        xall[4096:4096 + 16] = xs[16 * c:16 * (c + 1)]
        pall = np.zeros((17 * 128, 256), np.float32)
        pall[0:2048] = pp[2048 * c:2048 * (c + 1)]
        pall[2048:2048 + 16] = psm[16 * c:16 * (c + 1)]
        pos = np.zeros(33 * 128, np.int64)
        pos[0:2048] = np.maximum(2048 * (c - 1) + np.arange(2048), 0)
        pos[2048:4096] = 2048 * c + np.arange(2048)
        pos[4096:] = 8192
        m = dict(shared)
        m.update({"xall": xall, "pall": pall, "cs": _rope_tab(pos),
                  "flag": np.full((128, 1), 0.0 if c == 0 else 1.0, np.float32),
                  "ck": np.ascontiguousarray(ck[16 * c:16 * (c + 1)]), "cv": np.ascontiguousarray(cv[16 * c:16 * (c + 1)])})
        in_maps.append(m)
    return in_maps


def kernel(**inp):
    if "nc" not in _NC_CACHE:
        _NC_CACHE["nc"] = build()
    nc = _NC_CACHE["nc"]
    in_maps = make_in_maps(inp)
    res = run_bass_kernel_spmd(nc, in_maps, core_ids=list(range(8)))
    R = res.results
    y_prompt = np.concatenate([np.asarray(r["y_own"]) for r in R], axis=0).reshape(1, 16384, 1024)
    y_sample = np.concatenate([np.asarray(r["y_smp"]) for r in R], axis=0).reshape(128, 1, 1024)
    nkp = np.asarray(R[7]["k_own"]).reshape(1, 1, 2048, 8, 64)
    nvp = np.asarray(R[7]["v_own"]).reshape(1, 1, 2048, 8, 64)
    cat = lambda n: np.concatenate([np.asarray(r[n]) for r in R], axis=0).reshape(1, 128, 1, 8, 64)
    return (y_prompt.astype(np.float32), y_sample.astype(np.float32), nkp.astype(np.float32), nvp.astype(np.float32),
            cat("ks").astype(np.float32), cat("vs").astype(np.float32), cat("cvs").astype(np.float32))
```

```python
import numpy as np
from contextlib import ExitStack
import concourse.bass as bass
import concourse.mybir as mybir
from concourse.bass_utils import run_bass_kernel_spmd

F32 = mybir.dt.float32
BF16 = mybir.dt.bfloat16
I32 = mybir.dt.int32
U32 = mybir.dt.uint32
AF = mybir.ActivationFunctionType
ALU = mybir.AluOpType
AX = mybir.AxisListType

ENGS = ("pe", "act", "dve", "pool", "sp")
EPS = 1e-6
NSLOT = 128
CUT = 99


class Trk:
    __slots__ = ("w", "r", "dsem", "excl")

    def __init__(self):
        self.w = None
        self.r = []
        self.dsem = None
        self.excl = False


class DSem:
    __slots__ = ("sem", "n", "bg")

    def __init__(self, sem):
        self.sem = sem
        self.n = 0
        self.bg = False


class Sched:
    def __init__(self, nc, stack):
        self.nc = nc
        self.stack = stack
        self.ops = {e: [] for e in ENGS}
        self.cnt = {e: 0 for e in ENGS}
        self.sem = {e: stack.enter_context(nc.semaphore("es_" + e)) for e in ENGS}
        self.seen = {e: {} for e in ENGS}
        self.nds = 0
        self.final = []
        self.dsems = []
        self.pending = {e: [] for e in ENGS}

    def new_dsem(self):
        self.nds += 1
        d = DSem(self.stack.enter_context(self.nc.semaphore("ds%d" % self.nds)))
        self.dsems.append(d)
        return d

    def barrier(self, include_bg=False):
        evs = [(self.sem[e], self.cnt[e]) for e in ENGS if self.cnt[e] > 0]
        evs += [(d.sem, 16 * d.n) for d in self.dsems if d.n > 0 and (include_bg or not d.bg)]
        for e in ENGS:
            own = id(self.sem[e])
            for s_, v in evs:
                k = id(s_)
                if e == "pe" and k == own:
                    continue
                if self.seen[e].get(k, 0) >= v:
                    continue
                self.seen[e][k] = v
                self.pending[e].append((s_, v))

    def _deps(self, eng, reads, writes):
        need = {}

        def add(ev):
            if ev is None:
                return
            s, v = ev
            k = id(s)
            if k not in need or need[k][1] < v:
                need[k] = (s, v)
        for t in reads:
            add(t.w)
        for t in writes:
            add(t.w)
            for ev in t.r:
                add(ev)
        waits = []
        seen = self.seen[eng]
        own = id(self.sem[eng])
        for k, (s, v) in need.items():
            if eng == "pe" and k == own:
                continue
            if seen.get(k, 0) >= v:
                continue
            seen[k] = v
            waits.append((s, v))
        if self.pending[eng]:
            waits = self.pending[eng] + waits
            self.pending[eng] = []
        return waits

    def _mark(self, ev, reads, writes):
        for t in writes:
            t.w = ev
            t.r = []
        for t in reads:
            if t not in writes:
                t.r.append(ev)
                if len(t.r) > 64:
                    t.r = t.r[-48:]

    def op(self, eng, fn, reads=(), writes=()):
        ex = [t for t in reads if t.excl and t not in writes]
        if ex:
            writes = list(writes) + ex
        waits = self._deps(eng, reads, writes)
        self.cnt[eng] += 1
        ev = (self.sem[eng], self.cnt[eng])
        self.ops[eng].append((waits, fn, (self.sem[eng], 1)))
        self._mark(ev, reads, writes)
        return ev

    def dma(self, q, fn, reads=(), writes=(), dsem=None, final=False):
        if dsem is None:
            host = writes[0] if writes else reads[0]
            if host.dsem is None:
                host.dsem = self.new_dsem()
            dsem = host.dsem
        waits = self._deps(q, reads, writes)
        if dsem.n > 0:
            k = id(dsem.sem)
            v = 16 * dsem.n
            if self.seen[q].get(k, 0) < v:
                self.seen[q][k] = v
                waits.append((dsem.sem, v))
        dsem.n += 1
        ev = (dsem.sem, 16 * dsem.n)
        self.ops[q].append((waits, fn, (dsem.sem, 16)))
        self._mark(ev, reads, writes)
        if final:
            self.final.append(ev)
        return ev

    def emit(self):
        nc = self.nc
        fin = {}
        for s, v in self.final:
            k = id(s)
            if k not in fin or fin[k][1] < v:
                fin[k] = (s, v)
        finals = list(fin.values())
        with nc.Block() as block:
            def run(engname):
                def body(e):
                    for waits, fn, (s, amt) in self.ops[engname]:
                        for ws, wv in waits:
                            e.wait_ge(ws, wv)
                        fn(e).then_inc(s, amt)
                    if engname == "sp":
                        for ws, wv in finals:
                            e.wait_ge(ws, wv)
                return body
            block.tensor(run("pe"))
            block.scalar(run("act"))
            block.vector(run("dve"))
            block.gpsimd(run("pool"))
            block.sync(run("sp"))


def fv(ap, dims):
    base = ap.ap
    return bass.AP(ap.tensor, ap.offset, [list(base[0])] + [list(d) for d in dims])


class Buf:
    def __init__(self, t):
        self.t = t
        self.k = Trk()


def build(dbg=None, slots1=None, p2=None, p3=None):
    dbg = dbg or {}
    slots1 = list(range(33)) if slots1 is None else slots1
    p2 = list(range(17)) if p2 is None else p2
    p3 = list(range(17)) if p3 is None else p3
    nc = bass.Bass("TRN2", target_bir_lowering=False)

    def din(name, shape, dt=F32):
        return nc.dram_tensor(name, list(shape), dt, kind="ExternalInput").ap()

    def dout(name, shape, dt=F32):
        return nc.dram_tensor(name, list(shape), dt, kind="ExternalOutput").ap()

    xall = din("xall", [33 * 128, 1024]); pall = din("pall", [17 * 128, 256]); cs_d = din("cs", [33 * 128, 16])
    flag_d = din("flag", [128, 1]); ck = din("ck", [16, 2048, 512]); cv = din("cv", [16, 2048, 512])
    ident_d = din("ident", [128, 128]); mask_d = din("maskT", [128, 17 * 128]); sel_d = din("sel", [128, 256])
    iota_d = din("iota16", [128, 16]); tri_d = din("triT", [128, 128])
    g1T_d = din("g1T", [128, 8]); gabT_d = din("gabT", [128, 8]); g2T_d = din("g2T", [128, 8]); g3T_d = din("g3T", [128, 8])
    g2_d = din("g2row", [1, 1024]); gqk_d = din("gqk", [1, 1024]); gv_d = din("gv", [1, 512])
    bsT_d = din("bsT", [128, 8]); w00_d = din("w00", [1, 8]); b0_d = din("b0", [1, 8])
    w_in = din("w_in", [1024, 2560]); wsT_d = din("wsT", [128, 8 * 128]); w_out = din("w_out", [1024, 1024])
    wqT_d = din("wqT", [2048, 1024]); skT_d = din("skT", [128, 16 * 128])
    peer_u = din("peer_u", [16384, 1024]); peer_v = din("peer_v", [16384, 1024])
    w_gate = din("w_gate", [1024, 1024]); w_proj = din("w_proj", [256, 1024])

    y_own = dout("y_own", [2048, 1024]); y_smp = dout("y_smp", [16, 1024])
    k_own = dout("k_own", [2048, 512]); v_own = dout("v_own", [2048, 512])
    ks_o = dout("ks", [16, 512]); vs_o = dout("vs", [16, 512]); cvs_o = dout("cvs", [16, 512])
    dbg_o = {k: dout("dbg_" + k, shp) for k, shp in dbg.items()}

    h1_d = nc.dram_tensor("h1s", [17 * 128, 1024], F32, kind="Internal").ap()
    qs_d = nc.dram_tensor("qsc", [1, 16 * 512], F32, kind="Internal").ap()
    ptab = nc.dram_tensor("ptab", [16384, 2048], BF16, kind="Internal").ap()

    with ExitStack() as st0:
        S = Sched(nc, st0)

        def sbuf(st, name, shape, dt=F32):
            return Buf(st.enter_context(nc.sbuf_tensor("sb_" + name, list(shape), dt)))

        def K(bufs):
            return [b.k for b in bufs]

        def act(out, in_, func, r, w, **kw):
            S.op("act", lambda e: e.activation(out=out, in_=in_, func=func, **kw), K(r), K(w))

        def tt(eng, out, in0, in1, op, r, w):
            S.op(eng, lambda e: e.tensor_tensor(out=out, in0=in0, in1=in1, op=op), K(r), K(w))

        def ts(eng, out, in0, s1, s2, op0, op1, r, w):
            S.op(eng, lambda e: e.tensor_scalar(out=out, in0=in0, scalar1=s1, scalar2=s2, op0=op0, op1=op1), K(r), K(w))

        def tss(eng, out, in_, scalar, op, r, w):
            S.op(eng, lambda e: e.tensor_single_scalar(out=out, in_=in_, scalar=scalar, op=op), K(r), K(w))

        def stt(eng, out, in0, scalar, in1, op0, op1, r, w, accum_out=None):
            if accum_out is None:
                S.op(eng, lambda e: e.scalar_tensor_tensor(out=out, in0=in0, scalar=scalar, in1=in1, op0=op0, op1=op1), K(r), K(w))
            else:
                S.op(eng, lambda e: e.scalar_tensor_tensor(out=out, in0=in0, scalar=scalar, in1=in1, op0=op0, op1=op1, accum_out=accum_out), K(r), K(w))

        def cp(eng, out, in_, r, w):
            S.op(eng, lambda e: e.tensor_copy(out=out, in_=in_), K(r), K(w))

        def mset(eng, out, val, w):
            S.op(eng, lambda e: e.memset(out, val), [], K(w))

        def red(eng, out, in_, op, r, w):
            S.op(eng, lambda e: e.tensor_reduce(out=out, in_=in_, axis=AX.X, op=op), K(r), K(w))

        def recip(out, in_, r, w):
            S.op("dve", lambda e: e.reciprocal(out=out, in_=in_), K(r), K(w))

        def mm(out, lhsT, rhs, start, stop, r, w):
            S.op("pe", lambda e: e.matmul(out, lhsT=lhsT, rhs=rhs, start=start, stop=stop), K(r), K(w))

        def tr(out, in_, ident, r, w):
            S.op("pe", lambda e: e.transpose(out=out, in_=in_, identity=ident), K(r), K(w))

        def dma(q, out, in_, r, w, final=False, dsem=None):
            S.dma(q, lambda e: e.dma_start(out=out, in_=in_), K(r), K(w), final=final, dsem=dsem)

        def dbgout(name, ap, b):
            if name in dbg_o:
                dma("sp", dbg_o[name], ap, [b], [], final=True)

        ident = sbuf(st0, "ident", [128, 128]); identb = sbuf(st0, "identb", [128, 128], BF16)
        iota16 = sbuf(st0, "iota16", [128, 16])
        rstd2_all = sbuf(st0, "rstd2_all", [128, 17])
        dma("sp", ident.t[:], ident_d, [], [ident])
        dma("sp", iota16.t[:], iota_d, [], [iota16])
        act(identb.t[:], ident.t[:], AF.Copy, [ident], [identb])

        PS = [Buf(st0.enter_context(nc.psum_tensor("ps%d" % i, [128, 1024], F32))) for i in range(4)]
        bank = [Buf(None) for _ in range(8)]
        for b_ in bank:
            b_.k.excl = True

        def pbank(i, P=128, lo=0, hi=512):
            return PS[i // 2].t[0:P, (i % 2) * 512 + lo:(i % 2) * 512 + hi]

        def pbank_b(i):
            return PS[i // 2].t[:].bitcast(BF16)[:, (i % 2) * 1024:(i % 2) * 1024 + 1024]

        def rms_scale(st_small, src_ap, P, n, r, name):
            ssq, rs, junk = st_small
            mset("dve", ssq.t[0:P, :], 0.0, [ssq])
            act(junk.t[0:P, 0:n], src_ap, AF.Square, r + [ssq], [junk, ssq], accum_out=ssq.t[0:P, :])
            act(rs.t[0:P, :], ssq.t[0:P, :], AF.Sqrt, [ssq], [rs], bias=EPS, scale=1.0 / n)
            recip(rs.t[0:P, :], rs.t[0:P, :], [rs], [rs])
            return rs

        def load_weight(st_tmp, stage, w_dram, n_chunks, ncols, dst, gcol):
            for c in range(n_chunks):
                sb_ = stage[c % 2]
                dma("sp", sb_.t[:, 0:ncols], w_dram[c * 128:(c + 1) * 128, :], [], [sb_])
                if gcol is None:
                    act(dst.t[:, c, 0:ncols], sb_.t[:, 0:ncols], AF.Copy, [sb_], [dst])
                else:
                    act(dst.t[:, c, 0:ncols], sb_.t[:, 0:ncols], AF.Copy, [sb_, gcol], [dst], scale=gcol.t[:, c:c + 1])

        with ExitStack() as st1:
            S.barrier()
            w_in_b = sbuf(st1, "w_in_b", [128, 8, 2560], BF16)
            w_out_b = sbuf(st1, "w_out_b", [128, 8, 1024], BF16)
            wmT = sbuf(st1, "wmT", [128, 8, 128], BF16)
            KT = sbuf(st1, "KT", [128, 4, 32 * 128], BF16)
            VA = sbuf(st1, "VA", [128, 32, 8, 65], BF16)
            maskb = sbuf(st1, "maskb", [128, 17 * 128], BF16)
            selb = sbuf(st1, "selb", [128, 256], BF16)
            gqk = sbuf(st1, "gqk", [128, 1024]); gv = sbuf(st1, "gv", [128, 512])
            bsT = sbuf(st1, "bsT", [128, 8]); w00 = sbuf(st1, "w00", [128, 8]); b0 = sbuf(st1, "b0", [128, 8])
            g1T = sbuf(st1, "g1T", [128, 8]); gabT = sbuf(st1, "gabT", [128, 8]); flag = sbuf(st1, "flag", [128, 1])
            KTs = [Buf(None) for _ in range(32)]
            VAs = [Buf(None) for _ in range(32)]

            with ExitStack() as sts:
                S.barrier()
                stage = [sbuf(sts, "stage%d" % i, [128, 2560]) for i in range(2)]
                tri = sbuf(sts, "tri", [128, 128])
                for (dst, src) in ((g1T, g1T_d), (gabT, gabT_d), (flag, flag_d), (bsT, bsT_d), (tri, tri_d)):
                    dma("sp", dst.t[:], src, [], [dst])
                dma("sp", gqk.t[:], gqk_d.partition_broadcast(128), [], [gqk])
                dma("sp", gv.t[:], gv_d.partition_broadcast(128), [], [gv])
                dma("sp", w00.t[:], w00_d.partition_broadcast(128), [], [w00])
                dma("sp", b0.t[:], b0_d.partition_broadcast(128), [], [b0])
                load_weight(sts, stage, w_in, 8, 2560, w_in_b, g1T)
                load_weight(sts, stage, w_out, 8, 1024, w_out_b, gabT)
                dma("sp", stage[0].t[:, 0:17 * 128], mask_d, [], [stage[0]])
                act(maskb.t[:], stage[0].t[:, 0:17 * 128], AF.Copy, [stage[0]], [maskb])
                dma("sp", stage[1].t[:, 0:256], sel_d, [], [stage[1]])
                act(selb.t[:], stage[1].t[:, 0:256], AF.Copy, [stage[1]], [selb])
                dma("sp", stage[0].t[:, 0:1024], wsT_d, [], [stage[0]])
                tt("dve", wmT.t[:], fv(stage[0].t[:, 0:1], [(128, 8), (1, 128)]), fv(tri.t[:, 0:1], [(0, 8), (1, 128)]),
                   ALU.mult, [stage[0], tri], [wmT])
                if "wmT" in dbg_o:
                    act(stage[1].t[:, 0:1024], wmT.t[:].rearrange("p g i -> p (g i)"), AF.Copy, [wmT], [stage[1]])
                    dbgout("wmT", stage[1].t[:, 0:1024], stage[1])
                    dbgout("tri", tri.t[:, :], tri)
                mset("dve", VA.t[:, 16:32, :, 64:65], 1.0, [VA])
                cp("dve", VA.t[:, 0:16, :, 64], fv(flag.t[:, 0:1], [(0, 16), (0, 8)]), [flag], [VA])

            with ExitStack() as stt_:
                S.barrier()
                tabk = Buf(None)
                tsems = [S.new_dsem() for _ in range(16)]
                for d_ in tsems:
                    d_.bg = True
                kk_ = 0
                for r0 in range(0, 16384, 1024):
                    for (src_, c0_) in ((peer_u, 0), (peer_v, 1024)):
                        def f_(e, src_=src_, c0_=c0_, r0=r0):
                            return e.dma_start(out=ptab[r0:r0 + 1024, c0_:c0_ + 1024], in_=src_[r0:r0 + 1024, :])
                        S.dma("pool", f_, [], K([tabk]), dsem=tsems[kk_ % 16])
                        kk_ += 1
                xb = [sbuf(stt_, "xb%d" % i, [128, 1024]) for i in range(2)]
                csb = [sbuf(stt_, "csb%d" % i, [128, 16]) for i in range(2)]
                ssq = sbuf(stt_, "ssq", [128, 1]); rs = sbuf(stt_, "rs", [128, 1]); junk = sbuf(stt_, "junk", [128, 1024], BF16)
                small = (ssq, rs, junk)
                n1 = sbuf(stt_, "n1", [128, 1024], BF16); n1T = sbuf(stt_, "n1T", [128, 8, 128], BF16)
                sq = sbuf(stt_, "sq", [128, 1024]); qkn = sbuf(stt_, "qkn", [128, 1024]); qkb = sbuf(stt_, "qkb", [128, 1024], BF16)
                ssh = sbuf(stt_, "ssh", [128, 16]); rh = sbuf(stt_, "rh", [128, 16]); rt = sbuf(stt_, "rt", [128, 4, 16, 8])
                QT = sbuf(stt_, "QT", [128, 4, 256], BF16); vt = sbuf(stt_, "vt", [128, 512])
                mset("dve", QT.t[:, :, :], 0.0, [QT])
                ug = sbuf(stt_, "ug", [128, 1024]); vn = sbuf(stt_, "vn", [128, 512]); vnb = sbuf(stt_, "vnb", [128, 512], BF16)
                gm = sbuf(stt_, "gm", [128, 512]); ab = sbuf(stt_, "ab", [128, 1024], BF16); abT = sbuf(stt_, "abT", [128, 8, 128], BF16)
                eT = [sbuf(stt_, "eT%d" % i, [128, 512], BF16) for i in range(2)]
                rden = sbuf(stt_, "rden", [128, 8]); att = sbuf(stt_, "att", [128, 512]); h1t = sbuf(stt_, "h1t", [128, 1024])
                NBG = 2
                qbc = sbuf(stt_, "qbc", [128, NBG * 512])
                kcs = [sbuf(stt_, "kc%d" % i, [128, NBG * 512]) for i in range(2)]
                vcs = [sbuf(stt_, "vc%d" % i, [128, NBG * 512]) for i in range(2)]
                prod = Buf(None); prod.t = sq.t; prod.k = sq.k
                sc = sbuf(stt_, "sc", [128, NBG * 8]); evt = sbuf(stt_, "evt", [128, NBG, 8, 65], BF16)
                osb = Buf(None); osb.t = ug.t[:, 0:520].rearrange("p (a b c) -> p a b c", a=2, b=4); osb.k = ug.k
                sself = sbuf(stt_, "sself", [128, 8]); qsrc = Buf(None)

                def load_cs(slot):
                    P = 16 if slot == 32 else 128
                    dma("sp", csb[slot % 2].t[0:P, :], cs_d[slot * 128:slot * 128 + P, :], [], [csb[slot % 2]])

                def load_x(slot, with_cs=True):
                    b = slot % 2
                    P = 16 if slot == 32 else 128
                    dma("sp", xb[b].t[0:P, :], xall[slot * 128:slot * 128 + P, :], [], [xb[b]])
                    if with_cs:
                        load_cs(slot)

                def tile1(slot, nxt):
                    mode = "halo" if slot < 16 else ("own" if slot < 32 else "sample")
                    P = 16 if mode == "sample" else 128
                    x = xb[slot % 2]; c_s = csb[slot % 2]
                    if nxt is not None:
                        load_x(nxt)
                    r1 = rms_scale(small, x.t[0:P, :], P, 1024, [x], "r1")
                    act(n1.t[0:P, :], x.t[0:P, :], AF.Copy, [x, r1], [n1], scale=r1.t[0:P, 0:1])
                    b3 = pbank_b(3)
                    for c in range(8):
                        tr(b3[:, c * 128:c * 128 + P], n1.t[0:P, c * 128:(c + 1) * 128], identb.t[0:P, 0:P], [n1, identb], [bank[3]])
                    act(n1T.t[:, :, 0:P], fv(b3[:, 0:1], [(128, 8), (1, P)]), AF.Copy, [bank[3]], [n1T])
                    colblocks = [(1, 512), (2, 1024)] if mode == "halo" else [(0, 0), (1, 512), (2, 1024), (4, 1536), (5, 2048)]
                    for bk, col in colblocks:
                        for c in range(8):
                            mm(pbank(bk, P), n1T.t[:, c, 0:P], w_in_b.t[:, c, col:col + 512], c == 0, c == 7, [n1T, w_in_b], [bank[bk]])
                    zqk = PS[0].t[0:P, :]
                    act(sq.t[0:P, :], zqk, AF.Square, [bank[0], bank[1]], [sq])
                    red("dve", ssh.t[0:P, :], sq.t[0:P, :].rearrange("p (a b) -> p a b", a=16), ALU.add, [sq], [ssh])
                    act(rh.t[0:P, :], ssh.t[0:P, :], AF.Sqrt, [ssh], [rh], bias=EPS, scale=1.0 / 64)
                    recip(rh.t[0:P, :], rh.t[0:P, :], [rh], [rh])
                    q3 = qkn.t[0:P, :].rearrange("p (a b) -> p a b", a=16)
                    tt("dve", q3, zqk.rearrange("p (a b) -> p a b", a=16), fv(rh.t[0:P, 0:1], [(1, 16), (0, 64)]), ALU.mult,
                       [bank[0], bank[1], rh], [qkn])
                    tt("dve", qkn.t[0:P, :], qkn.t[0:P, :], gqk.t[0:P, :], ALU.mult, [qkn, gqk], [qkn])
                    x1 = q3[:, :, 0:8]; x2 = q3[:, :, 8:16]
                    cosb = fv(c_s.t[0:P, 0:1], [(0, 16), (1, 8)]); sinb = fv(c_s.t[0:P, 8:9], [(0, 16), (1, 8)])
                    tt("dve", rt.t[0:P, 0], x1, cosb, ALU.mult, [qkn, c_s], [rt])
                    tt("dve", rt.t[0:P, 1], x2, sinb, ALU.mult, [qkn, c_s], [rt])
                    tt("dve", rt.t[0:P, 2], x2, cosb, ALU.mult, [qkn, c_s], [rt])
                    tt("dve", rt.t[0:P, 3], x1, sinb, ALU.mult, [qkn, c_s], [rt])
                    tt("dve", x1, rt.t[0:P, 0], rt.t[0:P, 1], ALU.subtract, [rt], [qkn])
                    tt("dve", x2, rt.t[0:P, 2], rt.t[0:P, 3], ALU.add, [rt], [qkn])
                    act(qkb.t[0:P, :], qkn.t[0:P, :], AF.Copy, [qkn], [qkb])
                    if CUT <= -2:
                        return
                    if mode == "own":
                        o0 = (slot - 16) * 128
                        dma("sp", k_own[o0:o0 + 128, :], qkn.t[:, 512:1024], [qkn], [], final=True)
                        if CUT <= -1:
                            return
                    elif mode == "sample":
                        dma("sp", ks_o, qkn.t[0:16, 512:1024], [qkn], [], final=True)
                    crange = range(4, 8) if mode == "halo" else range(8)
                    for c in crange:
                        tr(b3[:, c * 128:c * 128 + P], qkb.t[0:P, c * 128:(c + 1) * 128], identb.t[0:P, 0:P], [qkb, identb], [bank[3]])
                    if mode == "own":
                        act(QT.t[0:64, :, 0:128], fv(b3[0:64, 0:1], [(128, 4), (1, 128)]), AF.Copy, [bank[3]], [QT])
                        act(QT.t[64:128, :, 128:256], fv(b3[64:128, 0:1], [(128, 4), (1, 128)]), AF.Copy, [bank[3]], [QT])
                    if mode != "sample":
                        cp("dve", KT.t[:, :, slot * 128:(slot + 1) * 128], fv(b3[:, 512:513], [(128, 4), (1, 128)]), [bank[3]], [KTs[slot]])
                    if CUT <= 0 and mode == "own":
                        return
                    if mode != "halo":
                        act(vt.t[0:P, :], pbank(2, P), AF.Copy, [bank[2]], [vt])
                        if mode == "own":
                            dma("sp", v_own[o0:o0 + 128, :], vt.t[:, :], [vt], [], final=True)
                        else:
                            dma("sp", vs_o, vt.t[0:16, :], [vt], [], final=True)
                    if mode != "sample":
                        cp("dve", VA.t[:, slot, :, 0:64], pbank(2, P).rearrange("p (h c) -> p h c", h=8), [bank[2]], [VAs[slot]])
                    if mode == "halo" or CUT <= 1:
                        return
                    act(ug.t[0:P, :], PS[2].t[0:P, :], AF.Gelu_apprx_tanh, [bank[4], bank[5]], [ug])
                    act(sq.t[0:P, 0:512], ug.t[0:P, 512:1024], AF.Square, [ug], [sq])
                    red("dve", ssh.t[0:P, 0:8], sq.t[0:P, 0:512].rearrange("p (a b) -> p a b", a=8), ALU.add, [sq], [ssh])
                    act(rh.t[0:P, 0:8], ssh.t[0:P, 0:8], AF.Sqrt, [ssh], [rh], bias=EPS, scale=1.0 / 64)
                    recip(rh.t[0:P, 0:8], rh.t[0:P, 0:8], [rh], [rh])
                    vn3 = vn.t[0:P, :].rearrange("p (a b) -> p a b", a=8)
                    tt("dve", vn3, ug.t[0:P, 512:1024].rearrange("p (a b) -> p a b", a=8), fv(rh.t[0:P, 0:1], [(1, 8), (0, 64)]),
                       ALU.mult, [ug, rh], [vn])
                    tt("dve", vn.t[0:P, :], vn.t[0:P, :], gv.t[0:P, :], ALU.mult, [vn, gv], [vn])
                    gm3 = gm.t[0:P, :].rearrange("p (a b) -> p a b", a=8)
                    if mode == "own":
                        act(vnb.t[0:P, :], vn.t[0:P, :], AF.Copy, [vn], [vnb])
                        for g in range(8):
                            mm(pbank(2, P, g * 64, (g + 1) * 64), wmT.t[:, g, 0:P], vnb.t[:, g * 64:(g + 1) * 64], True, True, [wmT, vnb], [bank[2]])
                        tt("dve", gm3, pbank(2, P).rearrange("p (a b) -> p a b", a=8), fv(bsT.t[0:P, 0:1], [(1, 8), (0, 64)]), ALU.add,
                           [bank[2], bsT], [gm])
                    else:
                        dma("sp", cvs_o, vn.t[0:16, :], [vn], [], final=True)
                        tt("dve", gm3, vn3, fv(w00.t[0:P, 0:1], [(1, 8), (0, 64)]), ALU.mult, [vn, w00], [gm])
                        tt("dve", gm3, gm3, fv(b0.t[0:P, 0:1], [(1, 8), (0, 64)]), ALU.add, [gm, b0], [gm])
                    if slot == 16:
                        dbgout("ug0", ug.t[:, :], ug)
                        dbgout("vn0", vn.t[:, :], vn)
                        dbgout("mix0", gm.t[:, :], gm)
                    tt("dve", gm.t[0:P, :], gm.t[0:P, :], ug.t[0:P, 0:512], ALU.mult, [gm, ug], [gm])
                    rb = rms_scale(small, gm.t[0:P, :], P, 512, [gm], "rb")
                    act(ab.t[0:P, 512:1024], gm.t[0:P, :], AF.Copy, [gm, rb], [ab], scale=rb.t[0:P, 0:1])
                    if CUT <= 2:
                        return
                    if mode == "own":
                        i0 = slot - 16
                        groups = [(hp, grp) for hp in range(4) for grp in range(9)]

                        def a_scores(gi):
                            hp, grp = groups[gi]
                            j0 = grp * 2
                            nj = min(2, 17 - j0)
                            sb_i = gi % 2
                            for jj in range(nj):
                                s_ = i0 + j0 + jj
                                mm(pbank(sb_i, 128, jj * 256, (jj + 1) * 256), KT.t[:, hp, s_ * 128:(s_ + 1) * 128],
                                   QT.t[:, hp, :], True, True, [KTs[s_], QT], [bank[sb_i]])

                        def a_soft(gi):
                            hp, grp = groups[gi]
                            j0 = grp * 2
                            nj = min(2, 17 - j0)
                            sb_i = gi % 2
                            et = eT[sb_i]
                            act(et.t[:, 0:nj * 256], pbank(sb_i, 128, 0, nj * 256), AF.Exp, [bank[sb_i]], [et], scale=0.125)
                            tt("pool", et.t[:, 0:nj * 256].rearrange("p (j e q) -> p j e q", j=nj, e=2),
                               et.t[:, 0:nj * 256].rearrange("p (j e q) -> p j e q", j=nj, e=2),
                               fv(maskb.t[:, j0 * 128:j0 * 128 + 1], [(128, nj), (0, 2), (1, 128)]), ALU.mult, [et, maskb], [et])

                        def a_pv(gi):
                            hp, grp = groups[gi]
                            j0 = grp * 2
                            nj = min(2, 17 - j0)
                            et = eT[gi % 2]
                            for jj in range(nj):
                                s_ = i0 + j0 + jj
                                j = j0 + jj
                                for e_ in range(2):
                                    h = 2 * hp + e_
                                    ob = 6 + e_
                                    o_ap = pbank(ob, 128, hp * 65, hp * 65 + 65)
                                    c0 = jj * 256 + e_ * 128
                                    mm(o_ap, et.t[:, c0:c0 + 128], VA.t[:, s_, h, :], j == 0, j == 16, [et, VAs[s_], VA], [bank[ob]])

                        a_scores(0)
                        for gi in range(len(groups)):
                            if gi + 1 < len(groups):
                                a_scores(gi + 1)
                            a_soft(gi)
                            a_pv(gi)
                        o_src = fv(PS[3].t[0:128, 0:1], [(512, 2), (65, 4), (1, 65)])
                        o_r = [bank[6], bank[7]]
                        att_v = fv(att.t[0:P, 0:1], [(64, 2), (128, 4), (1, 64)])
                        rden_v = fv(rden.t[0:P, 0:1], [(1, 2), (2, 4)])
                        rden_b = fv(rden.t[0:P, 0:1], [(1, 2), (2, 4), (0, 64)])
                    else:
                        dma("sp", qs_d.rearrange("o (b f) -> (o b) f", b=16), qkn.t[0:16, 0:512], [qkn], [qsrc])
                        first = True
                        for bg in range(16 // NBG):
                            dma("sp", qbc.t[:, :], qs_d[0:1, bg * NBG * 512:(bg + 1) * NBG * 512].partition_broadcast(128), [qsrc], [qbc])
                            for pi, dil in enumerate((1, 4, 16)):
                                st_ = 2048 - 128 * dil
                                kc = kcs[(bg * 3 + pi) % 2]; vc = vcs[(bg * 3 + pi) % 2]
                                dma("sp", kc.t[:, :].rearrange("p (b f) -> p b f", b=NBG),
                                    ck[bg * NBG:(bg + 1) * NBG, st_:2048:dil, :].rearrange("b k f -> k b f"), [], [kc])
                                dma("sp", vc.t[:, :].rearrange("p (b f) -> p b f", b=NBG),
                                    cv[bg * NBG:(bg + 1) * NBG, st_:2048:dil, :].rearrange("b k f -> k b f"), [], [vc])
                                tt("pool", prod.t[:, :], kc.t[:, :], qbc.t[:, :], ALU.mult, [kc, qbc], [prod])
                                red("dve", sc.t[:, :], prod.t[:, :].rearrange("p (a b) -> p a b", a=NBG * 8), ALU.add, [prod], [sc])
                                act(sc.t[:, :], sc.t[:, :], AF.Exp, [sc], [sc], scale=0.125)
                                tt("dve", evt.t[:, :, :, 0:64], vc.t[:, :].rearrange("p (b h c) -> p b h c", b=NBG, h=8),
                                   fv(sc.t[:, 0:1], [(8, NBG), (1, 8), (0, 64)]), ALU.mult, [vc, sc], [evt])
                                cp("dve", evt.t[:, :, :, 64], sc.t[:, :].rearrange("p (b h) -> p b h", b=NBG), [sc], [evt])
                                for bb in range(NBG):
                                    bglob = bg * NBG + bb
                                    last = (bg == 16 // NBG - 1 and pi == 2 and bb == NBG - 1)
                                    for half in range(2):
                                        mm(pbank(6 + half, 16, 0, 260), selb.t[:, bglob * 16:(bglob + 1) * 16],
                                           evt.t[:, bb, half * 4:(half + 1) * 4, :].rearrange("p h c -> p (h c)"), first, last, [selb, evt], [bank[6 + half]])
                                    first = False
                        cp("dve", osb.t[0:16], fv(PS[3].t[0:16, 0:1], [(512, 2), (65, 4), (1, 65)]), [bank[6], bank[7]], [osb])
                        tt("dve", prod.t[0:16, 0:512], qkn.t[0:16, 0:512], qkn.t[0:16, 512:1024], ALU.mult, [qkn], [prod])
                        red("dve", sself.t[0:16, :], prod.t[0:16, 0:512].rearrange("p (a b) -> p a b", a=8), ALU.add, [prod], [sself])
                        act(sself.t[0:16, :], sself.t[0:16, :], AF.Exp, [sself], [sself], scale=0.125)
                        tss("dve", sself.t[0:16, :], sself.t[0:16, :], 3.0, ALU.mult, [sself], [sself])
                        o4 = osb.t[0:16].rearrange("p a b c -> p (a b) c")
                        tt("dve", prod.t[0:16, 0:512].rearrange("p (h c) -> p h c", h=8), vt.t[0:16, :].rearrange("p (h c) -> p h c", h=8),
                           fv(sself.t[0:16, 0:1], [(1, 8), (0, 64)]), ALU.mult, [vt, sself], [prod])
                        tt("dve", o4[:, :, 0:64], o4[:, :, 0:64], prod.t[0:16, 0:512].rearrange("p (h c) -> p h c", h=8), ALU.add, [osb, prod], [osb])
                        tt("dve", o4[:, :, 64], o4[:, :, 64], sself.t[0:16, :], ALU.add, [osb, sself], [osb])
                        o_src = osb.t[0:16]
                        o_r = [osb]
                        att_v = att.t[0:P, :].rearrange("p (a b c) -> p a b c", a=2, b=4)
                        rden_v = rden.t[0:P, :].rearrange("p (a b) -> p a b", a=2)
                        rden_b = fv(rden.t[0:P, 0:1], [(4, 2), (1, 4), (0, 64)])
                    if CUT <= 3:
                        return
                    recip(rden_v, o_src[:, :, :, 64], o_r, [rden])
                    tt("dve", att_v, o_src[:, :, :, 0:64], rden_b, ALU.mult, o_r + [rden], [att])
                    ra = rms_scale(small, att.t[0:P, :], P, 512, [att], "ra")
                    act(ab.t[0:P, 0:512], att.t[0:P, :], AF.Copy, [att, ra], [ab], scale=ra.t[0:P, 0:1])
                    for c in range(8):
                        tr(b3[:, c * 128:c * 128 + P], ab.t[0:P, c * 128:(c + 1) * 128], identb.t[0:P, 0:P], [ab, identb], [bank[3]])
                    act(abT.t[:, :, 0:P], fv(b3[:, 0:1], [(128, 8), (1, P)]), AF.Copy, [bank[3]], [abT])
                    for nb in range(2):
                        for c in range(8):
                            mm(pbank(4 + nb, P), abT.t[:, c, 0:P], w_out_b.t[:, c, nb * 512:(nb + 1) * 512], c == 0, c == 7, [abT, w_out_b], [bank[4 + nb]])
                    tt("dve", h1t.t[0:P, :], PS[2].t[0:P, :], x.t[0:P, :], ALU.add, [bank[4], bank[5], x], [h1t])
                    ti = slot - 16
                    dma("sp", h1_d[ti * 128:ti * 128 + P, :], h1t.t[0:P, :], [h1t], [h1s[ti]], dsem=h1sems[ti % 2])
                    if ti == 0:
                        dbgout("att0", att.t[:, :], att)
                        dbgout("gm0", gm.t[:, :], gm)
                        dbgout("h10", h1t.t[:, :], h1t)
                    if mode == "sample":
                        dbgout("atts", att.t[:, :], att)
                        dbgout("h1s_", h1t.t[:, :], h1t)

                def halo_bufs(slot):
                    return (n1, n1T) if slot % 2 == 0 else (ab, abT)

                def halo_front(slot, nxt):
                    x = xb[slot % 2]
                    if nxt is not None:
                        load_x(nxt, with_cs=False)
                    n1_, n1T_ = halo_bufs(slot)
                    r1 = rms_scale(small, x.t[:, :], 128, 1024, [x], "r1")
                    act(n1_.t[:, :], x.t[:, :], AF.Copy, [x, r1], [n1_], scale=r1.t[:, 0:1])
                    b3 = pbank_b(3)
                    for c in range(8):
                        tr(b3[:, c * 128:(c + 1) * 128], n1_.t[:, c * 128:(c + 1) * 128], identb.t[:, :], [n1_, identb], [bank[3]])
                    act(n1T_.t[:, :, :], fv(b3[:, 0:1], [(128, 8), (1, 128)]), AF.Copy, [bank[3]], [n1T_])

                def halo_back(slot):
                    c_s = csb[slot % 2]
                    n1_, n1T_ = halo_bufs(slot)
                    b3 = pbank_b(3)
                    for bk, col in ((1, 512), (2, 1024)):
                        for c in range(8):
                            mm(pbank(bk), n1T_.t[:, c, :], w_in_b.t[:, c, col:col + 512], c == 0, c == 7, [n1T_, w_in_b], [bank[bk]])
                    zk = pbank(1)
                    act(sq.t[:, 0:512], zk, AF.Square, [bank[1]], [sq])
                    red("dve", ssh.t[:, 0:8], sq.t[:, 0:512].rearrange("p (a b) -> p a b", a=8), ALU.add, [sq], [ssh])
                    act(rh.t[:, 0:8], ssh.t[:, 0:8], AF.Sqrt, [ssh], [rh], bias=EPS, scale=1.0 / 64)
                    recip(rh.t[:, 0:8], rh.t[:, 0:8], [rh], [rh])
                    k3 = qkn.t[:, 512:1024].rearrange("p (a b) -> p a b", a=8)
                    tt("dve", k3, zk.rearrange("p (a b) -> p a b", a=8), fv(rh.t[:, 0:1], [(1, 8), (0, 64)]), ALU.mult, [bank[1], rh], [qkn])
                    tt("dve", qkn.t[:, 512:1024], qkn.t[:, 512:1024], gqk.t[:, 512:1024], ALU.mult, [qkn, gqk], [qkn])
                    x1 = k3[:, :, 0:8]; x2 = k3[:, :, 8:16]
                    cosb = fv(c_s.t[:, 0:1], [(0, 8), (1, 8)]); sinb = fv(c_s.t[:, 8:9], [(0, 8), (1, 8)])
                    tt("dve", rt.t[:, 0, 0:8], x1, cosb, ALU.mult, [qkn, c_s], [rt])
                    tt("dve", rt.t[:, 1, 0:8], x2, sinb, ALU.mult, [qkn, c_s], [rt])
                    tt("dve", rt.t[:, 2, 0:8], x2, cosb, ALU.mult, [qkn, c_s], [rt])
                    tt("dve", rt.t[:, 3, 0:8], x1, sinb, ALU.mult, [qkn, c_s], [rt])
                    tt("dve", x1, rt.t[:, 0, 0:8], rt.t[:, 1, 0:8], ALU.subtract, [rt], [qkn])
                    tt("dve", x2, rt.t[:, 2, 0:8], rt.t[:, 3, 0:8], ALU.add, [rt], [qkn])
                    act(qkb.t[:, 512:1024], qkn.t[:, 512:1024], AF.Copy, [qkn], [qkb])
                    for c in range(4, 8):
                        tr(b3[:, c * 128:(c + 1) * 128], qkb.t[:, c * 128:(c + 1) * 128], identb.t[:, :], [qkb, identb], [bank[3]])
                    cp("dve", KT.t[:, :, slot * 128:(slot + 1) * 128], fv(b3[:, 512:513], [(128, 4), (1, 128)]), [bank[3]], [KTs[slot]])
                    cp("dve", VA.t[:, slot, :, 0:64], pbank(2).rearrange("p (h c) -> p h c", h=8), [bank[2]], [VAs[slot]])

                h1s = [Buf(None) for _ in range(17)]
                h1sems = [S.new_dsem() for _ in range(2)]
                if slots1 == list(range(33)):
                    load_x(0)
                    load_cs(1)
                    halo_front(0, 1)
                    for s_ in range(16):
                        if s_ + 1 < 16:
                            halo_front(s_ + 1, s_ + 2)
                        halo_back(s_)
                        if s_ + 2 <= 16:
                            load_cs(s_ + 2)
                    for slot in range(16, 33):
                        tile1(slot, slot + 1 if slot + 1 <= 32 else None)
                else:
                    if slots1:
                        load_x(slots1[0])
                    for si, slot in enumerate(slots1):
                        tile1(slot, slots1[si + 1] if si + 1 < len(slots1) else None)

        eidx_all = sbuf(st0, "eidx_all", [128, 17, NSLOT], I32)
        g_all = sbuf(st0, "g_all", [128, 17, NSLOT])
        w_gate_b = sbuf(st0, "w_gate_b", [128, 8, 1024], BF16)
        w_proj_b = sbuf(st0, "w_proj_b", [128, 2, 1024], BF16)
        g2bc = sbuf(st0, "g2bc", [128, 1024]); g3T = sbuf(st0, "g3T", [128, 8])
        with ExitStack() as st2:
            S.barrier()
            Wqk = sbuf(st2, "Wqk", [128, 8, 2048], BF16)
            Wqk_e = Buf(None); Wqk_e.t = Wqk.t
            Wqk_o = Buf(None); Wqk_o.t = Wqk.t
            g2T = sbuf(st2, "g2T", [128, 8])
            dma("sp", g2T.t[:], g2T_d, [], [g2T])
            with ExitStack() as sts:
                S.barrier()
                skT = sbuf(sts, "skT", [128, 2048])
                wq = [sbuf(sts, "wq%d" % i, [128, 1024]) for i in range(2)]
                dma("sp", skT.t[:], skT_d, [], [skT])
                for b in range(16):
                    w_ = wq[b % 2]
                    dma("sp", w_.t[:], wqT_d[b * 128:(b + 1) * 128, :], [], [w_])
                    for dc in range(8):
                        pb_i = 2 * (b % 2) + dc % 2
                        mm(pbank(pb_i, 128, (dc // 2) * 128, (dc // 2) * 128 + 128), w_.t[:, dc * 128:(dc + 1) * 128], skT.t[:, b * 128:(b + 1) * 128],
                           True, True, [w_, skT], [bank[pb_i]])
                    for dc in range(8):
                        pb_i = 2 * (b % 2) + dc % 2
                        if dc % 2 == 0:
                            act(Wqk.t[:, dc, b * 128:(b + 1) * 128], pbank(pb_i, 128, (dc // 2) * 128, (dc // 2) * 128 + 128), AF.Copy,
                                [bank[pb_i], g2T], [Wqk_e], scale=g2T.t[:, dc:dc + 1])
                        else:
                            tss("dve", Wqk.t[:, dc, b * 128:(b + 1) * 128], pbank(pb_i, 128, (dc // 2) * 128, (dc // 2) * 128 + 128),
                                g2T.t[:, dc:dc + 1], ALU.mult, [bank[pb_i], g2T], [Wqk_o])
            with ExitStack() as stt_:
                S.barrier()
                stg3 = [sbuf(stt_, "stg3_%d" % i, [128, 1024]) for i in range(2)]
                hb_ = [sbuf(stt_, "h1b%d" % i, [128, 1024]) for i in range(2)]
                ssq = sbuf(stt_, "ssq2", [128, 1]); rs = sbuf(stt_, "rs2", [128, 1]); junk = sbuf(stt_, "junk2", [128, 1024], BF16)
                hbf = sbuf(stt_, "hbf", [128, 1024], BF16); hT = sbuf(stt_, "hT", [128, 8, 128], BF16)
                ssbs = [sbuf(stt_, "ssb%d" % i, [128, 2048]) for i in range(2)]; s2 = sbuf(stt_, "s2b", [128, 2048])
                tops = sbuf(stt_, "tops", [128, 16, 16]); topi = sbuf(stt_, "topi", [128, 16, 16], U32); topf = sbuf(stt_, "topf", [128, 16, 16])
                cand = sbuf(stt_, "cand", [128, 8, 256]); cand2 = sbuf(stt_, "cand2", [128, 8, 256])
                best = sbuf(stt_, "best", [128, 8, 16]); pos = sbuf(stt_, "pos", [128, 8, 16], U32)
                pa = sbuf(stt_, "pa", [128, 8, 16], I32); pb_ = sbuf(stt_, "pb", [128, 8, 16], I32)
                paf = sbuf(stt_, "paf", [128, 8, 16]); pbf = sbuf(stt_, "pbf", [128, 8, 16])
                oh = sbuf(stt_, "oh", [128, 8, 16, 16]); e0 = sbuf(stt_, "e0", [128, 8, 16]); e1 = sbuf(stt_, "e1", [128, 8, 16])
                mx = sbuf(stt_, "mx", [128, 8]); sm = sbuf(stt_, "sm", [128, 8]); gex = sbuf(stt_, "gex", [128, 8, 16]); gsum = sbuf(stt_, "gsum", [128, 8, 16])

                def load_h(ti, bufs):
                    P = 16 if ti == 16 else 128
                    dma("sp", bufs[ti % 2].t[0:P, :], h1_d[ti * 128:ti * 128 + P, :], [h1s[ti]], [bufs[ti % 2]])

                def front2(ti, nxt):
                    P = 16 if ti == 16 else 128
                    h = hb_[ti % 2]
                    ssb = ssbs[ti % 2]
                    if nxt is not None:
                        load_h(nxt, hb_)
                    r2 = rms_scale((ssq, rs, junk), h.t[0:P, :], P, 1024, [h], "r2")
                    cp("dve", rstd2_all.t[0:P, ti:ti + 1], r2.t[0:P, :], [r2], [rstd2_all])
                    act(hbf.t[0:P, :], h.t[0:P, :], AF.Copy, [h], [hbf])
                    b3 = pbank_b(7)
                    for c in range(8):
                        tr(b3[:, c * 128:c * 128 + P], hbf.t[0:P, c * 128:(c + 1) * 128], identb.t[0:P, 0:P], [hbf, identb], [bank[7]])
                    act(hT.t[:, :, 0:P], fv(b3[:, 0:1], [(128, 8), (1, P)]), AF.Copy, [bank[7]], [hT])
                    for nb in range(4):
                        for c in range(8):
                            mm(pbank(nb, P), hT.t[:, c, 0:P], Wqk.t[:, c, nb * 512:(nb + 1) * 512], c == 0, c == 7, [hT, Wqk_e, Wqk_o], [bank[nb]])
                    for half in range(2):
                        act(ssb.t[0:P, half * 1024:(half + 1) * 1024], PS[half].t[0:P, :], AF.Copy, [bank[2 * half], bank[2 * half + 1], r2], [ssb],
                            scale=r2.t[0:P, 0:1])

                def back2(ti):
                    P = 16 if ti == 16 else 128
                    ssb = ssbs[ti % 2]

                    def top16(src, scr, g, n, vals, idxs):
                        sv = src.t[0:P, g * n:(g + 1) * n] if len(src.t.shape) == 2 else src.t[0:P, g, :]
                        s2v = scr.t[0:P, g * n:(g + 1) * n] if len(scr.t.shape) == 2 else scr.t[0:P, g, :]
                        v0 = vals.t[0:P, g, 0:8]; v1 = vals.t[0:P, g, 8:16]
                        S.op("dve", lambda e: e.max(out=v0, in_=sv), K([src]), K([vals]))
                        S.op("dve", lambda e: e.max_index(out=idxs.t[0:P, g, 0:8], in_max=v0, in_values=sv), K([src, vals]), K([idxs]))
                        S.op("dve", lambda e: e.match_replace(out=s2v, in_to_replace=v0, in_values=sv, imm_value=-1e30), K([src, vals]), K([scr]))
                        S.op("dve", lambda e: e.max(out=v1, in_=s2v), K([scr]), K([vals]))
                        S.op("dve", lambda e: e.max_index(out=idxs.t[0:P, g, 8:16], in_max=v1, in_values=s2v), K([scr, vals]), K([idxs]))

                    for g in range(16):
                        top16(ssb, s2, g, 128, tops, topi)
                    cp("dve", topf.t[0:P], topi.t[0:P], [topi], [topf])
                    c4 = cand.t[0:P].rearrange("p h (a b) -> p h a b", a=16)
                    tt("dve", c4, fv(tops.t[0:P, 0, 0:1], [(32, 8), (1, 16), (0, 16)]), fv(tops.t[0:P, 1, 0:1], [(32, 8), (0, 16), (1, 16)]),
                       ALU.add, [tops], [cand])
                    for hh in range(8):
                        top16(cand, cand2, hh, 256, best, pos)
                    tss("dve", pa.t[0:P], pos.t[0:P].bitcast(I32), 4, ALU.arith_shift_right, [pos], [pa])
                    tss("dve", pb_.t[0:P], pos.t[0:P].bitcast(I32), 15, ALU.bitwise_and, [pos], [pb_])
                    cp("dve", paf.t[0:P], pa.t[0:P], [pa], [paf])
                    cp("dve", pbf.t[0:P], pb_.t[0:P], [pb_], [pbf])
                    iob = fv(iota16.t[0:P, 0:1], [(0, 8), (0, 16), (1, 16)])
                    for (pf, pidx, eo) in ((paf, 0, e0), (pbf, 1, e1)):
                        tt("dve", oh.t[0:P], iob, fv(pf.t[0:P, 0, 0:1], [(16, 8), (1, 16), (0, 16)]), ALU.is_equal, [iota16, pf], [oh])
                        tt("dve", oh.t[0:P], oh.t[0:P], fv(topf.t[0:P, pidx, 0:1], [(32, 8), (0, 16), (1, 16)]), ALU.mult, [oh, topf], [oh])
                        red("dve", eo.t[0:P], oh.t[0:P], ALU.add, [oh], [eo])
                    stt("dve", e0.t[0:P], e0.t[0:P], 128.0, e1.t[0:P], ALU.mult, ALU.add, [e0, e1], [e0])
                    cp("dve", eidx_all.t[0:P, ti, :], e0.t[0:P].rearrange("p h k -> p (h k)"), [e0], [eidx_all])
                    red("dve", mx.t[0:P], best.t[0:P], ALU.max, [best], [mx])
                    tt("dve", best.t[0:P], best.t[0:P], fv(mx.t[0:P, 0:1], [(1, 8), (0, 16)]), ALU.subtract, [best, mx], [best])
                    act(best.t[0:P], best.t[0:P], AF.Exp, [best], [best])
                    red("dve", sm.t[0:P], best.t[0:P], ALU.add, [best], [sm])
                    recip(sm.t[0:P], sm.t[0:P], [sm], [sm])
                    tt("dve", g_all.t[0:P, ti, :].rearrange("p (h k) -> p h k", h=8), best.t[0:P], fv(sm.t[0:P, 0:1], [(1, 8), (0, 16)]),
                       ALU.mult, [best, sm], [g_all])
                    if ti == 0:
                        dbgout("s0", ssb.t[:, :], ssb)
                        dbgout("eidx0", e0.t[:].rearrange("p h k -> p (h k)"), e0)
                        dbgout("g0", g_all.t[:, 0, :], g_all)

                if p2:
                    load_h(p2[0], hb_)
                    front2(p2[0], p2[1] if len(p2) > 1 else None)
                dma("sp", g3T.t[:], g3T_d, [], [g3T])
                dma("sp", g2bc.t[:], g2_d.partition_broadcast(128), [], [g2bc])
                load_weight(stt_, stg3, w_gate, 8, 1024, w_gate_b, g3T)
                load_weight(stt_, stg3, w_proj, 2, 1024, w_proj_b, None)
                for i_, ti in enumerate(p2):
                    if i_ + 1 < len(p2):
                        front2(p2[i_ + 1], p2[i_ + 2] if i_ + 2 < len(p2) else None)
                    back2(ti)

        with ExitStack() as st3:
            S.barrier(include_bg=True)
            with ExitStack() as stt_:
                S.barrier()
                NB = 24
                hb_ = [sbuf(stt_, "h3b%d" % i, [128, 1024]) for i in range(2)]
                pbuf = [sbuf(stt_, "pbuf%d" % i, [128, 256]) for i in range(2)]
                gb = [sbuf(stt_, "gb%d" % i, [128, 2048], BF16) for i in range(NB)]
                dgs = [sbuf(stt_, "dg%d" % i, [128, 128], BF16) for i in range(4)]
                n2b = sbuf(stt_, "n2b", [128, 1024], BF16); junkb = sbuf(stt_, "junkb", [128, 1024], BF16)
                accs = [sbuf(stt_, "acc%d" % i, [128, 1024]) for i in range(2)]
                actv = sbuf(stt_, "actv", [128, NSLOT]); coef = sbuf(stt_, "coef", [128, NSLOT]); coefg = sbuf(stt_, "coefg", [128, NSLOT])
                ssq = sbuf(stt_, "ssq3", [128, 1]); rs = sbuf(stt_, "rs3", [128, 1]); junk = sbuf(stt_, "junk3", [128, 1024], BF16)
                hbf = sbuf(stt_, "hbf3", [128, 1024], BF16); hT = sbuf(stt_, "hT3", [128, 8, 128], BF16)
                pbf16 = sbuf(stt_, "pbf16", [128, 256], BF16); pT = sbuf(stt_, "pT", [128, 2, 128], BF16)
                sg = sbuf(stt_, "sg", [128, 1024]); yt = sbuf(stt_, "yt", [128, 1024])

                def load3(ti):
                    P = 16 if ti == 16 else 128
                    dma("sp", hb_[ti % 2].t[0:P, :], h1_d[ti * 128:ti * 128 + P, :], [h1s[ti]], [hb_[ti % 2]])
                    dma("sp", pbuf[ti % 2].t[0:P, :], pall[ti * 128:ti * 128 + P, :], [], [pbuf[ti % 2]])

                def gather(dstb, ti, slot, P):
                    off = eidx_all.t[0:P, ti, slot:slot + 1]
                    S.dma("pool", lambda e: e.indirect_dma_start(out=dstb.t[0:P, :], out_offset=None, in_=ptab,
                                                                in_offset=bass.IndirectOffsetOnAxis(ap=off, axis=0)),
                          K([eidx_all]), K([dstb]))

                BS = 8
                NBAT = NSLOT // BS

                def colbufs(buf):
                    out = []
                    for _ in range(NBAT):
                        b_ = Buf(None); b_.t = buf.t
                        out.append(b_)
                    return out
                actv_b = colbufs(actv); coef_b = colbufs(coef); coefg_b = colbufs(coefg)

                gcnt = [0]

                def acc_add(ti):
                    P = 16 if ti == 16 else 128
                    h = hb_[ti % 2]; acc = accs[ti % 2]
                    tt("dve", acc.t[0:P, :], PS[0].t[0:P, :], h.t[0:P, :], ALU.add, [bank[0], bank[1], h], [acc])
                    if ti == 0:
                        dbgout("h20", acc.t[:, :], acc)

                def ple_s0(ti):
                    P = 16 if ti == 16 else 128
                    pp = pbuf[ti % 2]; acc = accs[ti % 2]
                    mset("dve", ssq.t[0:P, :], 0.0, [ssq])
                    act(junk.t[0:P, :], acc.t[0:P, :], AF.Square, [acc, ssq], [junk, ssq], accum_out=ssq.t[0:P, :])
                    act(rs.t[0:P, :], ssq.t[0:P, :], AF.Sqrt, [ssq], [rs], bias=EPS, scale=1.0 / 1024)
                    act(hbf.t[0:P, :], acc.t[0:P, :], AF.Copy, [acc], [hbf])
                    act(pbf16.t[0:P, :], pp.t[0:P, :], AF.Copy, [pp], [pbf16])
                    b3 = pbank_b(7)
                    for c in range(8):
                        tr(b3[:, c * 128:c * 128 + P], hbf.t[0:P, c * 128:(c + 1) * 128], identb.t[0:P, 0:P], [hbf, identb], [bank[7]])
                    act(hT.t[:, :, 0:P], fv(b3[:, 0:1], [(128, 8), (1, P)]), AF.Copy, [bank[7]], [hT])
                    for c in range(2):
                        tr(b3[:, c * 128:c * 128 + P], pbf16.t[0:P, c * 128:(c + 1) * 128], identb.t[0:P, 0:P], [pbf16, identb], [bank[7]])
                    act(pT.t[:, :, 0:P], fv(b3[:, 0:1], [(128, 2), (1, P)]), AF.Copy, [bank[7]], [pT])
                    for nb in range(2):
                        for c in range(8):
                            mm(pbank(2 + nb, P), hT.t[:, c, 0:P], w_gate_b.t[:, c, nb * 512:(nb + 1) * 512], c == 0, c == 7, [hT, w_gate_b], [bank[2 + nb]])
                    for nb in range(2):
                        for c in range(2):
                            mm(pbank(4 + nb, P), pT.t[:, c, 0:P], w_proj_b.t[:, c, nb * 512:(nb + 1) * 512], c == 0, c == 1, [pT, w_proj_b], [bank[4 + nb]])

                def ple_s1(ti):
                    P = 16 if ti == 16 else 128
                    recip(rs.t[0:P, :], rs.t[0:P, :], [rs], [rs])
                    act(sg.t[0:P, :], PS[1].t[0:P, :], AF.Sigmoid, [bank[2], bank[3], rs], [sg], scale=rs.t[0:P, 0:1])

                def ple_s2(ti):
                    P = 16 if ti == 16 else 128
                    acc = accs[ti % 2]
                    tt("dve", sg.t[0:P, :], sg.t[0:P, :], PS[2].t[0:P, :], ALU.mult, [sg, bank[4], bank[5]], [sg])
                    tt("dve", yt.t[0:P, :], sg.t[0:P, :], acc.t[0:P, :], ALU.add, [sg, acc], [yt])
                    if ti < 16:
                        dma("sp", y_own[ti * 128:(ti + 1) * 128, :], yt.t[:, :], [yt], [], final=True)
                    else:
                        dma("sp", y_smp, yt.t[0:16, :], [yt], [], final=True)

                def peer(ti, prev, nxt):
                    P = 16 if ti == 16 else 128
                    h = hb_[ti % 2]
                    base_ = gcnt[0]
                    gcnt[0] += NSLOT
                    stt("dve", n2b.t[0:P, :], h.t[0:P, :], rstd2_all.t[0:P, ti:ti + 1], g2bc.t[0:P, :], ALU.mult, ALU.mult, [h, rstd2_all, g2bc], [n2b])
                    mset("dve", actv.t[0:P, :], 0.0, actv_b)

                    def vphase(bq):
                        slq = slice(bq * BS, (bq + 1) * BS)
                        tt("dve", coefg.t[0:P, slq], coef.t[0:P, slq], g_all.t[0:P, ti, slq], ALU.mult, [coef_b[bq], g_all], [coefg_b[bq]])
                        for slot in range(bq * BS, (bq + 1) * BS):
                            g_ = gb[(base_ + slot) % NB]
                            d_ = dgs[slot % 4]
                            act(d_.t[0:P, 0:P], identb.t[0:P, 0:P], AF.Copy, [identb, coefg_b[bq]], [d_], scale=coefg.t[0:P, slot:slot + 1])
                            for nb in range(2):
                                mm(pbank(nb, P), d_.t[0:P, 0:P], g_.t[0:P, 1024 + nb * 512:1024 + (nb + 1) * 512], slot == 0, slot == NSLOT - 1,
                                   [d_, g_], [bank[nb]])

                    for b8 in range(NBAT):
                        sl = slice(b8 * BS, (b8 + 1) * BS)
                        for slot in range(b8 * BS, (b8 + 1) * BS):
                            g_ = gb[(base_ + slot) % NB]
                            gather(g_, ti, slot, P)
                            stt("dve", junkb.t[0:P, :], g_.t[0:P, 0:1024], 1.0, n2b.t[0:P, :], ALU.mult, ALU.mult, [g_, n2b], [junkb, actv_b[b8]],
                                accum_out=actv.t[0:P, slot:slot + 1])
                        act(coef.t[0:P, sl], actv.t[0:P, sl], AF.Gelu_apprx_tanh, [actv_b[b8]], [coef_b[b8]])
                        if b8 == 1 and prev is not None:
                            acc_add(prev)
                        if b8 >= 1:
                            vphase(b8 - 1)
                        if b8 == 1:
                            if prev is not None:
                                ple_s0(prev)
                            if nxt is not None:
                                load3(nxt)
                        if b8 == 3 and prev is not None:
                            ple_s1(prev)
                        if b8 == 5 and prev is not None:
                            ple_s2(prev)
                    vphase(NBAT - 1)

                if p3:
                    load3(p3[0])
                prev_ = None
                for i_, ti in enumerate(p3):
                    nxt_ = p3[i_ + 1] if i_ + 1 < len(p3) else None
                    peer(ti, prev_, nxt_)
                    prev_ = ti
                if p3:
                    acc_add(prev_)
                    ple_s0(prev_); ple_s1(prev_); ple_s2(prev_)

        S.emit()
    return nc


def _consts():
    k = np.arange(128)[:, None, None]
    j = np.arange(17)[None, :, None]
    q = np.arange(128)[None, None, :]
    d = (16 - j) * 128 + q - k
    m = ((d >= 0) & (d <= 128)).astype(np.float32)
    m += ((d >= 0) & (d <= 512) & (d % 4 == 0)).astype(np.float32)
    m += ((d >= 0) & (d <= 2048) & (d % 16 == 0)).astype(np.float32)
    maskT = np.ascontiguousarray(m.reshape(128, 17 * 128))
    sel = np.zeros((128, 16, 16), np.float32)
    for b in range(16):
        sel[:, b, b] = 1.0
    iota16 = np.broadcast_to(np.arange(16, dtype=np.float32), (128, 16)).copy()
    jj = np.arange(128)[:, None]
    ii = np.arange(128)[None, :]
    triT = (jj <= ii).astype(np.float32)
    return maskT, sel.reshape(128, 256), iota16, triT


def _rope_tab(pos):
    inv = 500000.0 ** (-np.arange(0, 16, 2, dtype=np.float64) / 16)
    ang = pos.astype(np.float64)[:, None] * inv[None, :]
    return np.concatenate([np.cos(ang), np.sin(ang)], axis=1).astype(np.float32)


_NC_CACHE = {}


def make_in_maps(inp):
    f = lambda a: np.ascontiguousarray(np.asarray(a, dtype=np.float32))
    xp = f(inp["x_prompt"])[0]
    xs = f(inp["x_sample"])[:, 0]
    pp = f(inp["p_prompt"])[0, 0]
    psm = f(inp["p_sample"])[0, :, 0]
    ck = f(inp["cache_k"])[0].reshape(128, 2048, 512)
    cv = f(inp["cache_v"])[0].reshape(128, 2048, 512)
    maskT, sel, iota16, triT = _consts()
    colT = lambda g: np.ascontiguousarray(f(g).reshape(-1, 128).T)
    shared = {
        "ident": np.eye(128, dtype=np.float32), "maskT": maskT, "sel": sel, "iota16": iota16, "triT": triT,
        "g1T": colT(inp["norm1_g"][0]),
        "gabT": colT(np.concatenate([f(inp["out_norm_a_g"])[0], f(inp["out_norm_b_g"])[0]])),
        "g2T": colT(inp["norm2_g"][0]), "g3T": colT(inp["norm3_g"][0]),
        "g2row": f(inp["norm2_g"]).reshape(1, 1024),
        "gqk": np.concatenate([np.tile(f(inp["q_norm_g"])[0], 8), np.tile(f(inp["k_norm_g"])[0], 8)]).reshape(1, 1024),
        "gv": f(inp["v_norm_g"]).reshape(1, 512),
        "bsT": np.ascontiguousarray(f(inp["spatial_b"])[0].T),
        "w00": np.ascontiguousarray(f(inp["spatial_w"])[0, :, 0, 0]).reshape(1, 8),
        "b0": np.ascontiguousarray(f(inp["spatial_b"])[0, :, 0]).reshape(1, 8),
        "w_in": f(inp["w_in"])[0],
        "wsT": np.ascontiguousarray(f(inp["spatial_w"])[0].transpose(2, 0, 1)).reshape(128, 1024),
        "w_out": f(inp["w_out"])[0],
        "wqT": np.ascontiguousarray(f(inp["peer_w_query"])[0].T),
        "skT": np.ascontiguousarray(f(inp["peer_sub_keys"])[0].reshape(16, 128, 128).transpose(2, 0, 1)).reshape(128, 2048),
        "peer_u": f(inp["peer_u"])[0], "peer_v": f(inp["peer_v"])[0],
        "w_gate": f(inp["ple_w_gate"])[0], "w_proj": f(inp["ple_w_proj"])[0],
    }
    in_maps = []
    for c in range(8):
        xall = np.zeros((33 * 128, 1024), np.float32)
        if c > 0:
            xall[0:2048] = xp[2048 * (c - 1):2048 * c]
        xall[2048:4096] = xp[2048 * c:2048 * (c + 1)]
        xall[4096:4096 + 16] = xs[16 * c:16 * (c + 1)]
        pall = np.zeros((17 * 128, 256), np.float32)
        pall[0:2048] = pp[2048 * c:2048 * (c + 1)]
        pall[2048:2048 + 16] = psm[16 * c:16 * (c + 1)]
        pos = np.zeros(33 * 128, np.int64)
        pos[0:2048] = np.maximum(2048 * (c - 1) + np.arange(2048), 0)
        pos[2048:4096] = 2048 * c + np.arange(2048)
        pos[4096:] = 8192
        m = dict(shared)
        m.update({"xall": xall, "pall": pall, "cs": _rope_tab(pos),
                  "flag": np.full((128, 1), 0.0 if c == 0 else 1.0, np.float32),
                  "ck": np.ascontiguousarray(ck[16 * c:16 * (c + 1)]), "cv": np.ascontiguousarray(cv[16 * c:16 * (c + 1)])})
        in_maps.append(m)
    return in_maps


def kernel(**inp):
    if "nc" not in _NC_CACHE:
        _NC_CACHE["nc"] = build()
    nc = _NC_CACHE["nc"]
    in_maps = make_in_maps(inp)
    res = run_bass_kernel_spmd(nc, in_maps, core_ids=list(range(8)))
    R = res.results
    y_prompt = np.concatenate([np.asarray(r["y_own"]) for r in R], axis=0).reshape(1, 16384, 1024)
    y_sample = np.concatenate([np.asarray(r["y_smp"]) for r in R], axis=0).reshape(128, 1, 1024)
    nkp = np.asarray(R[7]["k_own"]).reshape(1, 1, 2048, 8, 64)
    nvp = np.asarray(R[7]["v_own"]).reshape(1, 1, 2048, 8, 64)
    cat = lambda n: np.concatenate([np.asarray(r[n]) for r in R], axis=0).reshape(1, 128, 1, 8, 64)
    return (y_prompt.astype(np.float32), y_sample.astype(np.float32), nkp.astype(np.float32), nvp.astype(np.float32),
            cat("ks").astype(np.float32), cat("vs").astype(np.float32), cat("cvs").astype(np.float32))
```

```python
import numpy as np
from contextlib import ExitStack
import concourse.bass as bass
import concourse.mybir as mybir
from concourse.bass_utils import run_bass_kernel_spmd

F32 = mybir.dt.float32
BF16 = mybir.dt.bfloat16
I32 = mybir.dt.int32
U32 = mybir.dt.uint32
AF = mybir.ActivationFunctionType
ALU = mybir.AluOpType
AX = mybir.AxisListType

ENGS = ("pe", "act", "dve", "pool", "sp")
EPS = 1e-6
NSLOT = 128
CUT = 99


class Trk:
    __slots__ = ("w", "r", "dsem", "excl")

    def __init__(self):
        self.w = None
        self.r = []
        self.dsem = None
        self.excl = False


class DSem:
    __slots__ = ("sem", "n", "bg")

    def __init__(self, sem):
        self.sem = sem
        self.n = 0
        self.bg = False


class Sched:
    def __init__(self, nc, stack):
        self.nc = nc
        self.stack = stack
        self.ops = {e: [] for e in ENGS}
        self.cnt = {e: 0 for e in ENGS}
        self.sem = {e: stack.enter_context(nc.semaphore("es_" + e)) for e in ENGS}
        self.seen = {e: {} for e in ENGS}
        self.nds = 0
        self.final = []
        self.dsems = []
        self.pending = {e: [] for e in ENGS}

    def new_dsem(self):
        self.nds += 1
        d = DSem(self.stack.enter_context(self.nc.semaphore("ds%d" % self.nds)))
        self.dsems.append(d)
        return d

    def barrier(self, include_bg=False):
        evs = [(self.sem[e], self.cnt[e]) for e in ENGS if self.cnt[e] > 0]
        evs += [(d.sem, 16 * d.n) for d in self.dsems if d.n > 0 and (include_bg or not d.bg)]
        for e in ENGS:
            own = id(self.sem[e])
            for s_, v in evs:
                k = id(s_)
                if e == "pe" and k == own:
                    continue
                if self.seen[e].get(k, 0) >= v:
                    continue
                self.seen[e][k] = v
                self.pending[e].append((s_, v))

    def _deps(self, eng, reads, writes):
        need = {}

        def add(ev):
            if ev is None:
                return
            s, v = ev
            k = id(s)
            if k not in need or need[k][1] < v:
                need[k] = (s, v)
        for t in reads:
            add(t.w)
        for t in writes:
            add(t.w)
            for ev in t.r:
                add(ev)
        waits = []
        seen = self.seen[eng]
        own = id(self.sem[eng])
        for k, (s, v) in need.items():
            if eng == "pe" and k == own:
                continue
            if seen.get(k, 0) >= v:
                continue
            seen[k] = v
            waits.append((s, v))
        if self.pending[eng]:
            waits = self.pending[eng] + waits
            self.pending[eng] = []
        return waits

    def _mark(self, ev, reads, writes):
        for t in writes:
            t.w = ev
            t.r = []
        for t in reads:
            if t not in writes:
                t.r.append(ev)
                if len(t.r) > 64:
                    t.r = t.r[-48:]

    def op(self, eng, fn, reads=(), writes=()):
        ex = [t for t in reads if t.excl and t not in writes]
        if ex:
            writes = list(writes) + ex
        waits = self._deps(eng, reads, writes)
        self.cnt[eng] += 1
        ev = (self.sem[eng], self.cnt[eng])
        self.ops[eng].append((waits, fn, (self.sem[eng], 1)))
        self._mark(ev, reads, writes)
        return ev

    def dma(self, q, fn, reads=(), writes=(), dsem=None, final=False):
        if dsem is None:
            host = writes[0] if writes else reads[0]
            if host.dsem is None:
                host.dsem = self.new_dsem()
            dsem = host.dsem
        waits = self._deps(q, reads, writes)
        if dsem.n > 0:
            k = id(dsem.sem)
            v = 16 * dsem.n
            if self.seen[q].get(k, 0) < v:
                self.seen[q][k] = v
                waits.append((dsem.sem, v))
        dsem.n += 1
        ev = (dsem.sem, 16 * dsem.n)
        self.ops[q].append((waits, fn, (dsem.sem, 16)))
        self._mark(ev, reads, writes)
        if final:
            self.final.append(ev)
        return ev

    def emit(self):
        nc = self.nc
        fin = {}
        for s, v in self.final:
            k = id(s)
            if k not in fin or fin[k][1] < v:
                fin[k] = (s, v)
        finals = list(fin.values())
        with nc.Block() as block:
            def run(engname):
                def body(e):
                    for waits, fn, (s, amt) in self.ops[engname]:
                        for ws, wv in waits:
                            e.wait_ge(ws, wv)
                        fn(e).then_inc(s, amt)
                    if engname == "sp":
                        for ws, wv in finals:
                            e.wait_ge(ws, wv)
                return body
            block.tensor(run("pe"))
            block.scalar(run("act"))
            block.vector(run("dve"))
            block.gpsimd(run("pool"))
            block.sync(run("sp"))


def fv(ap, dims):
    base = ap.ap
    return bass.AP(ap.tensor, ap.offset, [list(base[0])] + [list(d) for d in dims])


class Buf:
    def __init__(self, t):
        self.t = t
        self.k = Trk()


def build(dbg=None, slots1=None, p2=None, p3=None):
    dbg = dbg or {}
    slots1 = list(range(33)) if slots1 is None else slots1
    p2 = list(range(17)) if p2 is None else p2
    p3 = list(range(17)) if p3 is None else p3
    nc = bass.Bass("TRN2", target_bir_lowering=False)

    def din(name, shape, dt=F32):
        return nc.dram_tensor(name, list(shape), dt, kind="ExternalInput").ap()

    def dout(name, shape, dt=F32):
        return nc.dram_tensor(name, list(shape), dt, kind="ExternalOutput").ap()

    xall = din("xall", [33 * 128, 1024]); pall = din("pall", [17 * 128, 256]); cs_d = din("cs", [33 * 128, 16])
    flag_d = din("flag", [128, 1]); ck = din("ck", [16, 2048, 512]); cv = din("cv", [16, 2048, 512])
    ident_d = din("ident", [128, 128]); mask_d = din("maskT", [128, 17 * 128]); sel_d = din("sel", [128, 256])
    iota_d = din("iota16", [128, 16]); tri_d = din("triT", [128, 128])
    g1T_d = din("g1T", [128, 8]); gabT_d = din("gabT", [128, 8]); g2T_d = din("g2T", [128, 8]); g3T_d = din("g3T", [128, 8])
    g2_d = din("g2row", [1, 1024]); gqk_d = din("gqk", [1, 1024]); gv_d = din("gv", [1, 512])
    bsT_d = din("bsT", [128, 8]); w00_d = din("w00", [1, 8]); b0_d = din("b0", [1, 8])
    w_in = din("w_in", [1024, 2560]); wsT_d = din("wsT", [128, 8 * 128]); w_out = din("w_out", [1024, 1024])
    wqT_d = din("wqT", [2048, 1024]); skT_d = din("skT", [128, 16 * 128])
    peer_u = din("peer_u", [16384, 1024]); peer_v = din("peer_v", [16384, 1024])
    w_gate = din("w_gate", [1024, 1024]); w_proj = din("w_proj", [256, 1024])

    y_own = dout("y_own", [2048, 1024]); y_smp = dout("y_smp", [16, 1024])
    k_own = dout("k_own", [2048, 512]); v_own = dout("v_own", [2048, 512])
    ks_o = dout("ks", [16, 512]); vs_o = dout("vs", [16, 512]); cvs_o = dout("cvs", [16, 512])
    dbg_o = {k: dout("dbg_" + k, shp) for k, shp in dbg.items()}

    h1_d = nc.dram_tensor("h1s", [17 * 128, 1024], F32, kind="Internal").ap()
    qs_d = nc.dram_tensor("qsc", [1, 16 * 512], F32, kind="Internal").ap()
    ptab = nc.dram_tensor("ptab", [16384, 2048], BF16, kind="Internal").ap()

    with ExitStack() as st0:
        S = Sched(nc, st0)

        def sbuf(st, name, shape, dt=F32):
            return Buf(st.enter_context(nc.sbuf_tensor("sb_" + name, list(shape), dt)))

        def K(bufs):
            return [b.k for b in bufs]

        def act(out, in_, func, r, w, **kw):
            S.op("act", lambda e: e.activation(out=out, in_=in_, func=func, **kw), K(r), K(w))

        def tt(eng, out, in0, in1, op, r, w):
            S.op(eng, lambda e: e.tensor_tensor(out=out, in0=in0, in1=in1, op=op), K(r), K(w))

        def ts(eng, out, in0, s1, s2, op0, op1, r, w):
            S.op(eng, lambda e: e.tensor_scalar(out=out, in0=in0, scalar1=s1, scalar2=s2, op0=op0, op1=op1), K(r), K(w))

        def tss(eng, out, in_, scalar, op, r, w):
            S.op(eng, lambda e: e.tensor_single_scalar(out=out, in_=in_, scalar=scalar, op=op), K(r), K(w))

        def stt(eng, out, in0, scalar, in1, op0, op1, r, w, accum_out=None):
            if accum_out is None:
                S.op(eng, lambda e: e.scalar_tensor_tensor(out=out, in0=in0, scalar=scalar, in1=in1, op0=op0, op1=op1), K(r), K(w))
            else:
                S.op(eng, lambda e: e.scalar_tensor_tensor(out=out, in0=in0, scalar=scalar, in1=in1, op0=op0, op1=op1, accum_out=accum_out), K(r), K(w))

        def cp(eng, out, in_, r, w):
            S.op(eng, lambda e: e.tensor_copy(out=out, in_=in_), K(r), K(w))

        def mset(eng, out, val, w):
            S.op(eng, lambda e: e.memset(out, val), [], K(w))

        def red(eng, out, in_, op, r, w):
            S.op(eng, lambda e: e.tensor_reduce(out=out, in_=in_, axis=AX.X, op=op), K(r), K(w))

        def recip(out, in_, r, w):
            S.op("dve", lambda e: e.reciprocal(out=out, in_=in_), K(r), K(w))

        def mm(out, lhsT, rhs, start, stop, r, w):
            S.op("pe", lambda e: e.matmul(out, lhsT=lhsT, rhs=rhs, start=start, stop=stop), K(r), K(w))

        def tr(out, in_, ident, r, w):
            S.op("pe", lambda e: e.transpose(out=out, in_=in_, identity=ident), K(r), K(w))

        def dma(q, out, in_, r, w, final=False, dsem=None):
            S.dma(q, lambda e: e.dma_start(out=out, in_=in_), K(r), K(w), final=final, dsem=dsem)

        def dbgout(name, ap, b):
            if name in dbg_o:
                dma("sp", dbg_o[name], ap, [b], [], final=True)

        ident = sbuf(st0, "ident", [128, 128]); identb = sbuf(st0, "identb", [128, 128], BF16)
        iota16 = sbuf(st0, "iota16", [128, 16])
        rstd2_all = sbuf(st0, "rstd2_all", [128, 17])
        dma("sp", ident.t[:], ident_d, [], [ident])
        dma("sp", iota16.t[:], iota_d, [], [iota16])
        act(identb.t[:], ident.t[:], AF.Copy, [ident], [identb])

        PS = [Buf(st0.enter_context(nc.psum_tensor("ps%d" % i, [128, 1024], F32))) for i in range(4)]
        bank = [Buf(None) for _ in range(8)]
        for b_ in bank:
            b_.k.excl = True

        def pbank(i, P=128, lo=0, hi=512):
            return PS[i // 2].t[0:P, (i % 2) * 512 + lo:(i % 2) * 512 + hi]

        def pbank_b(i):
            return PS[i // 2].t[:].bitcast(BF16)[:, (i % 2) * 1024:(i % 2) * 1024 + 1024]

        def rms_scale(st_small, src_ap, P, n, r, name):
            ssq, rs, junk = st_small
            mset("dve", ssq.t[0:P, :], 0.0, [ssq])
            act(junk.t[0:P, 0:n], src_ap, AF.Square, r + [ssq], [junk, ssq], accum_out=ssq.t[0:P, :])
            act(rs.t[0:P, :], ssq.t[0:P, :], AF.Sqrt, [ssq], [rs], bias=EPS, scale=1.0 / n)
            recip(rs.t[0:P, :], rs.t[0:P, :], [rs], [rs])
            return rs

        def load_weight(st_tmp, stage, w_dram, n_chunks, ncols, dst, gcol):
            for c in range(n_chunks):
                sb_ = stage[c % 2]
                dma("sp", sb_.t[:, 0:ncols], w_dram[c * 128:(c + 1) * 128, :], [], [sb_])
                if gcol is None:
                    act(dst.t[:, c, 0:ncols], sb_.t[:, 0:ncols], AF.Copy, [sb_], [dst])
                else:
                    act(dst.t[:, c, 0:ncols], sb_.t[:, 0:ncols], AF.Copy, [sb_, gcol], [dst], scale=gcol.t[:, c:c + 1])

        with ExitStack() as st1:
            S.barrier()
            w_in_b = sbuf(st1, "w_in_b", [128, 8, 2560], BF16)
            w_out_b = sbuf(st1, "w_out_b", [128, 8, 1024], BF16)
            wmT = sbuf(st1, "wmT", [128, 8, 128], BF16)
            KT = sbuf(st1, "KT", [128, 4, 32 * 128], BF16)
            VA = sbuf(st1, "VA", [128, 32, 8, 65], BF16)
            maskb = sbuf(st1, "maskb", [128, 17 * 128], BF16)
            selb = sbuf(st1, "selb", [128, 256], BF16)
            gqk = sbuf(st1, "gqk", [128, 1024]); gv = sbuf(st1, "gv", [128, 512])
            bsT = sbuf(st1, "bsT", [128, 8]); w00 = sbuf(st1, "w00", [128, 8]); b0 = sbuf(st1, "b0", [128, 8])
            g1T = sbuf(st1, "g1T", [128, 8]); gabT = sbuf(st1, "gabT", [128, 8]); flag = sbuf(st1, "flag", [128, 1])
            KTs = [Buf(None) for _ in range(32)]
            VAs = [Buf(None) for _ in range(32)]

            with ExitStack() as sts:
                S.barrier()
                stage = [sbuf(sts, "stage%d" % i, [128, 2560]) for i in range(2)]
                tri = sbuf(sts, "tri", [128, 128])
                for (dst, src) in ((g1T, g1T_d), (gabT, gabT_d), (flag, flag_d), (bsT, bsT_d), (tri, tri_d)):
                    dma("sp", dst.t[:], src, [], [dst])
                dma("sp", gqk.t[:], gqk_d.partition_broadcast(128), [], [gqk])
                dma("sp", gv.t[:], gv_d.partition_broadcast(128), [], [gv])
                dma("sp", w00.t[:], w00_d.partition_broadcast(128), [], [w00])
                dma("sp", b0.t[:], b0_d.partition_broadcast(128), [], [b0])
                load_weight(sts, stage, w_in, 8, 2560, w_in_b, g1T)
                load_weight(sts, stage, w_out, 8, 1024, w_out_b, gabT)
                dma("sp", stage[0].t[:, 0:17 * 128], mask_d, [], [stage[0]])
                act(maskb.t[:], stage[0].t[:, 0:17 * 128], AF.Copy, [stage[0]], [maskb])
                dma("sp", stage[1].t[:, 0:256], sel_d, [], [stage[1]])
                act(selb.t[:], stage[1].t[:, 0:256], AF.Copy, [stage[1]], [selb])
                dma("sp", stage[0].t[:, 0:1024], wsT_d, [], [stage[0]])
                tt("dve", wmT.t[:], fv(stage[0].t[:, 0:1], [(128, 8), (1, 128)]), fv(tri.t[:, 0:1], [(0, 8), (1, 128)]),
                   ALU.mult, [stage[0], tri], [wmT])
                if "wmT" in dbg_o:
                    act(stage[1].t[:, 0:1024], wmT.t[:].rearrange("p g i -> p (g i)"), AF.Copy, [wmT], [stage[1]])
                    dbgout("wmT", stage[1].t[:, 0:1024], stage[1])
                    dbgout("tri", tri.t[:, :], tri)
                mset("dve", VA.t[:, 16:32, :, 64:65], 1.0, [VA])
                cp("dve", VA.t[:, 0:16, :, 64], fv(flag.t[:, 0:1], [(0, 16), (0, 8)]), [flag], [VA])

            with ExitStack() as stt_:
                S.barrier()
                tabk = Buf(None)
                tsems = [S.new_dsem() for _ in range(16)]
                for d_ in tsems:
                    d_.bg = True
                kk_ = 0
                bg_pending = []
                for r0 in range(0, 16384, 1024):
                    for (src_, c0_) in ((peer_u, 0), (peer_v, 1024)):
                        def f_(e, src_=src_, c0_=c0_, r0=r0):
                            return e.dma_start(out=ptab[r0:r0 + 1024, c0_:c0_ + 1024], in_=src_[r0:r0 + 1024, :])
                        bg_pending.append((f_, tsems[kk_ % 16]))
                        kk_ += 1

                def issue_bg(n):
                    for _ in range(n):
                        if bg_pending:
                            f__, ds__ = bg_pending.pop(0)
                            S.dma("pool", f__, [], K([tabk]), dsem=ds__)
                issue_bg(16)
                xb = [sbuf(stt_, "xb%d" % i, [128, 1024]) for i in range(2)]
                csb = [sbuf(stt_, "csb%d" % i, [128, 16]) for i in range(2)]
                ssq = sbuf(stt_, "ssq", [128, 1]); rs = sbuf(stt_, "rs", [128, 1]); junk = sbuf(stt_, "junk", [128, 1024], BF16)
                small = (ssq, rs, junk)
                n1 = sbuf(stt_, "n1", [128, 1024], BF16); n1T = sbuf(stt_, "n1T", [128, 8, 128], BF16)
                sq = sbuf(stt_, "sq", [128, 1024]); qkn = sbuf(stt_, "qkn", [128, 1024]); qkb = sbuf(stt_, "qkb", [128, 1024], BF16)
                ssh = sbuf(stt_, "ssh", [128, 16]); rh = sbuf(stt_, "rh", [128, 16]); rt = sbuf(stt_, "rt", [128, 4, 16, 8])
                QT = sbuf(stt_, "QT", [128, 4, 256], BF16); vt = sbuf(stt_, "vt", [128, 512])
                mset("dve", QT.t[:, :, :], 0.0, [QT])
                ug = sbuf(stt_, "ug", [128, 1024]); vn = sbuf(stt_, "vn", [128, 512]); vnb = sbuf(stt_, "vnb", [128, 512], BF16)
                gm = sbuf(stt_, "gm", [128, 512]); ab = sbuf(stt_, "ab", [128, 1024], BF16); abT = sbuf(stt_, "abT", [128, 8, 128], BF16)
                eT = [sbuf(stt_, "eT%d" % i, [128, 512], BF16) for i in range(2)]
                rden = sbuf(stt_, "rden", [128, 8]); att = sbuf(stt_, "att", [128, 512]); h1t = sbuf(stt_, "h1t", [128, 1024])
                NBG = 2
                qbc = sbuf(stt_, "qbc", [128, NBG * 512])
                kcs = [sbuf(stt_, "kc%d" % i, [128, NBG * 512]) for i in range(2)]
                vcs = [sbuf(stt_, "vc%d" % i, [128, NBG * 512]) for i in range(2)]
                prod = Buf(None); prod.t = sq.t; prod.k = sq.k
                sc = sbuf(stt_, "sc", [128, NBG * 8]); evt = sbuf(stt_, "evt", [128, NBG, 8, 65], BF16)
                osb = Buf(None); osb.t = ug.t[:, 0:520].rearrange("p (a b c) -> p a b c", a=2, b=4); osb.k = ug.k
                sself = sbuf(stt_, "sself", [128, 8]); qsrc = Buf(None)

                def load_cs(slot):
                    P = 16 if slot == 32 else 128
                    dma("sp", csb[slot % 2].t[0:P, :], cs_d[slot * 128:slot * 128 + P, :], [], [csb[slot % 2]])

                def load_x(slot, with_cs=True):
                    b = slot % 2
                    P = 16 if slot == 32 else 128
                    dma("sp", xb[b].t[0:P, :], xall[slot * 128:slot * 128 + P, :], [], [xb[b]])
                    if with_cs:
                        load_cs(slot)

                def tile1(slot, nxt):
                    mode = "halo" if slot < 16 else ("own" if slot < 32 else "sample")
                    P = 16 if mode == "sample" else 128
                    x = xb[slot % 2]; c_s = csb[slot % 2]
                    if nxt is not None:
                        load_x(nxt)
                    r1 = rms_scale(small, x.t[0:P, :], P, 1024, [x], "r1")
                    act(n1.t[0:P, :], x.t[0:P, :], AF.Copy, [x, r1], [n1], scale=r1.t[0:P, 0:1])
                    b3 = pbank_b(3)
                    for c in range(8):
                        tr(b3[:, c * 128:c * 128 + P], n1.t[0:P, c * 128:(c + 1) * 128], identb.t[0:P, 0:P], [n1, identb], [bank[3]])
                    act(n1T.t[:, :, 0:P], fv(b3[:, 0:1], [(128, 8), (1, P)]), AF.Copy, [bank[3]], [n1T])
                    colblocks = [(1, 512), (2, 1024)] if mode == "halo" else [(0, 0), (1, 512), (2, 1024), (4, 1536), (5, 2048)]
                    for bk, col in colblocks:
                        for c in range(8):
                            mm(pbank(bk, P), n1T.t[:, c, 0:P], w_in_b.t[:, c, col:col + 512], c == 0, c == 7, [n1T, w_in_b], [bank[bk]])
                    zqk = PS[0].t[0:P, :]
                    act(sq.t[0:P, :], zqk, AF.Square, [bank[0], bank[1]], [sq])
                    red("dve", ssh.t[0:P, :], sq.t[0:P, :].rearrange("p (a b) -> p a b", a=16), ALU.add, [sq], [ssh])
                    act(rh.t[0:P, :], ssh.t[0:P, :], AF.Sqrt, [ssh], [rh], bias=EPS, scale=1.0 / 64)
                    recip(rh.t[0:P, :], rh.t[0:P, :], [rh], [rh])
                    q3 = qkn.t[0:P, :].rearrange("p (a b) -> p a b", a=16)
                    tt("dve", q3, zqk.rearrange("p (a b) -> p a b", a=16), fv(rh.t[0:P, 0:1], [(1, 16), (0, 64)]), ALU.mult,
                       [bank[0], bank[1], rh], [qkn])
                    tt("dve", qkn.t[0:P, :], qkn.t[0:P, :], gqk.t[0:P, :], ALU.mult, [qkn, gqk], [qkn])
                    x1 = q3[:, :, 0:8]; x2 = q3[:, :, 8:16]
                    cosb = fv(c_s.t[0:P, 0:1], [(0, 16), (1, 8)]); sinb = fv(c_s.t[0:P, 8:9], [(0, 16), (1, 8)])
                    tt("dve", rt.t[0:P, 0], x1, cosb, ALU.mult, [qkn, c_s], [rt])
                    tt("dve", rt.t[0:P, 1], x2, sinb, ALU.mult, [qkn, c_s], [rt])
                    tt("dve", rt.t[0:P, 2], x2, cosb, ALU.mult, [qkn, c_s], [rt])
                    tt("dve", rt.t[0:P, 3], x1, sinb, ALU.mult, [qkn, c_s], [rt])
                    tt("dve", x1, rt.t[0:P, 0], rt.t[0:P, 1], ALU.subtract, [rt], [qkn])
                    tt("dve", x2, rt.t[0:P, 2], rt.t[0:P, 3], ALU.add, [rt], [qkn])
                    act(qkb.t[0:P, :], qkn.t[0:P, :], AF.Copy, [qkn], [qkb])
                    if CUT <= -2:
                        return
                    if mode == "own":
                        o0 = (slot - 16) * 128
                        dma("sp", k_own[o0:o0 + 128, :], qkn.t[:, 512:1024], [qkn], [], final=True)
                        if CUT <= -1:
                            return
                    elif mode == "sample":
                        dma("sp", ks_o, qkn.t[0:16, 512:1024], [qkn], [], final=True)
                    crange = range(4, 8) if mode == "halo" else range(8)
                    for c in crange:
                        tr(b3[:, c * 128:c * 128 + P], qkb.t[0:P, c * 128:(c + 1) * 128], identb.t[0:P, 0:P], [qkb, identb], [bank[3]])
                    if mode == "own":
                        act(QT.t[0:64, :, 0:128], fv(b3[0:64, 0:1], [(128, 4), (1, 128)]), AF.Copy, [bank[3]], [QT])
                        act(QT.t[64:128, :, 128:256], fv(b3[64:128, 0:1], [(128, 4), (1, 128)]), AF.Copy, [bank[3]], [QT])
                    if mode != "sample":
                        cp("dve", KT.t[:, :, slot * 128:(slot + 1) * 128], fv(b3[:, 512:513], [(128, 4), (1, 128)]), [bank[3]], [KTs[slot]])
                    if CUT <= 0 and mode == "own":
                        return
                    if mode != "halo":
                        act(vt.t[0:P, :], pbank(2, P), AF.Copy, [bank[2]], [vt])
                        if mode == "own":
                            dma("sp", v_own[o0:o0 + 128, :], vt.t[:, :], [vt], [], final=True)
                        else:
                            dma("sp", vs_o, vt.t[0:16, :], [vt], [], final=True)
                    if mode != "sample":
                        cp("dve", VA.t[:, slot, :, 0:64], pbank(2, P).rearrange("p (h c) -> p h c", h=8), [bank[2]], [VAs[slot]])
                    if mode == "halo" or CUT <= 1:
                        return
                    act(ug.t[0:P, :], PS[2].t[0:P, :], AF.Gelu_apprx_tanh, [bank[4], bank[5]], [ug])
                    act(sq.t[0:P, 0:512], ug.t[0:P, 512:1024], AF.Square, [ug], [sq])
                    red("dve", ssh.t[0:P, 0:8], sq.t[0:P, 0:512].rearrange("p (a b) -> p a b", a=8), ALU.add, [sq], [ssh])
                    act(rh.t[0:P, 0:8], ssh.t[0:P, 0:8], AF.Sqrt, [ssh], [rh], bias=EPS, scale=1.0 / 64)
                    recip(rh.t[0:P, 0:8], rh.t[0:P, 0:8], [rh], [rh])
                    vn3 = vn.t[0:P, :].rearrange("p (a b) -> p a b", a=8)
                    tt("dve", vn3, ug.t[0:P, 512:1024].rearrange("p (a b) -> p a b", a=8), fv(rh.t[0:P, 0:1], [(1, 8), (0, 64)]),
                       ALU.mult, [ug, rh], [vn])
                    tt("dve", vn.t[0:P, :], vn.t[0:P, :], gv.t[0:P, :], ALU.mult, [vn, gv], [vn])
                    gm3 = gm.t[0:P, :].rearrange("p (a b) -> p a b", a=8)
                    if mode == "own":
                        act(vnb.t[0:P, :], vn.t[0:P, :], AF.Copy, [vn], [vnb])
                        for g in range(8):
                            mm(pbank(2, P, g * 64, (g + 1) * 64), wmT.t[:, g, 0:P], vnb.t[:, g * 64:(g + 1) * 64], True, True, [wmT, vnb], [bank[2]])
                        tt("dve", gm3, pbank(2, P).rearrange("p (a b) -> p a b", a=8), fv(bsT.t[0:P, 0:1], [(1, 8), (0, 64)]), ALU.add,
                           [bank[2], bsT], [gm])
                    else:
                        dma("sp", cvs_o, vn.t[0:16, :], [vn], [], final=True)
                        tt("dve", gm3, vn3, fv(w00.t[0:P, 0:1], [(1, 8), (0, 64)]), ALU.mult, [vn, w00], [gm])
                        tt("dve", gm3, gm3, fv(b0.t[0:P, 0:1], [(1, 8), (0, 64)]), ALU.add, [gm, b0], [gm])
                    if slot == 16:
                        dbgout("ug0", ug.t[:, :], ug)
                        dbgout("vn0", vn.t[:, :], vn)
                        dbgout("mix0", gm.t[:, :], gm)
                    tt("dve", gm.t[0:P, :], gm.t[0:P, :], ug.t[0:P, 0:512], ALU.mult, [gm, ug], [gm])
                    rb = rms_scale(small, gm.t[0:P, :], P, 512, [gm], "rb")
                    act(ab.t[0:P, 512:1024], gm.t[0:P, :], AF.Copy, [gm, rb], [ab], scale=rb.t[0:P, 0:1])
                    if CUT <= 2:
                        return
                    if mode == "own":
                        issue_bg(1)
                        i0 = slot - 16
                        groups = [(hp, grp) for hp in range(4) for grp in range(9)]

                        def a_scores(gi):
                            hp, grp = groups[gi]
                            j0 = grp * 2
                            nj = min(2, 17 - j0)
                            sb_i = gi % 2
                            for jj in range(nj):
                                s_ = i0 + j0 + jj
                                mm(pbank(sb_i, 128, jj * 256, (jj + 1) * 256), KT.t[:, hp, s_ * 128:(s_ + 1) * 128],
                                   QT.t[:, hp, :], True, True, [KTs[s_], QT], [bank[sb_i]])

                        def a_soft(gi):
                            hp, grp = groups[gi]
                            j0 = grp * 2
                            nj = min(2, 17 - j0)
                            sb_i = gi % 2
                            et = eT[sb_i]
                            act(et.t[:, 0:nj * 256], pbank(sb_i, 128, 0, nj * 256), AF.Exp, [bank[sb_i]], [et], scale=0.125)
                            tt("pool", et.t[:, 0:nj * 256].rearrange("p (j e q) -> p j e q", j=nj, e=2),
                               et.t[:, 0:nj * 256].rearrange("p (j e q) -> p j e q", j=nj, e=2),
                               fv(maskb.t[:, j0 * 128:j0 * 128 + 1], [(128, nj), (0, 2), (1, 128)]), ALU.mult, [et, maskb], [et])

                        def a_pv(gi):
                            hp, grp = groups[gi]
                            j0 = grp * 2
                            nj = min(2, 17 - j0)
                            et = eT[gi % 2]
                            for jj in range(nj):
                                s_ = i0 + j0 + jj
                                j = j0 + jj
                                for e_ in range(2):
                                    h = 2 * hp + e_
                                    ob = 6 + e_
                                    o_ap = pbank(ob, 128, hp * 65, hp * 65 + 65)
                                    c0 = jj * 256 + e_ * 128
                                    mm(o_ap, et.t[:, c0:c0 + 128], VA.t[:, s_, h, :], j == 0, j == 16, [et, VAs[s_], VA], [bank[ob]])

                        a_scores(0)
                        for gi in range(len(groups)):
                            if gi + 1 < len(groups):
                                a_scores(gi + 1)
                            a_soft(gi)
                            a_pv(gi)
                        o_src = fv(PS[3].t[0:128, 0:1], [(512, 2), (65, 4), (1, 65)])
                        o_r = [bank[6], bank[7]]
                        att_v = fv(att.t[0:P, 0:1], [(64, 2), (128, 4), (1, 64)])
                        rden_v = fv(rden.t[0:P, 0:1], [(1, 2), (2, 4)])
                        rden_b = fv(rden.t[0:P, 0:1], [(1, 2), (2, 4), (0, 64)])
                    else:
                        dma("sp", qs_d.rearrange("o (b f) -> (o b) f", b=16), qkn.t[0:16, 0:512], [qkn], [qsrc])
                        first = True
                        for bg in range(16 // NBG):
                            dma("sp", qbc.t[:, :], qs_d[0:1, bg * NBG * 512:(bg + 1) * NBG * 512].partition_broadcast(128), [qsrc], [qbc])
                            for pi, dil in enumerate((1, 4, 16)):
                                st_ = 2048 - 128 * dil
                                kc = kcs[(bg * 3 + pi) % 2]; vc = vcs[(bg * 3 + pi) % 2]
                                dma("sp", kc.t[:, :].rearrange("p (b f) -> p b f", b=NBG),
                                    ck[bg * NBG:(bg + 1) * NBG, st_:2048:dil, :].rearrange("b k f -> k b f"), [], [kc])
                                dma("sp", vc.t[:, :].rearrange("p (b f) -> p b f", b=NBG),
                                    cv[bg * NBG:(bg + 1) * NBG, st_:2048:dil, :].rearrange("b k f -> k b f"), [], [vc])
                                tt("pool", prod.t[:, :], kc.t[:, :], qbc.t[:, :], ALU.mult, [kc, qbc], [prod])
                                red("dve", sc.t[:, :], prod.t[:, :].rearrange("p (a b) -> p a b", a=NBG * 8), ALU.add, [prod], [sc])
                                act(sc.t[:, :], sc.t[:, :], AF.Exp, [sc], [sc], scale=0.125)
                                tt("dve", evt.t[:, :, :, 0:64], vc.t[:, :].rearrange("p (b h c) -> p b h c", b=NBG, h=8),
                                   fv(sc.t[:, 0:1], [(8, NBG), (1, 8), (0, 64)]), ALU.mult, [vc, sc], [evt])
                                cp("dve", evt.t[:, :, :, 64], sc.t[:, :].rearrange("p (b h) -> p b h", b=NBG), [sc], [evt])
                                for bb in range(NBG):
                                    bglob = bg * NBG + bb
                                    last = (bg == 16 // NBG - 1 and pi == 2 and bb == NBG - 1)
                                    for half in range(2):
                                        mm(pbank(6 + half, 16, 0, 260), selb.t[:, bglob * 16:(bglob + 1) * 16],
                                           evt.t[:, bb, half * 4:(half + 1) * 4, :].rearrange("p h c -> p (h c)"), first, last, [selb, evt], [bank[6 + half]])
                                    first = False
                        cp("dve", osb.t[0:16], fv(PS[3].t[0:16, 0:1], [(512, 2), (65, 4), (1, 65)]), [bank[6], bank[7]], [osb])
                        tt("dve", prod.t[0:16, 0:512], qkn.t[0:16, 0:512], qkn.t[0:16, 512:1024], ALU.mult, [qkn], [prod])
                        red("dve", sself.t[0:16, :], prod.t[0:16, 0:512].rearrange("p (a b) -> p a b", a=8), ALU.add, [prod], [sself])
                        act(sself.t[0:16, :], sself.t[0:16, :], AF.Exp, [sself], [sself], scale=0.125)
                        tss("dve", sself.t[0:16, :], sself.t[0:16, :], 3.0, ALU.mult, [sself], [sself])
                        o4 = osb.t[0:16].rearrange("p a b c -> p (a b) c")
                        tt("dve", prod.t[0:16, 0:512].rearrange("p (h c) -> p h c", h=8), vt.t[0:16, :].rearrange("p (h c) -> p h c", h=8),
                           fv(sself.t[0:16, 0:1], [(1, 8), (0, 64)]), ALU.mult, [vt, sself], [prod])
                        tt("dve", o4[:, :, 0:64], o4[:, :, 0:64], prod.t[0:16, 0:512].rearrange("p (h c) -> p h c", h=8), ALU.add, [osb, prod], [osb])
                        tt("dve", o4[:, :, 64], o4[:, :, 64], sself.t[0:16, :], ALU.add, [osb, sself], [osb])
                        o_src = osb.t[0:16]
                        o_r = [osb]
                        att_v = att.t[0:P, :].rearrange("p (a b c) -> p a b c", a=2, b=4)
                        rden_v = rden.t[0:P, :].rearrange("p (a b) -> p a b", a=2)
                        rden_b = fv(rden.t[0:P, 0:1], [(4, 2), (1, 4), (0, 64)])
                    if CUT <= 3:
                        return
                    recip(rden_v, o_src[:, :, :, 64], o_r, [rden])
                    tt("dve", att_v, o_src[:, :, :, 0:64], rden_b, ALU.mult, o_r + [rden], [att])
                    ra = rms_scale(small, att.t[0:P, :], P, 512, [att], "ra")
                    act(ab.t[0:P, 0:512], att.t[0:P, :], AF.Copy, [att, ra], [ab], scale=ra.t[0:P, 0:1])
                    for c in range(8):
                        tr(b3[:, c * 128:c * 128 + P], ab.t[0:P, c * 128:(c + 1) * 128], identb.t[0:P, 0:P], [ab, identb], [bank[3]])
                    act(abT.t[:, :, 0:P], fv(b3[:, 0:1], [(128, 8), (1, P)]), AF.Copy, [bank[3]], [abT])
                    for nb in range(2):
                        for c in range(8):
                            mm(pbank(4 + nb, P), abT.t[:, c, 0:P], w_out_b.t[:, c, nb * 512:(nb + 1) * 512], c == 0, c == 7, [abT, w_out_b], [bank[4 + nb]])
                    tt("dve", h1t.t[0:P, :], PS[2].t[0:P, :], x.t[0:P, :], ALU.add, [bank[4], bank[5], x], [h1t])
                    ti = slot - 16
                    dma("sp", h1_d[ti * 128:ti * 128 + P, :], h1t.t[0:P, :], [h1t], [h1s[ti]], dsem=h1sems[ti % 2])
                    if ti == 0:
                        dbgout("att0", att.t[:, :], att)
                        dbgout("gm0", gm.t[:, :], gm)
                        dbgout("h10", h1t.t[:, :], h1t)
                    if mode == "sample":
                        dbgout("atts", att.t[:, :], att)
                        dbgout("h1s_", h1t.t[:, :], h1t)

                def halo_bufs(slot):
                    return (n1, n1T) if slot % 2 == 0 else (ab, abT)

                def halo_front(slot, nxt):
                    x = xb[slot % 2]
                    if nxt is not None:
                        load_x(nxt, with_cs=False)
                    n1_, n1T_ = halo_bufs(slot)
                    r1 = rms_scale(small, x.t[:, :], 128, 1024, [x], "r1")
                    act(n1_.t[:, :], x.t[:, :], AF.Copy, [x, r1], [n1_], scale=r1.t[:, 0:1])
                    b3 = pbank_b(3)
                    for c in range(8):
                        tr(b3[:, c * 128:(c + 1) * 128], n1_.t[:, c * 128:(c + 1) * 128], identb.t[:, :], [n1_, identb], [bank[3]])
                    act(n1T_.t[:, :, :], fv(b3[:, 0:1], [(128, 8), (1, 128)]), AF.Copy, [bank[3]], [n1T_])

                def halo_back(slot):
                    c_s = csb[slot % 2]
                    n1_, n1T_ = halo_bufs(slot)
                    b3 = pbank_b(3)
                    for bk, col in ((1, 512), (2, 1024)):
                        for c in range(8):
                            mm(pbank(bk), n1T_.t[:, c, :], w_in_b.t[:, c, col:col + 512], c == 0, c == 7, [n1T_, w_in_b], [bank[bk]])
                    zk = pbank(1)
                    act(sq.t[:, 0:512], zk, AF.Square, [bank[1]], [sq])
                    red("dve", ssh.t[:, 0:8], sq.t[:, 0:512].rearrange("p (a b) -> p a b", a=8), ALU.add, [sq], [ssh])
                    act(rh.t[:, 0:8], ssh.t[:, 0:8], AF.Sqrt, [ssh], [rh], bias=EPS, scale=1.0 / 64)
                    recip(rh.t[:, 0:8], rh.t[:, 0:8], [rh], [rh])
                    k3 = qkn.t[:, 512:1024].rearrange("p (a b) -> p a b", a=8)
                    tt("dve", k3, zk.rearrange("p (a b) -> p a b", a=8), fv(rh.t[:, 0:1], [(1, 8), (0, 64)]), ALU.mult, [bank[1], rh], [qkn])
                    tt("dve", qkn.t[:, 512:1024], qkn.t[:, 512:1024], gqk.t[:, 512:1024], ALU.mult, [qkn, gqk], [qkn])
                    x1 = k3[:, :, 0:8]; x2 = k3[:, :, 8:16]
                    cosb = fv(c_s.t[:, 0:1], [(0, 8), (1, 8)]); sinb = fv(c_s.t[:, 8:9], [(0, 8), (1, 8)])
                    tt("dve", rt.t[:, 0, 0:8], x1, cosb, ALU.mult, [qkn, c_s], [rt])
                    tt("dve", rt.t[:, 1, 0:8], x2, sinb, ALU.mult, [qkn, c_s], [rt])
                    tt("dve", rt.t[:, 2, 0:8], x2, cosb, ALU.mult, [qkn, c_s], [rt])
                    tt("dve", rt.t[:, 3, 0:8], x1, sinb, ALU.mult, [qkn, c_s], [rt])
                    tt("dve", x1, rt.t[:, 0, 0:8], rt.t[:, 1, 0:8], ALU.subtract, [rt], [qkn])
                    tt("dve", x2, rt.t[:, 2, 0:8], rt.t[:, 3, 0:8], ALU.add, [rt], [qkn])
                    act(qkb.t[:, 512:1024], qkn.t[:, 512:1024], AF.Copy, [qkn], [qkb])
                    for c in range(4, 8):
                        tr(b3[:, c * 128:(c + 1) * 128], qkb.t[:, c * 128:(c + 1) * 128], identb.t[:, :], [qkb, identb], [bank[3]])
                    cp("dve", KT.t[:, :, slot * 128:(slot + 1) * 128], fv(b3[:, 512:513], [(128, 4), (1, 128)]), [bank[3]], [KTs[slot]])
                    cp("dve", VA.t[:, slot, :, 0:64], pbank(2).rearrange("p (h c) -> p h c", h=8), [bank[2]], [VAs[slot]])

                h1s = [Buf(None) for _ in range(17)]
                h1sems = [S.new_dsem() for _ in range(2)]
                if slots1 == list(range(33)):
                    load_x(0)
                    load_cs(1)
                    halo_front(0, 1)
                    for s_ in range(16):
                        if s_ + 1 < 16:
                            halo_front(s_ + 1, s_ + 2)
                        halo_back(s_)
                        if s_ + 2 <= 16:
                            load_cs(s_ + 2)
                    for slot in range(16, 33):
                        tile1(slot, slot + 1 if slot + 1 <= 32 else None)
                else:
                    if slots1:
                        load_x(slots1[0])
                    for si, slot in enumerate(slots1):
                        tile1(slot, slots1[si + 1] if si + 1 < len(slots1) else None)
                issue_bg(len(bg_pending))

        eidx_all = sbuf(st0, "eidx_all", [128, 17, NSLOT], I32)
        g_all = sbuf(st0, "g_all", [128, 17, NSLOT])
        w_gate_b = sbuf(st0, "w_gate_b", [128, 8, 1024], BF16)
        w_proj_b = sbuf(st0, "w_proj_b", [128, 2, 1024], BF16)
        g2bc = sbuf(st0, "g2bc", [128, 1024]); g3T = sbuf(st0, "g3T", [128, 8])
        with ExitStack() as st2:
            S.barrier()
            Wqk = sbuf(st2, "Wqk", [128, 8, 2048], BF16)
            Wqk_e = Buf(None); Wqk_e.t = Wqk.t
            Wqk_o = Buf(None); Wqk_o.t = Wqk.t
            g2T = sbuf(st2, "g2T", [128, 8])
            dma("sp", g2T.t[:], g2T_d, [], [g2T])
            with ExitStack() as sts:
                S.barrier()
                skT = sbuf(sts, "skT", [128, 2048])
                wq = [sbuf(sts, "wq%d" % i, [128, 1024]) for i in range(2)]
                dma("sp", skT.t[:], skT_d, [], [skT])
                for b in range(16):
                    w_ = wq[b % 2]
                    dma("sp", w_.t[:], wqT_d[b * 128:(b + 1) * 128, :], [], [w_])
                    for dc in range(8):
                        pb_i = 2 * (b % 2) + dc % 2
                        mm(pbank(pb_i, 128, (dc // 2) * 128, (dc // 2) * 128 + 128), w_.t[:, dc * 128:(dc + 1) * 128], skT.t[:, b * 128:(b + 1) * 128],
                           True, True, [w_, skT], [bank[pb_i]])
                    for dc in range(8):
                        pb_i = 2 * (b % 2) + dc % 2
                        if dc % 2 == 0:
                            act(Wqk.t[:, dc, b * 128:(b + 1) * 128], pbank(pb_i, 128, (dc // 2) * 128, (dc // 2) * 128 + 128), AF.Copy,
                                [bank[pb_i], g2T], [Wqk_e], scale=g2T.t[:, dc:dc + 1])
                        else:
                            tss("dve", Wqk.t[:, dc, b * 128:(b + 1) * 128], pbank(pb_i, 128, (dc // 2) * 128, (dc // 2) * 128 + 128),
                                g2T.t[:, dc:dc + 1], ALU.mult, [bank[pb_i], g2T], [Wqk_o])
            with ExitStack() as stt_:
                S.barrier()
                stg3 = [sbuf(stt_, "stg3_%d" % i, [128, 1024]) for i in range(2)]
                hb_ = [sbuf(stt_, "h1b%d" % i, [128, 1024]) for i in range(2)]
                ssq = sbuf(stt_, "ssq2", [128, 1]); rs = sbuf(stt_, "rs2", [128, 1]); junk = sbuf(stt_, "junk2", [128, 1024], BF16)
                hbf = sbuf(stt_, "hbf", [128, 1024], BF16); hT = sbuf(stt_, "hT", [128, 8, 128], BF16)
                ssbs = [sbuf(stt_, "ssb%d" % i, [128, 2048]) for i in range(2)]; s2 = sbuf(stt_, "s2b", [128, 2048])
                tops = sbuf(stt_, "tops", [128, 16, 16]); topi = sbuf(stt_, "topi", [128, 16, 16], U32); topf = sbuf(stt_, "topf", [128, 16, 16])
                cand = sbuf(stt_, "cand", [128, 8, 256]); cand2 = sbuf(stt_, "cand2", [128, 8, 256])
                best = sbuf(stt_, "best", [128, 8, 16]); pos = sbuf(stt_, "pos", [128, 8, 16], U32)
                pa = sbuf(stt_, "pa", [128, 8, 16], I32); pb_ = sbuf(stt_, "pb", [128, 8, 16], I32)
                paf = sbuf(stt_, "paf", [128, 8, 16]); pbf = sbuf(stt_, "pbf", [128, 8, 16])
                oh = sbuf(stt_, "oh", [128, 8, 16, 16]); e0 = sbuf(stt_, "e0", [128, 8, 16]); e1 = sbuf(stt_, "e1", [128, 8, 16])
                mx = sbuf(stt_, "mx", [128, 8]); sm = sbuf(stt_, "sm", [128, 8]); gex = sbuf(stt_, "gex", [128, 8, 16]); gsum = sbuf(stt_, "gsum", [128, 8, 16])

                def load_h(ti, bufs):
                    P = 16 if ti == 16 else 128
                    dma("sp", bufs[ti % 2].t[0:P, :], h1_d[ti * 128:ti * 128 + P, :], [h1s[ti]], [bufs[ti % 2]])

                def front2(ti, nxt):
                    P = 16 if ti == 16 else 128
                    h = hb_[ti % 2]
                    ssb = ssbs[ti % 2]
                    if nxt is not None:
                        load_h(nxt, hb_)
                    r2 = rms_scale((ssq, rs, junk), h.t[0:P, :], P, 1024, [h], "r2")
                    cp("dve", rstd2_all.t[0:P, ti:ti + 1], r2.t[0:P, :], [r2], [rstd2_all])
                    act(hbf.t[0:P, :], h.t[0:P, :], AF.Copy, [h], [hbf])
                    b3 = pbank_b(7)
                    for c in range(8):
                        tr(b3[:, c * 128:c * 128 + P], hbf.t[0:P, c * 128:(c + 1) * 128], identb.t[0:P, 0:P], [hbf, identb], [bank[7]])
                    act(hT.t[:, :, 0:P], fv(b3[:, 0:1], [(128, 8), (1, P)]), AF.Copy, [bank[7]], [hT])
                    for nb in range(4):
                        for c in range(8):
                            mm(pbank(nb, P), hT.t[:, c, 0:P], Wqk.t[:, c, nb * 512:(nb + 1) * 512], c == 0, c == 7, [hT, Wqk_e, Wqk_o], [bank[nb]])
                    for half in range(2):
                        act(ssb.t[0:P, half * 1024:(half + 1) * 1024], PS[half].t[0:P, :], AF.Copy, [bank[2 * half], bank[2 * half + 1], r2], [ssb],
                            scale=r2.t[0:P, 0:1])

                def back2(ti):
                    P = 16 if ti == 16 else 128
                    ssb = ssbs[ti % 2]

                    def top16(src, scr, g, n, vals, idxs):
                        sv = src.t[0:P, g * n:(g + 1) * n] if len(src.t.shape) == 2 else src.t[0:P, g, :]
                        s2v = scr.t[0:P, g * n:(g + 1) * n] if len(scr.t.shape) == 2 else scr.t[0:P, g, :]
                        v0 = vals.t[0:P, g, 0:8]; v1 = vals.t[0:P, g, 8:16]
                        S.op("dve", lambda e: e.max(out=v0, in_=sv), K([src]), K([vals]))
                        S.op("dve", lambda e: e.max_index(out=idxs.t[0:P, g, 0:8], in_max=v0, in_values=sv), K([src, vals]), K([idxs]))
                        S.op("dve", lambda e: e.match_replace(out=s2v, in_to_replace=v0, in_values=sv, imm_value=-1e30), K([src, vals]), K([scr]))
                        S.op("dve", lambda e: e.max(out=v1, in_=s2v), K([scr]), K([vals]))
                        S.op("dve", lambda e: e.max_index(out=idxs.t[0:P, g, 8:16], in_max=v1, in_values=s2v), K([scr, vals]), K([idxs]))

                    for g in range(16):
                        top16(ssb, s2, g, 128, tops, topi)
                    cp("dve", topf.t[0:P], topi.t[0:P], [topi], [topf])
                    c4 = cand.t[0:P].rearrange("p h (a b) -> p h a b", a=16)
                    tt("dve", c4, fv(tops.t[0:P, 0, 0:1], [(32, 8), (1, 16), (0, 16)]), fv(tops.t[0:P, 1, 0:1], [(32, 8), (0, 16), (1, 16)]),
                       ALU.add, [tops], [cand])
                    for hh in range(8):
                        top16(cand, cand2, hh, 256, best, pos)
                    tss("dve", pa.t[0:P], pos.t[0:P].bitcast(I32), 4, ALU.arith_shift_right, [pos], [pa])
                    tss("dve", pb_.t[0:P], pos.t[0:P].bitcast(I32), 15, ALU.bitwise_and, [pos], [pb_])
                    cp("dve", paf.t[0:P], pa.t[0:P], [pa], [paf])
                    cp("dve", pbf.t[0:P], pb_.t[0:P], [pb_], [pbf])
                    iob = fv(iota16.t[0:P, 0:1], [(0, 8), (0, 16), (1, 16)])
                    for (pf, pidx, eo) in ((paf, 0, e0), (pbf, 1, e1)):
                        tt("dve", oh.t[0:P], iob, fv(pf.t[0:P, 0, 0:1], [(16, 8), (1, 16), (0, 16)]), ALU.is_equal, [iota16, pf], [oh])
                        tt("dve", oh.t[0:P], oh.t[0:P], fv(topf.t[0:P, pidx, 0:1], [(32, 8), (0, 16), (1, 16)]), ALU.mult, [oh, topf], [oh])
                        red("dve", eo.t[0:P], oh.t[0:P], ALU.add, [oh], [eo])
                    stt("dve", e0.t[0:P], e0.t[0:P], 128.0, e1.t[0:P], ALU.mult, ALU.add, [e0, e1], [e0])
                    cp("dve", eidx_all.t[0:P, ti, :], e0.t[0:P].rearrange("p h k -> p (h k)"), [e0], [eidx_all])
                    red("dve", mx.t[0:P], best.t[0:P], ALU.max, [best], [mx])
                    tt("dve", best.t[0:P], best.t[0:P], fv(mx.t[0:P, 0:1], [(1, 8), (0, 16)]), ALU.subtract, [best, mx], [best])
                    act(best.t[0:P], best.t[0:P], AF.Exp, [best], [best])
                    red("dve", sm.t[0:P], best.t[0:P], ALU.add, [best], [sm])
                    recip(sm.t[0:P], sm.t[0:P], [sm], [sm])
                    tt("dve", g_all.t[0:P, ti, :].rearrange("p (h k) -> p h k", h=8), best.t[0:P], fv(sm.t[0:P, 0:1], [(1, 8), (0, 16)]),
                       ALU.mult, [best, sm], [g_all])
                    if ti == 0:
                        dbgout("s0", ssb.t[:, :], ssb)
                        dbgout("eidx0", e0.t[:].rearrange("p h k -> p (h k)"), e0)
                        dbgout("g0", g_all.t[:, 0, :], g_all)

                if p2:
                    load_h(p2[0], hb_)
                    front2(p2[0], p2[1] if len(p2) > 1 else None)
                dma("sp", g3T.t[:], g3T_d, [], [g3T])
                dma("sp", g2bc.t[:], g2_d.partition_broadcast(128), [], [g2bc])
                load_weight(stt_, stg3, w_gate, 8, 1024, w_gate_b, g3T)
                load_weight(stt_, stg3, w_proj, 2, 1024, w_proj_b, None)
                for i_, ti in enumerate(p2):
                    if i_ + 1 < len(p2):
                        front2(p2[i_ + 1], p2[i_ + 2] if i_ + 2 < len(p2) else None)
                    back2(ti)

        with ExitStack() as st3:
            S.barrier(include_bg=True)
            with ExitStack() as stt_:
                S.barrier()
                NB = 24
                hb_ = [sbuf(stt_, "h3b%d" % i, [128, 1024]) for i in range(2)]
                pbuf = [sbuf(stt_, "pbuf%d" % i, [128, 256]) for i in range(2)]
                gb = [sbuf(stt_, "gb%d" % i, [128, 2048], BF16) for i in range(NB)]
                dgs = [sbuf(stt_, "dg%d" % i, [128, 128], BF16) for i in range(4)]
                n2b = sbuf(stt_, "n2b", [128, 1024], BF16); junkb = sbuf(stt_, "junkb", [128, 1024], BF16)
                accs = [sbuf(stt_, "acc%d" % i, [128, 1024]) for i in range(2)]
                actv = sbuf(stt_, "actv", [128, NSLOT]); coef = sbuf(stt_, "coef", [128, NSLOT]); coefg = sbuf(stt_, "coefg", [128, NSLOT])
                ssq = sbuf(stt_, "ssq3", [128, 1]); rs = sbuf(stt_, "rs3", [128, 1]); junk = sbuf(stt_, "junk3", [128, 1024], BF16)
                hbf = sbuf(stt_, "hbf3", [128, 1024], BF16); hT = sbuf(stt_, "hT3", [128, 8, 128], BF16)
                pbf16 = sbuf(stt_, "pbf16", [128, 256], BF16); pT = sbuf(stt_, "pT", [128, 2, 128], BF16)
                sg = sbuf(stt_, "sg", [128, 1024]); yt = sbuf(stt_, "yt", [128, 1024])

                def load3(ti):
                    P = 16 if ti == 16 else 128
                    dma("sp", hb_[ti % 2].t[0:P, :], h1_d[ti * 128:ti * 128 + P, :], [h1s[ti]], [hb_[ti % 2]])
                    dma("sp", pbuf[ti % 2].t[0:P, :], pall[ti * 128:ti * 128 + P, :], [], [pbuf[ti % 2]])

                def gather(dstb, ti, slot, P):
                    off = eidx_all.t[0:P, ti, slot:slot + 1]
                    S.dma("pool", lambda e: e.indirect_dma_start(out=dstb.t[0:P, :], out_offset=None, in_=ptab,
                                                                in_offset=bass.IndirectOffsetOnAxis(ap=off, axis=0)),
                          K([eidx_all]), K([dstb]))

                BS = 8
                NBAT = NSLOT // BS

                def colbufs(buf):
                    out = []
                    for _ in range(NBAT):
                        b_ = Buf(None); b_.t = buf.t
                        out.append(b_)
                    return out
                actv_b = colbufs(actv); coef_b = colbufs(coef); coefg_b = colbufs(coefg)

                gcnt = [0]

                def acc_add(ti):
                    P = 16 if ti == 16 else 128
                    h = hb_[ti % 2]; acc = accs[ti % 2]
                    tt("dve", acc.t[0:P, :], PS[0].t[0:P, :], h.t[0:P, :], ALU.add, [bank[0], bank[1], h], [acc])
                    if ti == 0:
                        dbgout("h20", acc.t[:, :], acc)

                def ple_s0(ti):
                    P = 16 if ti == 16 else 128
                    pp = pbuf[ti % 2]; acc = accs[ti % 2]
                    mset("dve", ssq.t[0:P, :], 0.0, [ssq])
                    act(junk.t[0:P, :], acc.t[0:P, :], AF.Square, [acc, ssq], [junk, ssq], accum_out=ssq.t[0:P, :])
                    act(rs.t[0:P, :], ssq.t[0:P, :], AF.Sqrt, [ssq], [rs], bias=EPS, scale=1.0 / 1024)
                    act(hbf.t[0:P, :], acc.t[0:P, :], AF.Copy, [acc], [hbf])
                    act(pbf16.t[0:P, :], pp.t[0:P, :], AF.Copy, [pp], [pbf16])
                    b3 = pbank_b(7)
                    for c in range(8):
                        tr(b3[:, c * 128:c * 128 + P], hbf.t[0:P, c * 128:(c + 1) * 128], identb.t[0:P, 0:P], [hbf, identb], [bank[7]])
                    act(hT.t[:, :, 0:P], fv(b3[:, 0:1], [(128, 8), (1, P)]), AF.Copy, [bank[7]], [hT])
                    for c in range(2):
                        tr(b3[:, c * 128:c * 128 + P], pbf16.t[0:P, c * 128:(c + 1) * 128], identb.t[0:P, 0:P], [pbf16, identb], [bank[7]])
                    act(pT.t[:, :, 0:P], fv(b3[:, 0:1], [(128, 2), (1, P)]), AF.Copy, [bank[7]], [pT])
                    for nb in range(2):
                        for c in range(8):
                            mm(pbank(2 + nb, P), hT.t[:, c, 0:P], w_gate_b.t[:, c, nb * 512:(nb + 1) * 512], c == 0, c == 7, [hT, w_gate_b], [bank[2 + nb]])
                    for nb in range(2):
                        for c in range(2):
                            mm(pbank(4 + nb, P), pT.t[:, c, 0:P], w_proj_b.t[:, c, nb * 512:(nb + 1) * 512], c == 0, c == 1, [pT, w_proj_b], [bank[4 + nb]])

                def ple_s1(ti):
                    P = 16 if ti == 16 else 128
                    recip(rs.t[0:P, :], rs.t[0:P, :], [rs], [rs])
                    act(sg.t[0:P, :], PS[1].t[0:P, :], AF.Sigmoid, [bank[2], bank[3], rs], [sg], scale=rs.t[0:P, 0:1])

                def ple_s2(ti):
                    P = 16 if ti == 16 else 128
                    acc = accs[ti % 2]
                    tt("dve", sg.t[0:P, :], sg.t[0:P, :], PS[2].t[0:P, :], ALU.mult, [sg, bank[4], bank[5]], [sg])
                    tt("dve", yt.t[0:P, :], sg.t[0:P, :], acc.t[0:P, :], ALU.add, [sg, acc], [yt])
                    if ti < 16:
                        dma("sp", y_own[ti * 128:(ti + 1) * 128, :], yt.t[:, :], [yt], [], final=True)
                    else:
                        dma("sp", y_smp, yt.t[0:16, :], [yt], [], final=True)

                def peer(ti, prev, nxt):
                    P = 16 if ti == 16 else 128
                    h = hb_[ti % 2]
                    base_ = gcnt[0]
                    gcnt[0] += NSLOT
                    stt("dve", n2b.t[0:P, :], h.t[0:P, :], rstd2_all.t[0:P, ti:ti + 1], g2bc.t[0:P, :], ALU.mult, ALU.mult, [h, rstd2_all, g2bc], [n2b])
                    mset("dve", actv.t[0:P, :], 0.0, actv_b)

                    def vphase(bq):
                        slq = slice(bq * BS, (bq + 1) * BS)
                        tt("dve", coefg.t[0:P, slq], coef.t[0:P, slq], g_all.t[0:P, ti, slq], ALU.mult, [coef_b[bq], g_all], [coefg_b[bq]])
                        for slot in range(bq * BS, (bq + 1) * BS):
                            g_ = gb[(base_ + slot) % NB]
                            d_ = dgs[slot % 4]
                            act(d_.t[0:P, 0:P], identb.t[0:P, 0:P], AF.Copy, [identb, coefg_b[bq]], [d_], scale=coefg.t[0:P, slot:slot + 1])
                            for nb in range(2):
                                mm(pbank(nb, P), d_.t[0:P, 0:P], g_.t[0:P, 1024 + nb * 512:1024 + (nb + 1) * 512], slot == 0, slot == NSLOT - 1,
                                   [d_, g_], [bank[nb]])

                    for b8 in range(NBAT):
                        sl = slice(b8 * BS, (b8 + 1) * BS)
                        for slot in range(b8 * BS, (b8 + 1) * BS):
                            g_ = gb[(base_ + slot) % NB]
                            gather(g_, ti, slot, P)
                            stt("dve", junkb.t[0:P, :], g_.t[0:P, 0:1024], 1.0, n2b.t[0:P, :], ALU.mult, ALU.mult, [g_, n2b], [junkb, actv_b[b8]],
                                accum_out=actv.t[0:P, slot:slot + 1])
                        act(coef.t[0:P, sl], actv.t[0:P, sl], AF.Gelu_apprx_tanh, [actv_b[b8]], [coef_b[b8]])
                        if b8 == 1 and prev is not None:
                            acc_add(prev)
                        if b8 >= 1:
                            vphase(b8 - 1)
                        if b8 == 1:
                            if prev is not None:
                                ple_s0(prev)
                            if nxt is not None:
                                load3(nxt)
                        if b8 == 3 and prev is not None:
                            ple_s1(prev)
                        if b8 == 5 and prev is not None:
                            ple_s2(prev)
                    vphase(NBAT - 1)

                if p3:
                    load3(p3[0])
                prev_ = None
                for i_, ti in enumerate(p3):
                    nxt_ = p3[i_ + 1] if i_ + 1 < len(p3) else None
                    peer(ti, prev_, nxt_)
                    prev_ = ti
                if p3:
                    acc_add(prev_)
                    ple_s0(prev_); ple_s1(prev_); ple_s2(prev_)

        S.emit()
    return nc


def _consts():
    k = np.arange(128)[:, None, None]
    j = np.arange(17)[None, :, None]
    q = np.arange(128)[None, None, :]
    d = (16 - j) * 128 + q - k
    m = ((d >= 0) & (d <= 128)).astype(np.float32)
    m += ((d >= 0) & (d <= 512) & (d % 4 == 0)).astype(np.float32)
    m += ((d >= 0) & (d <= 2048) & (d % 16 == 0)).astype(np.float32)
    maskT = np.ascontiguousarray(m.reshape(128, 17 * 128))
    sel = np.zeros((128, 16, 16), np.float32)
    for b in range(16):
        sel[:, b, b] = 1.0
    iota16 = np.broadcast_to(np.arange(16, dtype=np.float32), (128, 16)).copy()
    jj = np.arange(128)[:, None]
    ii = np.arange(128)[None, :]
    triT = (jj <= ii).astype(np.float32)
    return maskT, sel.reshape(128, 256), iota16, triT


def _rope_tab(pos):
    inv = 500000.0 ** (-np.arange(0, 16, 2, dtype=np.float64) / 16)
    ang = pos.astype(np.float64)[:, None] * inv[None, :]
    return np.concatenate([np.cos(ang), np.sin(ang)], axis=1).astype(np.float32)


_NC_CACHE = {}


def make_in_maps(inp):
    f = lambda a: np.ascontiguousarray(np.asarray(a, dtype=np.float32))
    xp = f(inp["x_prompt"])[0]
    xs = f(inp["x_sample"])[:, 0]
    pp = f(inp["p_prompt"])[0, 0]
    psm = f(inp["p_sample"])[0, :, 0]
    ck = f(inp["cache_k"])[0].reshape(128, 2048, 512)
    cv = f(inp["cache_v"])[0].reshape(128, 2048, 512)
    maskT, sel, iota16, triT = _consts()
    colT = lambda g: np.ascontiguousarray(f(g).reshape(-1, 128).T)
    shared = {
        "ident": np.eye(128, dtype=np.float32), "maskT": maskT, "sel": sel, "iota16": iota16, "triT": triT,
        "g1T": colT(inp["norm1_g"][0]),
        "gabT": colT(np.concatenate([f(inp["out_norm_a_g"])[0], f(inp["out_norm_b_g"])[0]])),
        "g2T": colT(inp["norm2_g"][0]), "g3T": colT(inp["norm3_g"][0]),
        "g2row": f(inp["norm2_g"]).reshape(1, 1024),
        "gqk": np.concatenate([np.tile(f(inp["q_norm_g"])[0], 8), np.tile(f(inp["k_norm_g"])[0], 8)]).reshape(1, 1024),
        "gv": f(inp["v_norm_g"]).reshape(1, 512),
        "bsT": np.ascontiguousarray(f(inp["spatial_b"])[0].T),
        "w00": np.ascontiguousarray(f(inp["spatial_w"])[0, :, 0, 0]).reshape(1, 8),
        "b0": np.ascontiguousarray(f(inp["spatial_b"])[0, :, 0]).reshape(1, 8),
        "w_in": f(inp["w_in"])[0],
        "wsT": np.ascontiguousarray(f(inp["spatial_w"])[0].transpose(2, 0, 1)).reshape(128, 1024),
        "w_out": f(inp["w_out"])[0],
        "wqT": np.ascontiguousarray(f(inp["peer_w_query"])[0].T),
        "skT": np.ascontiguousarray(f(inp["peer_sub_keys"])[0].reshape(16, 128, 128).transpose(2, 0, 1)).reshape(128, 2048),
        "peer_u": f(inp["peer_u"])[0], "peer_v": f(inp["peer_v"])[0],
        "w_gate": f(inp["ple_w_gate"])[0], "w_proj": f(inp["ple_w_proj"])[0],
    }
    in_maps = []
    for c in range(8):
        xall = np.zeros((33 * 128, 1024), np.float32)
        if c > 0:
            xall[0:2048] = xp[2048 * (c - 1):2048 * c]
        xall[2048:4096] = xp[2048 * c:2048 * (c + 1)]
        xall[4096:4096 + 16] = xs[16 * c:16 * (c + 1)]
        pall = np.zeros((17 * 128, 256), np.float32)
        pall[0:2048] = pp[2048 * c:2048 * (c + 1)]
        pall[2048:2048 + 16] = psm[16 * c:16 * (c + 1)]
        pos = np.zeros(33 * 128, np.int64)
        pos[0:2048] = np.maximum(2048 * (c - 1) + np.arange(2048), 0)
        pos[2048:4096] = 2048 * c + np.arange(2048)
        pos[4096:] = 8192
        m = dict(shared)
        m.update({"xall": xall, "pall": pall, "cs": _rope_tab(pos),
                  "flag": np.full((128, 1), 0.0 if c == 0 else 1.0, np.float32),
                  "ck": np.ascontiguousarray(ck[16 * c:16 * (c + 1)]), "cv": np.ascontiguousarray(cv[16 * c:16 * (c + 1)])})
        in_maps.append(m)
    return in_maps


def kernel(**inp):
    if "nc" not in _NC_CACHE:
        _NC_CACHE["nc"] = build()
    nc = _NC_CACHE["nc"]
    in_maps = make_in_maps(inp)
    res = run_bass_kernel_spmd(nc, in_maps, core_ids=list(range(8)))
    R = res.results
    y_prompt = np.concatenate([np.asarray(r["y_own"]) for r in R], axis=0).reshape(1, 16384, 1024)
    y_sample = np.concatenate([np.asarray(r["y_smp"]) for r in R], axis=0).reshape(128, 1, 1024)
    nkp = np.asarray(R[7]["k_own"]).reshape(1, 1, 2048, 8, 64)
    nvp = np.asarray(R[7]["v_own"]).reshape(1, 1, 2048, 8, 64)
    cat = lambda n: np.concatenate([np.asarray(r[n]) for r in R], axis=0).reshape(1, 128, 1, 8, 64)
    return (y_prompt.astype(np.float32), y_sample.astype(np.float32), nkp.astype(np.float32), nvp.astype(np.float32),
            cat("ks").astype(np.float32), cat("vs").astype(np.float32), cat("cvs").astype(np.float32))
```

```python
import numpy as np
from contextlib import ExitStack
import concourse.bass as bass
import concourse.mybir as mybir
from concourse.bass_utils import run_bass_kernel_spmd

F32 = mybir.dt.float32
BF16 = mybir.dt.bfloat16
I32 = mybir.dt.int32
U32 = mybir.dt.uint32
AF = mybir.ActivationFunctionType
ALU = mybir.AluOpType
AX = mybir.AxisListType

ENGS = ("pe", "act", "dve", "pool", "sp")
EPS = 1e-6
NSLOT = 128
CUT = 99


class Trk:
    __slots__ = ("w", "r", "dsem", "excl")

    def __init__(self):
        self.w = None
        self.r = []
        self.dsem = None
        self.excl = False


class DSem:
    __slots__ = ("sem", "n", "bg")

    def __init__(self, sem):
        self.sem = sem
        self.n = 0
        self.bg = False


class Sched:
    def __init__(self, nc, stack):
        self.nc = nc
        self.stack = stack
        self.ops = {e: [] for e in ENGS}
        self.cnt = {e: 0 for e in ENGS}
        self.sem = {e: stack.enter_context(nc.semaphore("es_" + e)) for e in ENGS}
        self.seen = {e: {} for e in ENGS}
        self.nds = 0
        self.final = []
        self.dsems = []
        self.pending = {e: [] for e in ENGS}

    def new_dsem(self):
        self.nds += 1
        d = DSem(self.stack.enter_context(self.nc.semaphore("ds%d" % self.nds)))
        self.dsems.append(d)
        return d

    def barrier(self, include_bg=False):
        evs = [(self.sem[e], self.cnt[e]) for e in ENGS if self.cnt[e] > 0]
        evs += [(d.sem, 16 * d.n) for d in self.dsems if d.n > 0 and (include_bg or not d.bg)]
        for e in ENGS:
            own = id(self.sem[e])
            for s_, v in evs:
                k = id(s_)
                if e == "pe" and k == own:
                    continue
                if self.seen[e].get(k, 0) >= v:
                    continue
                self.seen[e][k] = v
                self.pending[e].append((s_, v))

    def _deps(self, eng, reads, writes):
        need = {}

        def add(ev):
            if ev is None:
                return
            s, v = ev
            k = id(s)
            if k not in need or need[k][1] < v:
                need[k] = (s, v)
        for t in reads:
            add(t.w)
        for t in writes:
            add(t.w)
            for ev in t.r:
                add(ev)
        waits = []
        seen = self.seen[eng]
        own = id(self.sem[eng])
        for k, (s, v) in need.items():
            if eng == "pe" and k == own:
                continue
            if seen.get(k, 0) >= v:
                continue
            seen[k] = v
            waits.append((s, v))
        if self.pending[eng]:
            waits = self.pending[eng] + waits
            self.pending[eng] = []
        return waits

    def _mark(self, ev, reads, writes):
        for t in writes:
            t.w = ev
            t.r = []
        for t in reads:
            if t not in writes:
                t.r.append(ev)
                if len(t.r) > 64:
                    t.r = t.r[-48:]

    def op(self, eng, fn, reads=(), writes=()):
        ex = [t for t in reads if t.excl and t not in writes]
        if ex:
            writes = list(writes) + ex
        waits = self._deps(eng, reads, writes)
        self.cnt[eng] += 1
        ev = (self.sem[eng], self.cnt[eng])
        self.ops[eng].append((waits, fn, (self.sem[eng], 1)))
        self._mark(ev, reads, writes)
        return ev

    def dma(self, q, fn, reads=(), writes=(), dsem=None, final=False):
        if dsem is None:
            host = writes[0] if writes else reads[0]
            if host.dsem is None:
                host.dsem = self.new_dsem()
            dsem = host.dsem
        waits = self._deps(q, reads, writes)
        if dsem.n > 0:
            k = id(dsem.sem)
            v = 16 * dsem.n
            if self.seen[q].get(k, 0) < v:
                self.seen[q][k] = v
                waits.append((dsem.sem, v))
        dsem.n += 1
        ev = (dsem.sem, 16 * dsem.n)
        self.ops[q].append((waits, fn, (dsem.sem, 16)))
        self._mark(ev, reads, writes)
        if final:
            self.final.append(ev)
        return ev

    def emit(self):
        nc = self.nc
        fin = {}
        for s, v in self.final:
            k = id(s)
            if k not in fin or fin[k][1] < v:
                fin[k] = (s, v)
        finals = list(fin.values())
        with nc.Block() as block:
            def run(engname):
                def body(e):
                    for waits, fn, (s, amt) in self.ops[engname]:
                        for ws, wv in waits:
                            e.wait_ge(ws, wv)
                        fn(e).then_inc(s, amt)
                    if engname == "sp":
                        for ws, wv in finals:
                            e.wait_ge(ws, wv)
                return body
            block.tensor(run("pe"))
            block.scalar(run("act"))
            block.vector(run("dve"))
            block.gpsimd(run("pool"))
            block.sync(run("sp"))


def fv(ap, dims):
    base = ap.ap
    return bass.AP(ap.tensor, ap.offset, [list(base[0])] + [list(d) for d in dims])


class Buf:
    def __init__(self, t):
        self.t = t
        self.k = Trk()


def build(dbg=None, slots1=None, p2=None, p3=None):
    dbg = dbg or {}
    slots1 = list(range(33)) if slots1 is None else slots1
    p2 = list(range(17)) if p2 is None else p2
    p3 = list(range(17)) if p3 is None else p3
    nc = bass.Bass("TRN2", target_bir_lowering=False)

    def din(name, shape, dt=F32):
        return nc.dram_tensor(name, list(shape), dt, kind="ExternalInput").ap()

    def dout(name, shape, dt=F32):
        return nc.dram_tensor(name, list(shape), dt, kind="ExternalOutput").ap()

    xall = din("xall", [33 * 128, 1024]); pall = din("pall", [17 * 128, 256]); cs_d = din("cs", [33 * 128, 16])
    flag_d = din("flag", [128, 1]); ck = din("ck", [16, 2048, 512]); cv = din("cv", [16, 2048, 512])
    ident_d = din("ident", [128, 128]); mask_d = din("maskT", [128, 17 * 128]); sel_d = din("sel", [128, 256])
    bsel_d = din("bsel", [128, 16])
    iota_d = din("iota16", [128, 16]); tri_d = din("triT", [128, 128])
    g1T_d = din("g1T", [128, 8]); gabT_d = din("gabT", [128, 8]); g2T_d = din("g2T", [128, 8]); g3T_d = din("g3T", [128, 8])
    g2_d = din("g2row", [1, 1024]); gqk_d = din("gqk", [1, 1024]); gv_d = din("gv", [1, 512])
    bsT_d = din("bsT", [128, 8]); w00_d = din("w00", [1, 8]); b0_d = din("b0", [1, 8])
    w_in = din("w_in", [1024, 2560]); wsT_d = din("wsT", [128, 8 * 128]); w_out = din("w_out", [1024, 1024])
    wqT_d = din("wqT", [2048, 1024]); skT_d = din("skT", [128, 16 * 128])
    peer_u = din("peer_u", [16384, 1024]); peer_v = din("peer_v", [16384, 1024])
    w_gate = din("w_gate", [1024, 1024]); w_proj = din("w_proj", [256, 1024])

    y_own = dout("y_own", [2048, 1024]); y_smp = dout("y_smp", [16, 1024])
    k_own = dout("k_own", [2048, 512]); v_own = dout("v_own", [2048, 512])
    ks_o = dout("ks", [16, 512]); vs_o = dout("vs", [16, 512]); cvs_o = dout("cvs", [16, 512])
    dbg_o = {k: dout("dbg_" + k, shp) for k, shp in dbg.items()}

    h1_d = nc.dram_tensor("h1s", [17 * 128, 1024], F32, kind="Internal").ap()
    qs_d = nc.dram_tensor("qsc", [1, 16 * 512], F32, kind="Internal").ap()
    ptab = nc.dram_tensor("ptab", [16384, 2048], BF16, kind="Internal").ap()
    sce_d = nc.dram_tensor("sc_e", [16, 128], I32, kind="Internal").ap()
    scg_d = nc.dram_tensor("sc_g", [16, 128], F32, kind="Internal").ap()
    scn_d = nc.dram_tensor("sc_n", [16, 1024], BF16, kind="Internal").ap()

    with ExitStack() as st0:
        S = Sched(nc, st0)

        def sbuf(st, name, shape, dt=F32):
            return Buf(st.enter_context(nc.sbuf_tensor("sb_" + name, list(shape), dt)))

        def K(bufs):
            return [b.k for b in bufs]

        def act(out, in_, func, r, w, **kw):
            S.op("act", lambda e: e.activation(out=out, in_=in_, func=func, **kw), K(r), K(w))

        def tt(eng, out, in0, in1, op, r, w):
            S.op(eng, lambda e: e.tensor_tensor(out=out, in0=in0, in1=in1, op=op), K(r), K(w))

        def ts(eng, out, in0, s1, s2, op0, op1, r, w):
            S.op(eng, lambda e: e.tensor_scalar(out=out, in0=in0, scalar1=s1, scalar2=s2, op0=op0, op1=op1), K(r), K(w))

        def tss(eng, out, in_, scalar, op, r, w):
            S.op(eng, lambda e: e.tensor_single_scalar(out=out, in_=in_, scalar=scalar, op=op), K(r), K(w))

        def stt(eng, out, in0, scalar, in1, op0, op1, r, w, accum_out=None):
            if accum_out is None:
                S.op(eng, lambda e: e.scalar_tensor_tensor(out=out, in0=in0, scalar=scalar, in1=in1, op0=op0, op1=op1), K(r), K(w))
            else:
                S.op(eng, lambda e: e.scalar_tensor_tensor(out=out, in0=in0, scalar=scalar, in1=in1, op0=op0, op1=op1, accum_out=accum_out), K(r), K(w))

        def cp(eng, out, in_, r, w):
            S.op(eng, lambda e: e.tensor_copy(out=out, in_=in_), K(r), K(w))

        def mset(eng, out, val, w):
            S.op(eng, lambda e: e.memset(out, val), [], K(w))

        def red(eng, out, in_, op, r, w):
            S.op(eng, lambda e: e.tensor_reduce(out=out, in_=in_, axis=AX.X, op=op), K(r), K(w))

        def recip(out, in_, r, w):
            S.op("dve", lambda e: e.reciprocal(out=out, in_=in_), K(r), K(w))

        def mm(out, lhsT, rhs, start, stop, r, w):
            S.op("pe", lambda e: e.matmul(out, lhsT=lhsT, rhs=rhs, start=start, stop=stop), K(r), K(w))

        def tr(out, in_, ident, r, w):
            S.op("pe", lambda e: e.transpose(out=out, in_=in_, identity=ident), K(r), K(w))

        def dma(q, out, in_, r, w, final=False, dsem=None):
            S.dma(q, lambda e: e.dma_start(out=out, in_=in_), K(r), K(w), final=final, dsem=dsem)

        def dbgout(name, ap, b):
            if name in dbg_o:
                dma("sp", dbg_o[name], ap, [b], [], final=True)

        ident = sbuf(st0, "ident", [128, 128]); identb = sbuf(st0, "identb", [128, 128], BF16)
        iota16 = sbuf(st0, "iota16", [128, 16])
        rstd2_all = sbuf(st0, "rstd2_all", [128, 17])
        dma("sp", ident.t[:], ident_d, [], [ident])
        dma("sp", iota16.t[:], iota_d, [], [iota16])
        act(identb.t[:], ident.t[:], AF.Copy, [ident], [identb])

        PS = [Buf(st0.enter_context(nc.psum_tensor("ps%d" % i, [128, 1024], F32))) for i in range(4)]
        bank = [Buf(None) for _ in range(8)]
        for b_ in bank:
            b_.k.excl = True

        def pbank(i, P=128, lo=0, hi=512):
            return PS[i // 2].t[0:P, (i % 2) * 512 + lo:(i % 2) * 512 + hi]

        def pbank_b(i):
            return PS[i // 2].t[:].bitcast(BF16)[:, (i % 2) * 1024:(i % 2) * 1024 + 1024]

        def rms_scale(st_small, src_ap, P, n, r, name):
            ssq, rs, junk = st_small
            mset("dve", ssq.t[0:P, :], 0.0, [ssq])
            act(junk.t[0:P, 0:n], src_ap, AF.Square, r + [ssq], [junk, ssq], accum_out=ssq.t[0:P, :])
            act(rs.t[0:P, :], ssq.t[0:P, :], AF.Sqrt, [ssq], [rs], bias=EPS, scale=1.0 / n)
            recip(rs.t[0:P, :], rs.t[0:P, :], [rs], [rs])
            return rs

        def load_weight(st_tmp, stage, w_dram, n_chunks, ncols, dst, gcol):
            for c in range(n_chunks):
                sb_ = stage[c % 2]
                dma("sp", sb_.t[:, 0:ncols], w_dram[c * 128:(c + 1) * 128, :], [], [sb_])
                if gcol is None:
                    act(dst.t[:, c, 0:ncols], sb_.t[:, 0:ncols], AF.Copy, [sb_], [dst])
                else:
                    act(dst.t[:, c, 0:ncols], sb_.t[:, 0:ncols], AF.Copy, [sb_, gcol], [dst], scale=gcol.t[:, c:c + 1])

        with ExitStack() as st1:
            S.barrier()
            w_in_b = sbuf(st1, "w_in_b", [128, 8, 2560], BF16)
            w_out_b = sbuf(st1, "w_out_b", [128, 8, 1024], BF16)
            wmT = sbuf(st1, "wmT", [128, 8, 128], BF16)
            KT = sbuf(st1, "KT", [128, 4, 32 * 128], BF16)
            VA = sbuf(st1, "VA", [128, 32, 8, 65], BF16)
            maskb = sbuf(st1, "maskb", [128, 17 * 128], BF16)
            selb = sbuf(st1, "selb", [128, 256], BF16)
            gqk = sbuf(st1, "gqk", [128, 1024]); gv = sbuf(st1, "gv", [128, 512])
            bsT = sbuf(st1, "bsT", [128, 8]); w00 = sbuf(st1, "w00", [128, 8]); b0 = sbuf(st1, "b0", [128, 8])
            g1T = sbuf(st1, "g1T", [128, 8]); gabT = sbuf(st1, "gabT", [128, 8]); flag = sbuf(st1, "flag", [128, 1])
            KTs = [Buf(None) for _ in range(32)]
            VAs = [Buf(None) for _ in range(32)]

            with ExitStack() as sts:
                S.barrier()
                stage = [sbuf(sts, "stage%d" % i, [128, 2560]) for i in range(2)]
                tri = sbuf(sts, "tri", [128, 128])
                for (dst, src) in ((g1T, g1T_d), (gabT, gabT_d), (flag, flag_d), (bsT, bsT_d), (tri, tri_d)):
                    dma("sp", dst.t[:], src, [], [dst])
                dma("sp", gqk.t[:], gqk_d.partition_broadcast(128), [], [gqk])
                dma("sp", gv.t[:], gv_d.partition_broadcast(128), [], [gv])
                dma("sp", w00.t[:], w00_d.partition_broadcast(128), [], [w00])
                dma("sp", b0.t[:], b0_d.partition_broadcast(128), [], [b0])
                load_weight(sts, stage, w_in, 8, 2560, w_in_b, g1T)
                load_weight(sts, stage, w_out, 8, 1024, w_out_b, gabT)
                dma("sp", stage[0].t[:, 0:17 * 128], mask_d, [], [stage[0]])
                act(maskb.t[:], stage[0].t[:, 0:17 * 128], AF.Copy, [stage[0]], [maskb])
                dma("sp", stage[1].t[:, 0:256], sel_d, [], [stage[1]])
                act(selb.t[:], stage[1].t[:, 0:256], AF.Copy, [stage[1]], [selb])
                dma("sp", stage[0].t[:, 0:1024], wsT_d, [], [stage[0]])
                tt("dve", wmT.t[:], fv(stage[0].t[:, 0:1], [(128, 8), (1, 128)]), fv(tri.t[:, 0:1], [(0, 8), (1, 128)]),
                   ALU.mult, [stage[0], tri], [wmT])
                if "wmT" in dbg_o:
                    act(stage[1].t[:, 0:1024], wmT.t[:].rearrange("p g i -> p (g i)"), AF.Copy, [wmT], [stage[1]])
                    dbgout("wmT", stage[1].t[:, 0:1024], stage[1])
                    dbgout("tri", tri.t[:, :], tri)
                mset("dve", VA.t[:, 16:32, :, 64:65], 1.0, [VA])
                cp("dve", VA.t[:, 0:16, :, 64], fv(flag.t[:, 0:1], [(0, 16), (0, 8)]), [flag], [VA])

            with ExitStack() as stt_:
                S.barrier()
                tabk = Buf(None)
                tsems = [S.new_dsem() for _ in range(16)]
                for d_ in tsems:
                    d_.bg = True
                kk_ = 0
                bg_pending = []
                for r0 in range(0, 16384, 1024):
                    for (src_, c0_) in ((peer_u, 0), (peer_v, 1024)):
                        def f_(e, src_=src_, c0_=c0_, r0=r0):
                            return e.dma_start(out=ptab[r0:r0 + 1024, c0_:c0_ + 1024], in_=src_[r0:r0 + 1024, :])
                        bg_pending.append((f_, tsems[kk_ % 16]))
                        kk_ += 1

                def issue_bg(n):
                    for _ in range(n):
                        if bg_pending:
                            f__, ds__ = bg_pending.pop(0)
                            S.dma("pool", f__, [], K([tabk]), dsem=ds__)
                issue_bg(16)
                xb = [sbuf(stt_, "xb%d" % i, [128, 1024]) for i in range(2)]
                csb = [sbuf(stt_, "csb%d" % i, [128, 16]) for i in range(2)]
                ssq = sbuf(stt_, "ssq", [128, 1]); rs = sbuf(stt_, "rs", [128, 1]); junk = sbuf(stt_, "junk", [128, 1024], BF16)
                small = (ssq, rs, junk)
                n1 = sbuf(stt_, "n1", [128, 1024], BF16); n1T = sbuf(stt_, "n1T", [128, 8, 128], BF16)
                sq = sbuf(stt_, "sq", [128, 1024]); qkn = sbuf(stt_, "qkn", [128, 1024]); qkb = sbuf(stt_, "qkb", [128, 1024], BF16)
                ssh = sbuf(stt_, "ssh", [128, 16]); rh = sbuf(stt_, "rh", [128, 16]); rt = sbuf(stt_, "rt", [128, 4, 16, 8])
                QT = sbuf(stt_, "QT", [128, 4, 256], BF16); vt = sbuf(stt_, "vt", [128, 512])
                mset("dve", QT.t[:, :, :], 0.0, [QT])
                ug = sbuf(stt_, "ug", [128, 1024]); vn = sbuf(stt_, "vn", [128, 512]); vnb = sbuf(stt_, "vnb", [128, 512], BF16)
                gm = sbuf(stt_, "gm", [128, 512]); ab = sbuf(stt_, "ab", [128, 1024], BF16); abT = sbuf(stt_, "abT", [128, 8, 128], BF16)
                eT = [sbuf(stt_, "eT%d" % i, [128, 512], BF16) for i in range(2)]
                rden = sbuf(stt_, "rden", [128, 8]); att = sbuf(stt_, "att", [128, 512]); h1t = sbuf(stt_, "h1t", [128, 1024])
                NBG = 2
                qbc = sbuf(stt_, "qbc", [128, NBG * 512])
                kcs = [sbuf(stt_, "kc%d" % i, [128, NBG * 512]) for i in range(2)]
                vcs = [sbuf(stt_, "vc%d" % i, [128, NBG * 512]) for i in range(2)]
                prod = Buf(None); prod.t = sq.t; prod.k = sq.k
                sc = sbuf(stt_, "sc", [128, NBG * 8]); evt = sbuf(stt_, "evt", [128, NBG, 8, 65], BF16)
                osb = Buf(None); osb.t = ug.t[:, 0:520].rearrange("p (a b c) -> p a b c", a=2, b=4); osb.k = ug.k
                sself = sbuf(stt_, "sself", [128, 8]); qsrc = Buf(None)

                def load_cs(slot):
                    P = 16 if slot == 32 else 128
                    dma("sp", csb[slot % 2].t[0:P, :], cs_d[slot * 128:slot * 128 + P, :], [], [csb[slot % 2]])

                def load_x(slot, with_cs=True):
                    b = slot % 2
                    P = 16 if slot == 32 else 128
                    dma("sp", xb[b].t[0:P, :], xall[slot * 128:slot * 128 + P, :], [], [xb[b]])
                    if with_cs:
                        load_cs(slot)

                def tile1(slot, nxt):
                    mode = "halo" if slot < 16 else ("own" if slot < 32 else "sample")
                    P = 16 if mode == "sample" else 128
                    x = xb[slot % 2]; c_s = csb[slot % 2]
                    if nxt is not None:
                        load_x(nxt)
                    r1 = rms_scale(small, x.t[0:P, :], P, 1024, [x], "r1")
                    act(n1.t[0:P, :], x.t[0:P, :], AF.Copy, [x, r1], [n1], scale=r1.t[0:P, 0:1])
                    b3 = pbank_b(3)
                    for c in range(8):
                        tr(b3[:, c * 128:c * 128 + P], n1.t[0:P, c * 128:(c + 1) * 128], identb.t[0:P, 0:P], [n1, identb], [bank[3]])
                    act(n1T.t[:, :, 0:P], fv(b3[:, 0:1], [(128, 8), (1, P)]), AF.Copy, [bank[3]], [n1T])
                    colblocks = [(1, 512), (2, 1024)] if mode == "halo" else [(0, 0), (1, 512), (2, 1024), (4, 1536), (5, 2048)]
                    for bk, col in colblocks:
                        for c in range(8):
                            mm(pbank(bk, P), n1T.t[:, c, 0:P], w_in_b.t[:, c, col:col + 512], c == 0, c == 7, [n1T, w_in_b], [bank[bk]])
                    zqk = PS[0].t[0:P, :]
                    act(sq.t[0:P, :], zqk, AF.Square, [bank[0], bank[1]], [sq])
                    red("dve", ssh.t[0:P, :], sq.t[0:P, :].rearrange("p (a b) -> p a b", a=16), ALU.add, [sq], [ssh])
                    act(rh.t[0:P, :], ssh.t[0:P, :], AF.Sqrt, [ssh], [rh], bias=EPS, scale=1.0 / 64)
                    recip(rh.t[0:P, :], rh.t[0:P, :], [rh], [rh])
                    q3 = qkn.t[0:P, :].rearrange("p (a b) -> p a b", a=16)
                    tt("dve", q3, zqk.rearrange("p (a b) -> p a b", a=16), fv(rh.t[0:P, 0:1], [(1, 16), (0, 64)]), ALU.mult,
                       [bank[0], bank[1], rh], [qkn])
                    tt("dve", qkn.t[0:P, :], qkn.t[0:P, :], gqk.t[0:P, :], ALU.mult, [qkn, gqk], [qkn])
                    x1 = q3[:, :, 0:8]; x2 = q3[:, :, 8:16]
                    cosb = fv(c_s.t[0:P, 0:1], [(0, 16), (1, 8)]); sinb = fv(c_s.t[0:P, 8:9], [(0, 16), (1, 8)])
                    tt("dve", rt.t[0:P, 0], x1, cosb, ALU.mult, [qkn, c_s], [rt])
                    tt("dve", rt.t[0:P, 1], x2, sinb, ALU.mult, [qkn, c_s], [rt])
                    tt("dve", rt.t[0:P, 2], x2, cosb, ALU.mult, [qkn, c_s], [rt])
                    tt("dve", rt.t[0:P, 3], x1, sinb, ALU.mult, [qkn, c_s], [rt])
                    tt("dve", x1, rt.t[0:P, 0], rt.t[0:P, 1], ALU.subtract, [rt], [qkn])
                    tt("dve", x2, rt.t[0:P, 2], rt.t[0:P, 3], ALU.add, [rt], [qkn])
                    act(qkb.t[0:P, :], qkn.t[0:P, :], AF.Copy, [qkn], [qkb])
                    if CUT <= -2:
                        return
                    if mode == "own":
                        o0 = (slot - 16) * 128
                        dma("sp", k_own[o0:o0 + 128, :], qkn.t[:, 512:1024], [qkn], [], final=True)
                        if CUT <= -1:
                            return
                    elif mode == "sample":
                        dma("sp", ks_o, qkn.t[0:16, 512:1024], [qkn], [], final=True)
                    crange = range(4, 8) if mode == "halo" else range(8)
                    for c in crange:
                        tr(b3[:, c * 128:c * 128 + P], qkb.t[0:P, c * 128:(c + 1) * 128], identb.t[0:P, 0:P], [qkb, identb], [bank[3]])
                    if mode == "own":
                        act(QT.t[0:64, :, 0:128], fv(b3[0:64, 0:1], [(128, 4), (1, 128)]), AF.Copy, [bank[3]], [QT])
                        act(QT.t[64:128, :, 128:256], fv(b3[64:128, 0:1], [(128, 4), (1, 128)]), AF.Copy, [bank[3]], [QT])
                    if mode != "sample":
                        cp("dve", KT.t[:, :, slot * 128:(slot + 1) * 128], fv(b3[:, 512:513], [(128, 4), (1, 128)]), [bank[3]], [KTs[slot]])
                    if CUT <= 0 and mode == "own":
                        return
                    if mode != "halo":
                        act(vt.t[0:P, :], pbank(2, P), AF.Copy, [bank[2]], [vt])
                        if mode == "own":
                            dma("sp", v_own[o0:o0 + 128, :], vt.t[:, :], [vt], [], final=True)
                        else:
                            dma("sp", vs_o, vt.t[0:16, :], [vt], [], final=True)
                    if mode != "sample":
                        cp("dve", VA.t[:, slot, :, 0:64], pbank(2, P).rearrange("p (h c) -> p h c", h=8), [bank[2]], [VAs[slot]])
                    if mode == "halo" or CUT <= 1:
                        return
                    act(ug.t[0:P, :], PS[2].t[0:P, :], AF.Gelu_apprx_tanh, [bank[4], bank[5]], [ug])
                    act(sq.t[0:P, 0:512], ug.t[0:P, 512:1024], AF.Square, [ug], [sq])
                    red("dve", ssh.t[0:P, 0:8], sq.t[0:P, 0:512].rearrange("p (a b) -> p a b", a=8), ALU.add, [sq], [ssh])
                    act(rh.t[0:P, 0:8], ssh.t[0:P, 0:8], AF.Sqrt, [ssh], [rh], bias=EPS, scale=1.0 / 64)
                    recip(rh.t[0:P, 0:8], rh.t[0:P, 0:8], [rh], [rh])
                    vn3 = vn.t[0:P, :].rearrange("p (a b) -> p a b", a=8)
                    tt("dve", vn3, ug.t[0:P, 512:1024].rearrange("p (a b) -> p a b", a=8), fv(rh.t[0:P, 0:1], [(1, 8), (0, 64)]),
                       ALU.mult, [ug, rh], [vn])
                    tt("dve", vn.t[0:P, :], vn.t[0:P, :], gv.t[0:P, :], ALU.mult, [vn, gv], [vn])
                    gm3 = gm.t[0:P, :].rearrange("p (a b) -> p a b", a=8)
                    if mode == "own":
                        act(vnb.t[0:P, :], vn.t[0:P, :], AF.Copy, [vn], [vnb])
                        for g in range(8):
                            mm(pbank(2, P, g * 64, (g + 1) * 64), wmT.t[:, g, 0:P], vnb.t[:, g * 64:(g + 1) * 64], True, True, [wmT, vnb], [bank[2]])
                        tt("dve", gm3, pbank(2, P).rearrange("p (a b) -> p a b", a=8), fv(bsT.t[0:P, 0:1], [(1, 8), (0, 64)]), ALU.add,
                           [bank[2], bsT], [gm])
                    else:
                        dma("sp", cvs_o, vn.t[0:16, :], [vn], [], final=True)
                        tt("dve", gm3, vn3, fv(w00.t[0:P, 0:1], [(1, 8), (0, 64)]), ALU.mult, [vn, w00], [gm])
                        tt("dve", gm3, gm3, fv(b0.t[0:P, 0:1], [(1, 8), (0, 64)]), ALU.add, [gm, b0], [gm])
                    if slot == 16:
                        dbgout("ug0", ug.t[:, :], ug)
                        dbgout("vn0", vn.t[:, :], vn)
                        dbgout("mix0", gm.t[:, :], gm)
                    tt("dve", gm.t[0:P, :], gm.t[0:P, :], ug.t[0:P, 0:512], ALU.mult, [gm, ug], [gm])
                    rb = rms_scale(small, gm.t[0:P, :], P, 512, [gm], "rb")
                    act(ab.t[0:P, 512:1024], gm.t[0:P, :], AF.Copy, [gm, rb], [ab], scale=rb.t[0:P, 0:1])
                    if CUT <= 2:
                        return
                    if mode == "own":
                        issue_bg(1)
                        i0 = slot - 16
                        groups = [(hp, grp) for hp in range(4) for grp in range(9)]

                        def a_scores(gi):
                            hp, grp = groups[gi]
                            j0 = grp * 2
                            nj = min(2, 17 - j0)
                            sb_i = gi % 2
                            for jj in range(nj):
                                s_ = i0 + j0 + jj
                                mm(pbank(sb_i, 128, jj * 256, (jj + 1) * 256), KT.t[:, hp, s_ * 128:(s_ + 1) * 128],
                                   QT.t[:, hp, :], True, True, [KTs[s_], QT], [bank[sb_i]])

                        def a_soft(gi):
                            hp, grp = groups[gi]
                            j0 = grp * 2
                            nj = min(2, 17 - j0)
                            sb_i = gi % 2
                            et = eT[sb_i]
                            act(et.t[:, 0:nj * 256], pbank(sb_i, 128, 0, nj * 256), AF.Exp, [bank[sb_i]], [et], scale=0.125)
                            tt("pool", et.t[:, 0:nj * 256].rearrange("p (j e q) -> p j e q", j=nj, e=2),
                               et.t[:, 0:nj * 256].rearrange("p (j e q) -> p j e q", j=nj, e=2),
                               fv(maskb.t[:, j0 * 128:j0 * 128 + 1], [(128, nj), (0, 2), (1, 128)]), ALU.mult, [et, maskb], [et])

                        def a_pv(gi):
                            hp, grp = groups[gi]
                            j0 = grp * 2
                            nj = min(2, 17 - j0)
                            et = eT[gi % 2]
                            for jj in range(nj):
                                s_ = i0 + j0 + jj
                                j = j0 + jj
                                for e_ in range(2):
                                    h = 2 * hp + e_
                                    ob = 6 + e_
                                    o_ap = pbank(ob, 128, hp * 65, hp * 65 + 65)
                                    c0 = jj * 256 + e_ * 128
                                    mm(o_ap, et.t[:, c0:c0 + 128], VA.t[:, s_, h, :], j == 0, j == 16, [et, VAs[s_], VA], [bank[ob]])

                        a_scores(0)
                        for gi in range(len(groups)):
                            if gi + 1 < len(groups):
                                a_scores(gi + 1)
                            a_soft(gi)
                            a_pv(gi)
                        o_src = fv(PS[3].t[0:128, 0:1], [(512, 2), (65, 4), (1, 65)])
                        o_r = [bank[6], bank[7]]
                        att_v = fv(att.t[0:P, 0:1], [(64, 2), (128, 4), (1, 64)])
                        rden_v = fv(rden.t[0:P, 0:1], [(1, 2), (2, 4)])
                        rden_b = fv(rden.t[0:P, 0:1], [(1, 2), (2, 4), (0, 64)])
                    else:
                        dma("sp", qs_d.rearrange("o (b f) -> (o b) f", b=16), qkn.t[0:16, 0:512], [qkn], [qsrc])
                        first = True
                        for bg in range(16 // NBG):
                            dma("sp", qbc.t[:, :], qs_d[0:1, bg * NBG * 512:(bg + 1) * NBG * 512].partition_broadcast(128), [qsrc], [qbc])
                            for pi, dil in enumerate((1, 4, 16)):
                                st_ = 2048 - 128 * dil
                                kc = kcs[(bg * 3 + pi) % 2]; vc = vcs[(bg * 3 + pi) % 2]
                                dma("sp", kc.t[:, :].rearrange("p (b f) -> p b f", b=NBG),
                                    ck[bg * NBG:(bg + 1) * NBG, st_:2048:dil, :].rearrange("b k f -> k b f"), [], [kc])
                                dma("sp", vc.t[:, :].rearrange("p (b f) -> p b f", b=NBG),
                                    cv[bg * NBG:(bg + 1) * NBG, st_:2048:dil, :].rearrange("b k f -> k b f"), [], [vc])
                                tt("pool", prod.t[:, :], kc.t[:, :], qbc.t[:, :], ALU.mult, [kc, qbc], [prod])
                                red("dve", sc.t[:, :], prod.t[:, :].rearrange("p (a b) -> p a b", a=NBG * 8), ALU.add, [prod], [sc])
                                act(sc.t[:, :], sc.t[:, :], AF.Exp, [sc], [sc], scale=0.125)
                                tt("dve", evt.t[:, :, :, 0:64], vc.t[:, :].rearrange("p (b h c) -> p b h c", b=NBG, h=8),
                                   fv(sc.t[:, 0:1], [(8, NBG), (1, 8), (0, 64)]), ALU.mult, [vc, sc], [evt])
                                cp("dve", evt.t[:, :, :, 64], sc.t[:, :].rearrange("p (b h) -> p b h", b=NBG), [sc], [evt])
                                for bb in range(NBG):
                                    bglob = bg * NBG + bb
                                    last = (bg == 16 // NBG - 1 and pi == 2 and bb == NBG - 1)
                                    for half in range(2):
                                        mm(pbank(6 + half, 16, 0, 260), selb.t[:, bglob * 16:(bglob + 1) * 16],
                                           evt.t[:, bb, half * 4:(half + 1) * 4, :].rearrange("p h c -> p (h c)"), first, last, [selb, evt], [bank[6 + half]])
                                    first = False
                        cp("dve", osb.t[0:16], fv(PS[3].t[0:16, 0:1], [(512, 2), (65, 4), (1, 65)]), [bank[6], bank[7]], [osb])
                        tt("dve", prod.t[0:16, 0:512], qkn.t[0:16, 0:512], qkn.t[0:16, 512:1024], ALU.mult, [qkn], [prod])
                        red("dve", sself.t[0:16, :], prod.t[0:16, 0:512].rearrange("p (a b) -> p a b", a=8), ALU.add, [prod], [sself])
                        act(sself.t[0:16, :], sself.t[0:16, :], AF.Exp, [sself], [sself], scale=0.125)
                        tss("dve", sself.t[0:16, :], sself.t[0:16, :], 3.0, ALU.mult, [sself], [sself])
                        o4 = osb.t[0:16].rearrange("p a b c -> p (a b) c")
                        tt("dve", prod.t[0:16, 0:512].rearrange("p (h c) -> p h c", h=8), vt.t[0:16, :].rearrange("p (h c) -> p h c", h=8),
                           fv(sself.t[0:16, 0:1], [(1, 8), (0, 64)]), ALU.mult, [vt, sself], [prod])
                        tt("dve", o4[:, :, 0:64], o4[:, :, 0:64], prod.t[0:16, 0:512].rearrange("p (h c) -> p h c", h=8), ALU.add, [osb, prod], [osb])
                        tt("dve", o4[:, :, 64], o4[:, :, 64], sself.t[0:16, :], ALU.add, [osb, sself], [osb])
                        o_src = osb.t[0:16]
                        o_r = [osb]
                        att_v = att.t[0:P, :].rearrange("p (a b c) -> p a b c", a=2, b=4)
                        rden_v = rden.t[0:P, :].rearrange("p (a b) -> p a b", a=2)
                        rden_b = fv(rden.t[0:P, 0:1], [(4, 2), (1, 4), (0, 64)])
                    if CUT <= 3:
                        return
                    recip(rden_v, o_src[:, :, :, 64], o_r, [rden])
                    tt("dve", att_v, o_src[:, :, :, 0:64], rden_b, ALU.mult, o_r + [rden], [att])
                    ra = rms_scale(small, att.t[0:P, :], P, 512, [att], "ra")
                    act(ab.t[0:P, 0:512], att.t[0:P, :], AF.Copy, [att, ra], [ab], scale=ra.t[0:P, 0:1])
                    for c in range(8):
                        tr(b3[:, c * 128:c * 128 + P], ab.t[0:P, c * 128:(c + 1) * 128], identb.t[0:P, 0:P], [ab, identb], [bank[3]])
                    act(abT.t[:, :, 0:P], fv(b3[:, 0:1], [(128, 8), (1, P)]), AF.Copy, [bank[3]], [abT])
                    for nb in range(2):
                        for c in range(8):
                            mm(pbank(4 + nb, P), abT.t[:, c, 0:P], w_out_b.t[:, c, nb * 512:(nb + 1) * 512], c == 0, c == 7, [abT, w_out_b], [bank[4 + nb]])
                    tt("dve", h1t.t[0:P, :], PS[2].t[0:P, :], x.t[0:P, :], ALU.add, [bank[4], bank[5], x], [h1t])
                    ti = slot - 16
                    dma("sp", h1_d[ti * 128:ti * 128 + P, :], h1t.t[0:P, :], [h1t], [h1s[ti]], dsem=h1sems[ti % 2])
                    if ti == 0:
                        dbgout("att0", att.t[:, :], att)
                        dbgout("gm0", gm.t[:, :], gm)
                        dbgout("h10", h1t.t[:, :], h1t)
                    if mode == "sample":
                        dbgout("atts", att.t[:, :], att)
                        dbgout("h1s_", h1t.t[:, :], h1t)

                def halo_bufs(slot):
                    return (n1, n1T) if slot % 2 == 0 else (ab, abT)

                def halo_front(slot, nxt):
                    x = xb[slot % 2]
                    if nxt is not None:
                        load_x(nxt, with_cs=False)
                    n1_, n1T_ = halo_bufs(slot)
                    r1 = rms_scale(small, x.t[:, :], 128, 1024, [x], "r1")
                    act(n1_.t[:, :], x.t[:, :], AF.Copy, [x, r1], [n1_], scale=r1.t[:, 0:1])
                    b3 = pbank_b(3)
                    for c in range(8):
                        tr(b3[:, c * 128:(c + 1) * 128], n1_.t[:, c * 128:(c + 1) * 128], identb.t[:, :], [n1_, identb], [bank[3]])
                    act(n1T_.t[:, :, :], fv(b3[:, 0:1], [(128, 8), (1, 128)]), AF.Copy, [bank[3]], [n1T_])

                def halo_back(slot):
                    c_s = csb[slot % 2]
                    n1_, n1T_ = halo_bufs(slot)
                    b3 = pbank_b(3)
                    for bk, col in ((1, 512), (2, 1024)):
                        for c in range(8):
                            mm(pbank(bk), n1T_.t[:, c, :], w_in_b.t[:, c, col:col + 512], c == 0, c == 7, [n1T_, w_in_b], [bank[bk]])
                    zk = pbank(1)
                    act(sq.t[:, 0:512], zk, AF.Square, [bank[1]], [sq])
                    red("dve", ssh.t[:, 0:8], sq.t[:, 0:512].rearrange("p (a b) -> p a b", a=8), ALU.add, [sq], [ssh])
                    act(rh.t[:, 0:8], ssh.t[:, 0:8], AF.Sqrt, [ssh], [rh], bias=EPS, scale=1.0 / 64)
                    recip(rh.t[:, 0:8], rh.t[:, 0:8], [rh], [rh])
                    k3 = qkn.t[:, 512:1024].rearrange("p (a b) -> p a b", a=8)
                    tt("dve", k3, zk.rearrange("p (a b) -> p a b", a=8), fv(rh.t[:, 0:1], [(1, 8), (0, 64)]), ALU.mult, [bank[1], rh], [qkn])
                    tt("dve", qkn.t[:, 512:1024], qkn.t[:, 512:1024], gqk.t[:, 512:1024], ALU.mult, [qkn, gqk], [qkn])
                    x1 = k3[:, :, 0:8]; x2 = k3[:, :, 8:16]
                    cosb = fv(c_s.t[:, 0:1], [(0, 8), (1, 8)]); sinb = fv(c_s.t[:, 8:9], [(0, 8), (1, 8)])
                    tt("dve", rt.t[:, 0, 0:8], x1, cosb, ALU.mult, [qkn, c_s], [rt])
                    tt("dve", rt.t[:, 1, 0:8], x2, sinb, ALU.mult, [qkn, c_s], [rt])
                    tt("dve", rt.t[:, 2, 0:8], x2, cosb, ALU.mult, [qkn, c_s], [rt])
                    tt("dve", rt.t[:, 3, 0:8], x1, sinb, ALU.mult, [qkn, c_s], [rt])
                    tt("dve", x1, rt.t[:, 0, 0:8], rt.t[:, 1, 0:8], ALU.subtract, [rt], [qkn])
                    tt("dve", x2, rt.t[:, 2, 0:8], rt.t[:, 3, 0:8], ALU.add, [rt], [qkn])
                    act(qkb.t[:, 512:1024], qkn.t[:, 512:1024], AF.Copy, [qkn], [qkb])
                    for c in range(4, 8):
                        tr(b3[:, c * 128:(c + 1) * 128], qkb.t[:, c * 128:(c + 1) * 128], identb.t[:, :], [qkb, identb], [bank[3]])
                    cp("dve", KT.t[:, :, slot * 128:(slot + 1) * 128], fv(b3[:, 512:513], [(128, 4), (1, 128)]), [bank[3]], [KTs[slot]])
                    cp("dve", VA.t[:, slot, :, 0:64], pbank(2).rearrange("p (h c) -> p h c", h=8), [bank[2]], [VAs[slot]])

                h1s = [Buf(None) for _ in range(17)]
                h1sems = [S.new_dsem() for _ in range(2)]
                if slots1 == list(range(33)):
                    load_x(0)
                    load_cs(1)
                    halo_front(0, 1)
                    for s_ in range(16):
                        if s_ + 1 < 16:
                            halo_front(s_ + 1, s_ + 2)
                        halo_back(s_)
                        if s_ + 2 <= 16:
                            load_cs(s_ + 2)
                    for slot in range(16, 33):
                        tile1(slot, slot + 1 if slot + 1 <= 32 else None)
                else:
                    if slots1:
                        load_x(slots1[0])
                    for si, slot in enumerate(slots1):
                        tile1(slot, slots1[si + 1] if si + 1 < len(slots1) else None)
                issue_bg(len(bg_pending))

        eidx_all = sbuf(st0, "eidx_all", [128, 17, NSLOT], I32)
        g_all = sbuf(st0, "g_all", [128, 17, NSLOT])
        w_gate_b = sbuf(st0, "w_gate_b", [128, 8, 1024], BF16)
        w_proj_b = sbuf(st0, "w_proj_b", [128, 2, 1024], BF16)
        g2bc = sbuf(st0, "g2bc", [128, 1024]); g3T = sbuf(st0, "g3T", [128, 8])
        with ExitStack() as st2:
            S.barrier()
            Wqk = sbuf(st2, "Wqk", [128, 8, 2048], BF16)
            Wqk_e = Buf(None); Wqk_e.t = Wqk.t
            Wqk_o = Buf(None); Wqk_o.t = Wqk.t
            g2T = sbuf(st2, "g2T", [128, 8])
            dma("sp", g2T.t[:], g2T_d, [], [g2T])
            with ExitStack() as sts:
                S.barrier()
                skT = sbuf(sts, "skT", [128, 2048])
                wq = [sbuf(sts, "wq%d" % i, [128, 1024]) for i in range(2)]
                dma("sp", skT.t[:], skT_d, [], [skT])
                for b in range(16):
                    w_ = wq[b % 2]
                    dma("sp", w_.t[:], wqT_d[b * 128:(b + 1) * 128, :], [], [w_])
                    for dc in range(8):
                        pb_i = 2 * (b % 2) + dc % 2
                        mm(pbank(pb_i, 128, (dc // 2) * 128, (dc // 2) * 128 + 128), w_.t[:, dc * 128:(dc + 1) * 128], skT.t[:, b * 128:(b + 1) * 128],
                           True, True, [w_, skT], [bank[pb_i]])
                    for dc in range(8):
                        pb_i = 2 * (b % 2) + dc % 2
                        if dc % 2 == 0:
                            act(Wqk.t[:, dc, b * 128:(b + 1) * 128], pbank(pb_i, 128, (dc // 2) * 128, (dc // 2) * 128 + 128), AF.Copy,
                                [bank[pb_i], g2T], [Wqk_e], scale=g2T.t[:, dc:dc + 1])
                        else:
                            tss("dve", Wqk.t[:, dc, b * 128:(b + 1) * 128], pbank(pb_i, 128, (dc // 2) * 128, (dc // 2) * 128 + 128),
                                g2T.t[:, dc:dc + 1], ALU.mult, [bank[pb_i], g2T], [Wqk_o])
            with ExitStack() as stt_:
                S.barrier()
                stg3 = [sbuf(stt_, "stg3_%d" % i, [128, 1024]) for i in range(2)]
                hb_ = [sbuf(stt_, "h1b%d" % i, [128, 1024]) for i in range(2)]
                ssq = sbuf(stt_, "ssq2", [128, 1]); rs = sbuf(stt_, "rs2", [128, 1]); junk = sbuf(stt_, "junk2", [128, 1024], BF16)
                hbf = sbuf(stt_, "hbf", [128, 1024], BF16); hT = sbuf(stt_, "hT", [128, 8, 128], BF16)
                ssbs = [sbuf(stt_, "ssb%d" % i, [128, 2048]) for i in range(2)]; s2 = sbuf(stt_, "s2b", [128, 2048])
                tops = sbuf(stt_, "tops", [128, 16, 16]); topi = sbuf(stt_, "topi", [128, 16, 16], U32); topf = sbuf(stt_, "topf", [128, 16, 16])
                cand = sbuf(stt_, "cand", [128, 8, 256]); cand2 = sbuf(stt_, "cand2", [128, 8, 256])
                best = sbuf(stt_, "best", [128, 8, 16]); pos = sbuf(stt_, "pos", [128, 8, 16], U32)
                pa = sbuf(stt_, "pa", [128, 8, 16], I32); pb_ = sbuf(stt_, "pb", [128, 8, 16], I32)
                paf = sbuf(stt_, "paf", [128, 8, 16]); pbf = sbuf(stt_, "pbf", [128, 8, 16])
                oh = sbuf(stt_, "oh", [128, 8, 16, 16]); e0 = sbuf(stt_, "e0", [128, 8, 16]); e1 = sbuf(stt_, "e1", [128, 8, 16])
                mx = sbuf(stt_, "mx", [128, 8]); sm = sbuf(stt_, "sm", [128, 8]); gex = sbuf(stt_, "gex", [128, 8, 16]); gsum = sbuf(stt_, "gsum", [128, 8, 16])

                def load_h(ti, bufs):
                    P = 16 if ti == 16 else 128
                    dma("sp", bufs[ti % 2].t[0:P, :], h1_d[ti * 128:ti * 128 + P, :], [h1s[ti]], [bufs[ti % 2]])

                def front2(ti, nxt):
                    P = 16 if ti == 16 else 128
                    h = hb_[ti % 2]
                    ssb = ssbs[ti % 2]
                    if nxt is not None:
                        load_h(nxt, hb_)
                    r2 = rms_scale((ssq, rs, junk), h.t[0:P, :], P, 1024, [h], "r2")
                    cp("dve", rstd2_all.t[0:P, ti:ti + 1], r2.t[0:P, :], [r2], [rstd2_all])
                    act(hbf.t[0:P, :], h.t[0:P, :], AF.Copy, [h], [hbf])
                    b3 = pbank_b(7)
                    for c in range(8):
                        tr(b3[:, c * 128:c * 128 + P], hbf.t[0:P, c * 128:(c + 1) * 128], identb.t[0:P, 0:P], [hbf, identb], [bank[7]])
                    act(hT.t[:, :, 0:P], fv(b3[:, 0:1], [(128, 8), (1, P)]), AF.Copy, [bank[7]], [hT])
                    for nb in range(4):
                        for c in range(8):
                            mm(pbank(nb, P), hT.t[:, c, 0:P], Wqk.t[:, c, nb * 512:(nb + 1) * 512], c == 0, c == 7, [hT, Wqk_e, Wqk_o], [bank[nb]])
                    for half in range(2):
                        act(ssb.t[0:P, half * 1024:(half + 1) * 1024], PS[half].t[0:P, :], AF.Copy, [bank[2 * half], bank[2 * half + 1], r2], [ssb],
                            scale=r2.t[0:P, 0:1])

                def back2(ti):
                    P = 16 if ti == 16 else 128
                    ssb = ssbs[ti % 2]

                    def top16(src, scr, g, n, vals, idxs):
                        sv = src.t[0:P, g * n:(g + 1) * n] if len(src.t.shape) == 2 else src.t[0:P, g, :]
                        s2v = scr.t[0:P, g * n:(g + 1) * n] if len(scr.t.shape) == 2 else scr.t[0:P, g, :]
                        v0 = vals.t[0:P, g, 0:8]; v1 = vals.t[0:P, g, 8:16]
                        S.op("dve", lambda e: e.max(out=v0, in_=sv), K([src]), K([vals]))
                        S.op("dve", lambda e: e.max_index(out=idxs.t[0:P, g, 0:8], in_max=v0, in_values=sv), K([src, vals]), K([idxs]))
                        S.op("dve", lambda e: e.match_replace(out=s2v, in_to_replace=v0, in_values=sv, imm_value=-1e30), K([src, vals]), K([scr]))
                        S.op("dve", lambda e: e.max(out=v1, in_=s2v), K([scr]), K([vals]))
                        S.op("dve", lambda e: e.max_index(out=idxs.t[0:P, g, 8:16], in_max=v1, in_values=s2v), K([scr, vals]), K([idxs]))

                    for g in range(16):
                        top16(ssb, s2, g, 128, tops, topi)
                    cp("dve", topf.t[0:P], topi.t[0:P], [topi], [topf])
                    c4 = cand.t[0:P].rearrange("p h (a b) -> p h a b", a=16)
                    tt("dve", c4, fv(tops.t[0:P, 0, 0:1], [(32, 8), (1, 16), (0, 16)]), fv(tops.t[0:P, 1, 0:1], [(32, 8), (0, 16), (1, 16)]),
                       ALU.add, [tops], [cand])
                    for hh in range(8):
                        top16(cand, cand2, hh, 256, best, pos)
                    tss("dve", pa.t[0:P], pos.t[0:P].bitcast(I32), 4, ALU.arith_shift_right, [pos], [pa])
                    tss("dve", pb_.t[0:P], pos.t[0:P].bitcast(I32), 15, ALU.bitwise_and, [pos], [pb_])
                    cp("dve", paf.t[0:P], pa.t[0:P], [pa], [paf])
                    cp("dve", pbf.t[0:P], pb_.t[0:P], [pb_], [pbf])
                    iob = fv(iota16.t[0:P, 0:1], [(0, 8), (0, 16), (1, 16)])
                    for (pf, pidx, eo) in ((paf, 0, e0), (pbf, 1, e1)):
                        tt("dve", oh.t[0:P], iob, fv(pf.t[0:P, 0, 0:1], [(16, 8), (1, 16), (0, 16)]), ALU.is_equal, [iota16, pf], [oh])
                        tt("dve", oh.t[0:P], oh.t[0:P], fv(topf.t[0:P, pidx, 0:1], [(32, 8), (0, 16), (1, 16)]), ALU.mult, [oh, topf], [oh])
                        red("dve", eo.t[0:P], oh.t[0:P], ALU.add, [oh], [eo])
                    stt("dve", e0.t[0:P], e0.t[0:P], 128.0, e1.t[0:P], ALU.mult, ALU.add, [e0, e1], [e0])
                    cp("dve", eidx_all.t[0:P, ti, :], e0.t[0:P].rearrange("p h k -> p (h k)"), [e0], [eidx_all])
                    red("dve", mx.t[0:P], best.t[0:P], ALU.max, [best], [mx])
                    tt("dve", best.t[0:P], best.t[0:P], fv(mx.t[0:P, 0:1], [(1, 8), (0, 16)]), ALU.subtract, [best, mx], [best])
                    act(best.t[0:P], best.t[0:P], AF.Exp, [best], [best])
                    red("dve", sm.t[0:P], best.t[0:P], ALU.add, [best], [sm])
                    recip(sm.t[0:P], sm.t[0:P], [sm], [sm])
                    tt("dve", g_all.t[0:P, ti, :].rearrange("p (h k) -> p h k", h=8), best.t[0:P], fv(sm.t[0:P, 0:1], [(1, 8), (0, 16)]),
                       ALU.mult, [best, sm], [g_all])
                    if ti == 0:
                        dbgout("s0", ssb.t[:, :], ssb)
                        dbgout("eidx0", e0.t[:].rearrange("p h k -> p (h k)"), e0)
                        dbgout("g0", g_all.t[:, 0, :], g_all)

                if p2:
                    load_h(p2[0], hb_)
                    front2(p2[0], p2[1] if len(p2) > 1 else None)
                dma("sp", g3T.t[:], g3T_d, [], [g3T])
                dma("sp", g2bc.t[:], g2_d.partition_broadcast(128), [], [g2bc])
                load_weight(stt_, stg3, w_gate, 8, 1024, w_gate_b, g3T)
                load_weight(stt_, stg3, w_proj, 2, 1024, w_proj_b, None)
                for i_, ti in enumerate(p2):
                    if i_ + 1 < len(p2):
                        front2(p2[i_ + 1], p2[i_ + 2] if i_ + 2 < len(p2) else None)
                    back2(ti)

        with ExitStack() as st3:
            S.barrier(include_bg=True)
            with ExitStack() as stt_:
                S.barrier()
                NB = 30
                hb_ = [sbuf(stt_, "h3b%d" % i, [128, 1024]) for i in range(2)]
                pbuf = [sbuf(stt_, "pbuf%d" % i, [128, 256]) for i in range(2)]
                gb = [sbuf(stt_, "gb%d" % i, [128, 2048], BF16) for i in range(NB)]
                dgs = [sbuf(stt_, "dg%d" % i, [128, 128], BF16) for i in range(4)]
                n2b = sbuf(stt_, "n2b", [128, 1024], BF16); junkb = sbuf(stt_, "junkb", [128, 1024], BF16)
                accs = [sbuf(stt_, "acc%d" % i, [128, 1024]) for i in range(2)]
                actv = sbuf(stt_, "actv", [128, NSLOT]); coef = sbuf(stt_, "coef", [128, NSLOT]); coefg = sbuf(stt_, "coefg", [128, NSLOT])
                ssq = sbuf(stt_, "ssq3", [128, 1]); rs = sbuf(stt_, "rs3", [128, 1]); junk = sbuf(stt_, "junk3", [128, 1024], BF16)
                hbf = sbuf(stt_, "hbf3", [128, 1024], BF16); hT = sbuf(stt_, "hT3", [128, 8, 128], BF16)
                pbf16 = sbuf(stt_, "pbf16", [128, 256], BF16); pT = sbuf(stt_, "pT", [128, 2, 128], BF16)
                sg = sbuf(stt_, "sg", [128, 1024]); yt = sbuf(stt_, "yt", [128, 1024])

                def load3(ti):
                    P = 16 if ti == 16 else 128
                    dma("sp", hb_[ti % 2].t[0:P, :], h1_d[ti * 128:ti * 128 + P, :], [h1s[ti]], [hb_[ti % 2]])
                    dma("sp", pbuf[ti % 2].t[0:P, :], pall[ti * 128:ti * 128 + P, :], [], [pbuf[ti % 2]])

                def gather(dstb, ti, slot, P):
                    off = eidx_all.t[0:P, ti, slot:slot + 1]
                    S.dma("pool", lambda e: e.indirect_dma_start(out=dstb.t[0:P, :], out_offset=None, in_=ptab,
                                                                in_offset=bass.IndirectOffsetOnAxis(ap=off, axis=0)),
                          K([eidx_all]), K([dstb]))

                BS = 8
                NBAT = NSLOT // BS

                def colbufs(buf):
                    out = []
                    for _ in range(NBAT):
                        b_ = Buf(None); b_.t = buf.t
                        out.append(b_)
                    return out
                actv_b = colbufs(actv); coef_b = colbufs(coef); coefg_b = colbufs(coefg)

                gcnt = [0]

                def acc_add(ti):
                    P = 16 if ti == 16 else 128
                    h = hb_[ti % 2]; acc = accs[ti % 2]
                    tt("dve", acc.t[0:P, :], PS[0].t[0:P, :], h.t[0:P, :], ALU.add, [bank[0], bank[1], h], [acc])
                    if ti == 0:
                        dbgout("h20", acc.t[:, :], acc)

                def ple_s0(ti):
                    P = 16 if ti == 16 else 128
                    pp = pbuf[ti % 2]; acc = accs[ti % 2]
                    mset("dve", ssq.t[0:P, :], 0.0, [ssq])
                    act(junk.t[0:P, :], acc.t[0:P, :], AF.Square, [acc, ssq], [junk, ssq], accum_out=ssq.t[0:P, :])
                    act(rs.t[0:P, :], ssq.t[0:P, :], AF.Sqrt, [ssq], [rs], bias=EPS, scale=1.0 / 1024)
                    act(hbf.t[0:P, :], acc.t[0:P, :], AF.Copy, [acc], [hbf])
                    act(pbf16.t[0:P, :], pp.t[0:P, :], AF.Copy, [pp], [pbf16])
                    b3 = pbank_b(7)
                    for c in range(8):
                        tr(b3[:, c * 128:c * 128 + P], hbf.t[0:P, c * 128:(c + 1) * 128], identb.t[0:P, 0:P], [hbf, identb], [bank[7]])
                    act(hT.t[:, :, 0:P], fv(b3[:, 0:1], [(128, 8), (1, P)]), AF.Copy, [bank[7]], [hT])
                    for c in range(2):
                        tr(b3[:, c * 128:c * 128 + P], pbf16.t[0:P, c * 128:(c + 1) * 128], identb.t[0:P, 0:P], [pbf16, identb], [bank[7]])
                    act(pT.t[:, :, 0:P], fv(b3[:, 0:1], [(128, 2), (1, P)]), AF.Copy, [bank[7]], [pT])
                    for nb in range(2):
                        for c in range(8):
                            mm(pbank(2 + nb, P), hT.t[:, c, 0:P], w_gate_b.t[:, c, nb * 512:(nb + 1) * 512], c == 0, c == 7, [hT, w_gate_b], [bank[2 + nb]])
                    for nb in range(2):
                        for c in range(2):
                            mm(pbank(4 + nb, P), pT.t[:, c, 0:P], w_proj_b.t[:, c, nb * 512:(nb + 1) * 512], c == 0, c == 1, [pT, w_proj_b], [bank[4 + nb]])

                def ple_s1(ti):
                    P = 16 if ti == 16 else 128
                    recip(rs.t[0:P, :], rs.t[0:P, :], [rs], [rs])
                    act(sg.t[0:P, :], PS[1].t[0:P, :], AF.Sigmoid, [bank[2], bank[3], rs], [sg], scale=rs.t[0:P, 0:1])

                def ple_s2(ti):
                    P = 16 if ti == 16 else 128
                    acc = accs[ti % 2]
                    tt("dve", sg.t[0:P, :], sg.t[0:P, :], PS[2].t[0:P, :], ALU.mult, [sg, bank[4], bank[5]], [sg])
                    tt("dve", yt.t[0:P, :], sg.t[0:P, :], acc.t[0:P, :], ALU.add, [sg, acc], [yt])
                    if ti < 16:
                        dma("sp", y_own[ti * 128:(ti + 1) * 128, :], yt.t[:, :], [yt], [], final=True)
                    else:
                        dma("sp", y_smp, yt.t[0:16, :], [yt], [], final=True)

                def peer(ti, prev, nxt):
                    P = 16 if ti == 16 else 128
                    h = hb_[ti % 2]
                    base_ = gcnt[0]
                    gcnt[0] += NSLOT
                    stt("dve", n2b.t[0:P, :], h.t[0:P, :], rstd2_all.t[0:P, ti:ti + 1], g2bc.t[0:P, :], ALU.mult, ALU.mult, [h, rstd2_all, g2bc], [n2b])
                    mset("dve", actv.t[0:P, :], 0.0, actv_b)

                    def vphase(bq):
                        slq = slice(bq * BS, (bq + 1) * BS)
                        tt("dve", coefg.t[0:P, slq], coef.t[0:P, slq], g_all.t[0:P, ti, slq], ALU.mult, [coef_b[bq], g_all], [coefg_b[bq]])
                        for slot in range(bq * BS, (bq + 1) * BS):
                            g_ = gb[(base_ + slot) % NB]
                            d_ = dgs[slot % 4]
                            act(d_.t[0:P, 0:P], identb.t[0:P, 0:P], AF.Copy, [identb, coefg_b[bq]], [d_], scale=coefg.t[0:P, slot:slot + 1])
                            for nb in range(2):
                                mm(pbank(nb, P), d_.t[0:P, 0:P], g_.t[0:P, 1024 + nb * 512:1024 + (nb + 1) * 512], slot == 0, slot == NSLOT - 1,
                                   [d_, g_], [bank[nb]])

                    for b8 in range(NBAT):
                        sl = slice(b8 * BS, (b8 + 1) * BS)
                        for slot in range(b8 * BS, (b8 + 1) * BS):
                            g_ = gb[(base_ + slot) % NB]
                            gather(g_, ti, slot, P)
                            stt("dve", junkb.t[0:P, :], g_.t[0:P, 0:1024], 1.0, n2b.t[0:P, :], ALU.mult, ALU.mult, [g_, n2b], [junkb, actv_b[b8]],
                                accum_out=actv.t[0:P, slot:slot + 1])
                        act(coef.t[0:P, sl], actv.t[0:P, sl], AF.Gelu_apprx_tanh, [actv_b[b8]], [coef_b[b8]])
                        if b8 == 1 and prev is not None:
                            acc_add(prev)
                        if b8 >= 1:
                            vphase(b8 - 1)
                        if b8 == 1:
                            if prev is not None:
                                ple_s0(prev)
                            if nxt is not None:
                                load3(nxt)
                        if b8 == 3 and prev is not None:
                            ple_s1(prev)
                        if b8 == 3 and nxt == 16:
                            sample_prep()
                        if b8 == 5 and prev is not None:
                            ple_s2(prev)
                    vphase(NBAT - 1)

                bself = sbuf(stt_, "bself", [128, 16]); bselb = sbuf(stt_, "bselb", [128, 16], BF16)
                eC = sbuf(stt_, "eC", [128, 16], I32); gC = sbuf(stt_, "gC", [128, 16]); n2rep = sbuf(stt_, "n2rep", [128, 1024], BF16)
                actvC = sbuf(stt_, "actvC", [128, 16]); coefC = sbuf(stt_, "coefC", [128, 16]); selw = [sbuf(stt_, "selw%d" % i, [128, 16], BF16) for i in range(2)]
                dma("sp", bself.t[:], bsel_d, [], [bself])
                act(bselb.t[:], bself.t[:], AF.Copy, [bself], [bselb])
                sc_e = Buf(None); sc_g = Buf(None); sc_n = Buf(None)

                n2s = sbuf(stt_, "n2s", [16, 1024], BF16)
                prep_done = [False]

                def sample_prep():
                    ti = 16
                    h = hb_[ti % 2]
                    prep_done[0] = True
                    stt("dve", n2s.t[0:16, :], h.t[0:16, :], rstd2_all.t[0:16, ti:ti + 1], g2bc.t[0:16, :], ALU.mult, ALU.mult, [h, rstd2_all, g2bc], [n2s])
                    dma("sp", sce_d, eidx_all.t[0:16, ti, :], [eidx_all], [sc_e])
                    dma("sp", scg_d, g_all.t[0:16, ti, :], [g_all], [sc_g])
                    dma("sp", scn_d, n2s.t[0:16, :], [n2s], [sc_n])
                    for t in range(16):
                        S.dma("sp", lambda e, t=t: e.dma_start(out=eC.t[8 * t:8 * t + 8, :], in_=sce_d[t, :].rearrange("(s j) -> j s", j=8),
                                                              allow_slow_non_contiguous=True), K([sc_e]), K([eC]))
                        S.dma("sp", lambda e, t=t: e.dma_start(out=gC.t[8 * t:8 * t + 8, :], in_=scg_d[t, :].rearrange("(s j) -> j s", j=8),
                                                              allow_slow_non_contiguous=True), K([sc_g]), K([gC]))
                        dma("sp", n2rep.t[8 * t:8 * t + 8, :], scn_d[t:t + 1, :].partition_broadcast(8), [sc_n], [n2rep])

                def peer_sample(prev):
                    if not prep_done[0]:
                        sample_prep()
                    base_ = gcnt[0]
                    gcnt[0] += 16
                    mset("dve", actvC.t[:, :], 0.0, [actvC])
                    for s_ in range(16):
                        g_ = gb[(base_ + s_) % NB]
                        off = eC.t[:, s_:s_ + 1]
                        S.dma("pool", lambda e, g_=g_, off=off: e.indirect_dma_start(out=g_.t[:, :], out_offset=None, in_=ptab,
                                                                                  in_offset=bass.IndirectOffsetOnAxis(ap=off, axis=0)),
                              K([eC]), K([g_]))
                        stt("dve", junkb.t[:, :], g_.t[:, 0:1024], 1.0, n2rep.t[:, :], ALU.mult, ALU.mult, [g_, n2rep], [junkb, actvC],
                            accum_out=actvC.t[:, s_:s_ + 1])
                    if prev is not None:
                        acc_add(prev)
                        ple_s0(prev)
                    act(coefC.t[:, :], actvC.t[:, :], AF.Gelu_apprx_tanh, [actvC], [coefC])
                    tt("dve", coefC.t[:, :], coefC.t[:, :], gC.t[:, :], ALU.mult, [coefC, gC], [coefC])
                    if prev is not None:
                        ple_s1(prev)
                    for s_ in range(16):
                        g_ = gb[(base_ + s_) % NB]
                        w_ = selw[s_ % 2]
                        act(w_.t[:, :], bselb.t[:, :], AF.Copy, [bselb, coefC], [w_], scale=coefC.t[:, s_:s_ + 1])
                        for nb in range(2):
                            mm(pbank(nb, 16), w_.t[:, :], g_.t[:, 1024 + nb * 512:1024 + (nb + 1) * 512], s_ == 0, s_ == 15, [w_, g_], [bank[nb]])
                    if prev is not None:
                        ple_s2(prev)

                if p3:
                    load3(p3[0])
                prev_ = None
                for i_, ti in enumerate(p3):
                    nxt_ = p3[i_ + 1] if i_ + 1 < len(p3) else None
                    if ti == 16:
                        peer_sample(prev_)
                    else:
                        peer(ti, prev_, nxt_)
                    prev_ = ti
                if p3:
                    acc_add(prev_)
                    ple_s0(prev_); ple_s1(prev_); ple_s2(prev_)

        S.emit()
    return nc


def _consts():
    k = np.arange(128)[:, None, None]
    j = np.arange(17)[None, :, None]
    q = np.arange(128)[None, None, :]
    d = (16 - j) * 128 + q - k
    m = ((d >= 0) & (d <= 128)).astype(np.float32)
    m += ((d >= 0) & (d <= 512) & (d % 4 == 0)).astype(np.float32)
    m += ((d >= 0) & (d <= 2048) & (d % 16 == 0)).astype(np.float32)
    maskT = np.ascontiguousarray(m.reshape(128, 17 * 128))
    sel = np.zeros((128, 16, 16), np.float32)
    for b in range(16):
        sel[:, b, b] = 1.0
    iota16 = np.broadcast_to(np.arange(16, dtype=np.float32), (128, 16)).copy()
    jj = np.arange(128)[:, None]
    ii = np.arange(128)[None, :]
    triT = (jj <= ii).astype(np.float32)
    return maskT, sel.reshape(128, 256), iota16, triT


def _rope_tab(pos):
    inv = 500000.0 ** (-np.arange(0, 16, 2, dtype=np.float64) / 16)
    ang = pos.astype(np.float64)[:, None] * inv[None, :]
    return np.concatenate([np.cos(ang), np.sin(ang)], axis=1).astype(np.float32)


_NC_CACHE = {}


def make_in_maps(inp):
    f = lambda a: np.ascontiguousarray(np.asarray(a, dtype=np.float32))
    xp = f(inp["x_prompt"])[0]
    xs = f(inp["x_sample"])[:, 0]
    pp = f(inp["p_prompt"])[0, 0]
    psm = f(inp["p_sample"])[0, :, 0]
    ck = f(inp["cache_k"])[0].reshape(128, 2048, 512)
    cv = f(inp["cache_v"])[0].reshape(128, 2048, 512)
    maskT, sel, iota16, triT = _consts()
    colT = lambda g: np.ascontiguousarray(f(g).reshape(-1, 128).T)
    shared = {
        "ident": np.eye(128, dtype=np.float32), "maskT": maskT, "sel": sel, "iota16": iota16, "triT": triT,
        "bsel": (np.arange(128)[:, None] // 8 == np.arange(16)[None, :]).astype(np.float32),
        "g1T": colT(inp["norm1_g"][0]),
        "gabT": colT(np.concatenate([f(inp["out_norm_a_g"])[0], f(inp["out_norm_b_g"])[0]])),
        "g2T": colT(inp["norm2_g"][0]), "g3T": colT(inp["norm3_g"][0]),
        "g2row": f(inp["norm2_g"]).reshape(1, 1024),
        "gqk": np.concatenate([np.tile(f(inp["q_norm_g"])[0], 8), np.tile(f(inp["k_norm_g"])[0], 8)]).reshape(1, 1024),
        "gv": f(inp["v_norm_g"]).reshape(1, 512),
        "bsT": np.ascontiguousarray(f(inp["spatial_b"])[0].T),
        "w00": np.ascontiguousarray(f(inp["spatial_w"])[0, :, 0, 0]).reshape(1, 8),
        "b0": np.ascontiguousarray(f(inp["spatial_b"])[0, :, 0]).reshape(1, 8),
        "w_in": f(inp["w_in"])[0],
        "wsT": np.ascontiguousarray(f(inp["spatial_w"])[0].transpose(2, 0, 1)).reshape(128, 1024),
        "w_out": f(inp["w_out"])[0],
        "wqT": np.ascontiguousarray(f(inp["peer_w_query"])[0].T),
        "skT": np.ascontiguousarray(f(inp["peer_sub_keys"])[0].reshape(16, 128, 128).transpose(2, 0, 1)).reshape(128, 2048),
        "peer_u": f(inp["peer_u"])[0], "peer_v": f(inp["peer_v"])[0],
        "w_gate": f(inp["ple_w_gate"])[0], "w_proj": f(inp["ple_w_proj"])[0],
    }
    in_maps = []
    for c in range(8):
        xall = np.zeros((33 * 128, 1024), np.float32)
        if c > 0:
            xall[0:2048] = xp[2048 * (c - 1):2048 * c]
        xall[2048:4096] = xp[2048 * c:2048 * (c + 1)]
        xall[4096:4096 + 16] = xs[16 * c:16 * (c + 1)]
        pall = np.zeros((17 * 128, 256), np.float32)
        pall[0:2048] = pp[2048 * c:2048 * (c + 1)]
        pall[2048:2048 + 16] = psm[16 * c:16 * (c + 1)]
        pos = np.zeros(33 * 128, np.int64)
        pos[0:2048] = np.maximum(2048 * (c - 1) + np.arange(2048), 0)
        pos[2048:4096] = 2048 * c + np.arange(2048)
        pos[4096:] = 8192
        m = dict(shared)
        m.update({"xall": xall, "pall": pall, "cs": _rope_tab(pos),
                  "flag": np.full((128, 1), 0.0 if c == 0 else 1.0, np.float32),
                  "ck": np.ascontiguousarray(ck[16 * c:16 * (c + 1)]), "cv": np.ascontiguousarray(cv[16 * c:16 * (c + 1)])})
        in_maps.append(m)
    return in_maps


def kernel(**inp):
    if "nc" not in _NC_CACHE:
        _NC_CACHE["nc"] = build()
    nc = _NC_CACHE["nc"]
    in_maps = make_in_maps(inp)
    res = run_bass_kernel_spmd(nc, in_maps, core_ids=list(range(8)))
    R = res.results
    y_prompt = np.concatenate([np.asarray(r["y_own"]) for r in R], axis=0).reshape(1, 16384, 1024)
    y_sample = np.concatenate([np.asarray(r["y_smp"]) for r in R], axis=0).reshape(128, 1, 1024)
    nkp = np.asarray(R[7]["k_own"]).reshape(1, 1, 2048, 8, 64)
    nvp = np.asarray(R[7]["v_own"]).reshape(1, 1, 2048, 8, 64)
    cat = lambda n: np.concatenate([np.asarray(r[n]) for r in R], axis=0).reshape(1, 128, 1, 8, 64)
    return (y_prompt.astype(np.float32), y_sample.astype(np.float32), nkp.astype(np.float32), nvp.astype(np.float32),
            cat("ks").astype(np.float32), cat("vs").astype(np.float32), cat("cvs").astype(np.float32))
```
